# Optimizing a Trainium2 kernel written in Bass

```python
import jax, jax.numpy as jnp
from jax import lax
import numpy as np

D_MODEL = 2048
BATCH = 2
SEQ = 8192
DEPTH = 4

MLA_HEADS = 8
MLA_NOPE_DIM = 128
MLA_ROPE_DIM = 64
MLA_V_DIM = 128
Q_LORA_RANK = 512
KV_LORA_RANK = 256
MLA_WIDTH = MLA_HEADS * MLA_V_DIM
RET_HEADS = 8
RET_QK_DIM = 128
RET_V_DIM = 128
RET_WIDTH = RET_HEADS * RET_V_DIM
RET_CHUNK = 128
MIX_WIDTH = MLA_WIDTH + RET_WIDTH
D_FF = 4 * D_MODEL
Q_BLOCK = 128
ROPE_BASE = 10000.0
NORM_EPS = 1e-6
GROUPNORM_EPS = 1e-6
IN_WIDTHS = (Q_LORA_RANK, KV_LORA_RANK, MLA_ROPE_DIM,
             RET_HEADS * RET_QK_DIM, RET_HEADS * RET_QK_DIM, RET_WIDTH, RET_WIDTH)
IN_WIDTH = Q_LORA_RANK + KV_LORA_RANK + MLA_ROPE_DIM + 2 * RET_HEADS * RET_QK_DIM + 2 * RET_WIDTH

kernel_name = "hybrid_mla_retention_parallel_heads"


def _split_points():
    pts, acc = [], 0
    for w in IN_WIDTHS[:-1]:
        acc += w
        pts.append(acc)
    return pts


def rmsnorm(x, g):
    xf = x.astype(jnp.float32)
    y = xf * lax.rsqrt(jnp.mean(xf * xf, axis=-1, keepdims=True) + NORM_EPS)
    return (y * g.astype(jnp.float32)).astype(x.dtype)


def rope_tables(positions, dim):
    inv_freq = ROPE_BASE ** (-jnp.arange(0, dim, 2, dtype=jnp.float32) / dim)
    ang = positions.astype(jnp.float32)[..., None] * inv_freq
    return jnp.cos(ang), jnp.sin(ang)


def apply_rope(x, cos, sin):
    half = x.shape[-1] // 2
    x1, x2 = x[..., :half], x[..., half:]
    out = jnp.concatenate([x1 * cos - x2 * sin, x2 * cos + x1 * sin], axis=-1)
    return out.astype(x.dtype)


def mla_attention(c_q, c_kv, k_rope, q_norm_g, kv_norm_g, w_uq, w_ukv, cos, sin):
    B, S, _ = c_q.shape
    H, DN, DR, DV = MLA_HEADS, MLA_NOPE_DIM, MLA_ROPE_DIM, MLA_V_DIM
    q = (rmsnorm(c_q, q_norm_g) @ w_uq).reshape(B, S, H, DN + DR)
    q_nope = q[..., :DN]
    q_rope = apply_rope(q[..., DN:], cos[:, :, None, :], sin[:, :, None, :])
    kv = (rmsnorm(c_kv, kv_norm_g) @ w_ukv).reshape(B, S, H, DN + DV)
    k_nope, v = kv[..., :DN], kv[..., DN:]
    k_r = apply_rope(k_rope, cos, sin)
    scale = (DN + DR) ** -0.5
    nb = S // Q_BLOCK
    qn_blocks = q_nope.reshape(B, nb, Q_BLOCK, H, DN).transpose(1, 0, 2, 3, 4)
    qr_blocks = q_rope.reshape(B, nb, Q_BLOCK, H, DR).transpose(1, 0, 2, 3, 4)
    key_pos = jnp.arange(S)

    def one_block(args):
        qn, qr, blk = args
        s = (jnp.einsum('bqhd,bkhd->bhqk', qn, k_nope)
             + jnp.einsum('bqhr,bkr->bhqk', qr, k_r)).astype(jnp.float32) * scale
        q_pos = blk * Q_BLOCK + jnp.arange(Q_BLOCK)
        mask = key_pos[None, :] <= q_pos[:, None]
        s = jnp.where(mask[None, None], s, -jnp.inf)
        p = jax.nn.softmax(s, axis=-1).astype(v.dtype)
        return jnp.einsum('bhqk,bkhd->bqhd', p, v)

    out = lax.map(one_block, (qn_blocks, qr_blocks, jnp.arange(nb)))
    return out.transpose(1, 0, 2, 3, 4).reshape(B, S, H * DV)


def retention(rq, rk, rv, rg, cos, sin):
    B, S, _ = rq.shape
    H, DK, DV, C = RET_HEADS, RET_QK_DIM, RET_V_DIM, RET_CHUNK
    nc = S // C
    q = apply_rope(rq.reshape(B, S, H, DK), cos[:, :, None, :], sin[:, :, None, :]).astype(jnp.float32) * DK ** -0.5
    k = apply_rope(rk.reshape(B, S, H, DK), cos[:, :, None, :], sin[:, :, None, :]).astype(jnp.float32)
    v = rv.reshape(B, S, H, DV).astype(jnp.float32)
    log_gamma = jnp.log1p(-jnp.exp2(-5.0 - jnp.arange(H, dtype=jnp.float32)))
    idx = jnp.arange(C, dtype=jnp.float32)
    rel = idx[:, None] - idx[None, :]
    decay_intra = jnp.where(rel >= 0, jnp.exp(log_gamma[:, None, None] * jnp.maximum(rel, 0.0)), 0.0)
    xi = jnp.exp(log_gamma[:, None] * (idx + 1.0))
    zeta = jnp.exp(log_gamma[:, None] * (C - 1.0 - idx))
    chunk_decay = jnp.exp(log_gamma * C)

    def to_chunks(t, d):
        return t.reshape(B, nc, C, H, d).transpose(1, 0, 3, 2, 4)

    def step(state, qkv):
        qc, kc, vc = qkv
        scores = jnp.einsum('bhqd,bhkd->bhqk', qc, kc) * decay_intra[None]
        o = (jnp.einsum('bhqk,bhkv->bhqv', scores, vc)
             + jnp.einsum('bhqd,bhdv->bhqv', qc * xi[None, :, :, None], state))
        state = (chunk_decay[None, :, None, None] * state
                 + jnp.einsum('bhkd,bhkv->bhdv', kc * zeta[None, :, :, None], vc))
        return state, o

    state0 = jnp.zeros((B, H, DK, DV), jnp.float32)
    _, o = lax.scan(step, state0, (to_chunks(q, DK), to_chunks(k, DK), to_chunks(v, DV)))
    o = o.transpose(1, 0, 3, 2, 4).reshape(B, S, H, DV)
    mu = jnp.mean(o, axis=-1, keepdims=True)
    var = jnp.mean(jnp.square(o - mu), axis=-1, keepdims=True)
    o = ((o - mu) * lax.rsqrt(var + GROUPNORM_EPS)).reshape(B, S, H * DV)
    return (o * jax.nn.silu(rg.astype(jnp.float32))).astype(rq.dtype)


def setup_inputs(seed: int = 0) -> dict:
    key = jax.random.key(seed)
    ks = jax.random.split(key, 16)
    f32 = jnp.float32

    def w(k, shape, fan_in):
        return jax.random.normal(k, shape, f32) * fan_in ** -0.5

    def gain(k, shape):
        return 1.0 + 0.02 * jax.random.normal(k, shape, f32)

    x = jax.random.normal(ks[0], (BATCH, SEQ, D_MODEL), f32)
    positions = jnp.broadcast_to(jnp.arange(SEQ, dtype=jnp.int32), (BATCH, SEQ))
    return {
        "x": x,
        "positions": positions,
        "attn_norm": gain(ks[1], (DEPTH, D_MODEL)),
        "w_in": w(ks[2], (DEPTH, D_MODEL, IN_WIDTH), D_MODEL),
        "q_norm": gain(ks[3], (DEPTH, Q_LORA_RANK)),
        "kv_norm": gain(ks[4], (DEPTH, KV_LORA_RANK)),
        "w_uq": w(ks[5], (DEPTH, Q_LORA_RANK, MLA_HEADS * (MLA_NOPE_DIM + MLA_ROPE_DIM)), Q_LORA_RANK),
        "w_ukv": w(ks[6], (DEPTH, KV_LORA_RANK, MLA_HEADS * (MLA_NOPE_DIM + MLA_V_DIM)), KV_LORA_RANK),
        "beta_attn": gain(ks[7], (DEPTH, MLA_WIDTH)),
        "beta_ret": gain(ks[8], (DEPTH, RET_WIDTH)),
        "w_o": w(ks[9], (DEPTH, MIX_WIDTH, D_MODEL), MIX_WIDTH),
        "mlp_norm": gain(ks[10], (DEPTH, D_MODEL)),
        "w_up": w(ks[11], (DEPTH, D_MODEL, D_FF), D_MODEL),
        "w_down": w(ks[12], (DEPTH, D_FF, D_MODEL), D_FF),
        "final_norm": gain(ks[13], (D_MODEL,)),
    }


def reference(x, positions, attn_norm, w_in, q_norm, kv_norm, w_uq, w_ukv, beta_attn, beta_ret,
              w_o, mlp_norm, w_up, w_down, final_norm):
    cos64, sin64 = rope_tables(positions, MLA_ROPE_DIM)
    cos128, sin128 = rope_tables(positions, RET_QK_DIM)
    split_pts = _split_points()
    for l in range(DEPTH):
        h = rmsnorm(x, attn_norm[l])
        proj = h @ w_in[l]
        c_q, c_kv, k_rope, rq, rk, rv, rg = jnp.split(proj, split_pts, axis=-1)
        a = mla_attention(c_q, c_kv, k_rope, q_norm[l], kv_norm[l], w_uq[l], w_ukv[l], cos64, sin64)
        r = retention(rq, rk, rv, rg, cos128, sin128)
        mixed = jnp.concatenate([rmsnorm(a, beta_attn[l]), r * beta_ret[l]], axis=-1)
        x = x + mixed @ w_o[l]
        h = rmsnorm(x, mlp_norm[l])
        x = x + jnp.square(jax.nn.relu(h @ w_up[l])) @ w_down[l]
    return rmsnorm(x, final_norm)
```

```python
import contextlib
import numpy as np
import concourse.bass as bass
import concourse.mybir as mybir
from concourse.bass_utils import run_bass_kernel_spmd

F32 = mybir.dt.float32
BF16 = mybir.dt.bfloat16
I32 = mybir.dt.int32
AF = mybir.ActivationFunctionType
ALU = mybir.AluOpType
AX = mybir.AxisListType

D = 2048
S = 8192
NL = 4
TOK = 2048
HALF = 1024
INW = 4928
DFF = 8192
GROUPS = [[0, 1, 2, 3], [4, 5, 6, 7]]
ATT_SCALE = 192.0 ** -0.5
EPS = 1e-6
MAGIC = 12582912.0
TWO_PI = 2.0 * np.pi
CW1 = 6.28125
CW2 = TWO_PI - 6.28125

ENGS = ("tensor", "vector", "scalar", "gpsimd", "sync")
SEM_LIMIT = 8000


class Buf:
    __slots__ = ("name", "last_w", "reads")

    def __init__(self, name):
        self.name = name
        self.last_w = None
        self.reads = []


class Op:
    __slots__ = ("eng", "fn", "deps", "kind", "sig", "has_dep", "dbuf")

    def __init__(self, eng, fn, kind):
        self.eng = eng
        self.fn = fn
        self.kind = kind
        self.deps = set()
        self.sig = None
        self.has_dep = False
        self.dbuf = None


class Prog:
    def __init__(self, nc):
        self.nc = nc
        self.ops = []
        self.by_eng = {e: [] for e in ENGS}
        self.last_of = {e: None for e in ENGS}
        self.pending = {e: set() for e in ENGS}
        self.dma_since_barrier = []

    def _add(self, eng, fn, reads, writes, kind):
        op = Op(eng, fn, kind)
        for b in reads:
            if b.last_w is not None:
                op.deps.add(b.last_w)
        for b in writes:
            if b.last_w is not None:
                op.deps.add(b.last_w)
            for r in b.reads:
                op.deps.add(r)
        for b in reads:
            b.reads.append(op)
        for b in writes:
            b.last_w = op
            b.reads = []
        op.deps.discard(op)
        if self.pending[eng]:
            op.deps |= self.pending[eng]
            self.pending[eng] = set()
        if eng == "tensor":
            op.deps = {d for d in op.deps if not (d.eng == "tensor" and d.kind == "c")}
        for d in op.deps:
            d.has_dep = True
        self.ops.append(op)
        self.by_eng[eng].append(op)
        if kind == "c":
            self.last_of[eng] = op
        else:
            self.dma_since_barrier.append(op)
        return op

    def c(self, eng, fn, reads=(), writes=()):
        return self._add(eng, fn, list(reads), list(writes), "c")

    def dma(self, eng, fn, reads=(), writes=()):
        op = self._add(eng, fn, list(reads), list(writes), "d")
        op.dbuf = writes[0]
        op.has_dep = True
        return op

    def cc(self, fn, reads=(), writes=()):
        op = self._add("gpsimd", fn, list(reads), list(writes), "cc")
        op.dbuf = writes[0]
        op.has_dep = True
        return op

    def barrier(self):
        deps = set(o for o in self.last_of.values() if o is not None) | set(self.dma_since_barrier)
        self.dma_since_barrier = []
        for e in ENGS:
            self.pending[e] |= deps

    def emit(self, final_wait_bufs=()):
        nc = self.nc
        eng_state = {e: [None, 0] for e in ENGS}
        sem_names = []

        def new_sem(tag):
            sem_names.append(tag)
            return len(sem_names) - 1

        dsem = {}
        for op in self.ops:
            if op.kind == "c":
                if op.has_dep:
                    st = eng_state[op.eng]
                    if st[0] is None or st[1] >= SEM_LIMIT:
                        st[0] = new_sem("e_" + op.eng)
                        st[1] = 0
                    st[1] += 1
                    op.sig = (st[0], st[1], 1)
            else:
                inc = 16 if op.kind == "d" else 1
                k = op.dbuf.name
                st = dsem.get(k)
                if st is None or st[1] >= SEM_LIMIT * 2:
                    st = [new_sem("d_" + op.dbuf.name), 0]
                    dsem[k] = st
                st[1] += inc
                op.sig = (st[0], st[1], inc)
        final = []
        for b in final_wait_bufs:
            st = dsem[b.name]
            final.append((st[0], st[1]))
        self.n_sems = len(sem_names)
        with contextlib.ExitStack() as es:
            handles = [es.enter_context(nc.semaphore(f"s{i}_{n}"[:40])) for i, n in enumerate(sem_names)]
            block = es.enter_context(nc.Block())

            def run(engname, eng):
                known = {}
                if engname == "sync":
                    self.pid = eng.partition_id()
                for op in self.by_eng[engname]:
                    need = {}
                    for d in op.deps:
                        s, v, _ = d.sig
                        if known.get(s, 0) >= v:
                            continue
                        if need.get(s, 0) < v:
                            need[s] = v
                    for s, v in need.items():
                        eng.wait_ge(handles[s], v)
                        known[s] = v
                    ins = op.fn(eng)
                    if op.sig is not None:
                        ins.then_inc(handles[op.sig[0]], op.sig[2])
                if engname == "sync":
                    for s, v in final:
                        eng.wait_ge(handles[s], v)

            @block.tensor
            def _(e):
                run("tensor", e)

            @block.vector
            def _(e):
                run("vector", e)

            @block.scalar
            def _(e):
                run("scalar", e)

            @block.gpsimd
            def _(e):
                run("gpsimd", e)

            @block.sync
            def _(e):
                run("sync", e)


class Tl:
    def __init__(self, ap, name):
        self.ap = ap
        self.b = Buf(name)

    def __getitem__(self, k):
        return self.ap[k]


DT_SIZE = {F32: 4, BF16: 2, I32: 4}


class Builder:
    def __init__(self, n_layers=NL, stage=99):
        self.n_layers = n_layers
        self.stage = stage
        nc = bass.Bass("TRN2", target_bir_lowering=False)
        self.nc = nc
        self.P = Prog(nc)
        self.es = contextlib.ExitStack()

    def dram_in(self, name, shape, dt):
        return Tl(self.nc.dram_tensor(name, list(shape), dt, kind="ExternalInput").ap(), name)

    def dram_out(self, name, shape, dt):
        return Tl(self.nc.dram_tensor(name, list(shape), dt, kind="ExternalOutput").ap(), name)

    def dram_tmp(self, name, shape, dt):
        return Tl(self.nc.dram_tensor(name, list(shape), dt, kind="Internal").ap(), name)

    def sb(self, name, shape, dt):
        n = 1
        for s_ in shape[1:]:
            n *= s_
        nbytes = n * DT_SIZE[dt]
        nbytes = (nbytes + 63) // 64 * 64
        off = self.aoff
        self.aoff += nbytes
        assert self.aoff <= self.asize, (name, self.aoff, self.asize)
        self.apeak = max(self.apeak, self.aoff)
        w = self.arena[0:shape[0], off // 4:(off + nbytes) // 4]
        if dt != F32:
            w = w.bitcast(dt)
        w = w[:, 0:n]
        if len(shape) == 3:
            w = w.rearrange("p (a b) -> p a b", a=shape[1])
        elif len(shape) == 4:
            w = w.rearrange("p (a b c) -> p a b c", a=shape[1], b=shape[2])
        return Tl(w, name)

    def mark(self):
        return self.aoff

    def release(self, mark):
        self.P.barrier()
        self.aoff = mark

    def MM(self, ps, out_ap, lhsT, rhs, start, stop, reads):
        self.P.c("tensor", lambda e: e.matmul(out_ap, lhsT=lhsT, rhs=rhs, start=start, stop=stop),
                 [t.b for t in reads], [ps.b])

    def TR(self, ps, out_ap, in_ap, reads):
        ident = self.ident
        self.P.c("tensor", lambda e: e.transpose(out_ap, in_ap, ident[:]),
                 [t.b for t in reads] + [ident.b], [ps.b])

    def ACT(self, out_ap, in_ap, func, reads, writes, bias=None, scale=1.0):
        if bias is None:
            fn = lambda e: e.activation(out=out_ap, in_=in_ap, func=func, scale=scale)
        else:
            fn = lambda e: e.activation(out=out_ap, in_=in_ap, func=func, bias=bias, scale=scale)
        self.P.c("scalar", fn, [t.b for t in reads], [t.b for t in writes])

    def TT(self, eng, out_ap, in0, in1, op, reads, writes):
        self.P.c(eng, lambda e: e.tensor_tensor(out=out_ap, in0=in0, in1=in1, op=op),
                 [t.b for t in reads], [t.b for t in writes])

    def TS(self, eng, out_ap, in0, s1, op0, reads, writes, s2=None, op1=None):
        if op1 is None:
            fn = lambda e: e.tensor_scalar(out=out_ap, in0=in0, scalar1=s1, scalar2=None, op0=op0)
        else:
            fn = lambda e: e.tensor_scalar(out=out_ap, in0=in0, scalar1=s1, scalar2=s2, op0=op0, op1=op1)
        self.P.c(eng, fn, [t.b for t in reads], [t.b for t in writes])

    def STT(self, eng, out_ap, in0, scalar, in1, op0, op1, reads, writes):
        self.P.c(eng, lambda e: e.scalar_tensor_tensor(out=out_ap, in0=in0, scalar=scalar, in1=in1, op0=op0, op1=op1),
                 [t.b for t in reads], [t.b for t in writes])

    def CP(self, eng, out_ap, in_ap, reads, writes):
        if eng == "scalar":
            self.ACT(out_ap, in_ap, AF.Copy, reads, writes)
        else:
            self.P.c(eng, lambda e: e.tensor_copy(out=out_ap, in_=in_ap), [t.b for t in reads], [t.b for t in writes])

    def RED(self, eng, out_ap, in_ap, reads, writes):
        self.P.c(eng, lambda e: e.tensor_reduce(out=out_ap, in_=in_ap, axis=AX.X, op=ALU.add),
                 [t.b for t in reads], [t.b for t in writes])

    def RCP(self, out_ap, in_ap, reads, writes):
        self.P.c("vector", lambda e: e.reciprocal(out=out_ap, in_=in_ap), [t.b for t in reads], [t.b for t in writes])

    def MEMSET(self, eng, ap, val, writes):
        self.P.c(eng, lambda e: e.memset(ap, val), [], [t.b for t in writes])

    def DMA(self, out_ap, in_ap, reads, writes, eng="sync"):
        self.P.dma(eng, lambda e: e.dma_start(out=out_ap, in_=in_ap), [t.b for t in reads], [t.b for t in writes])

    def rstd_from(self, out_tl, out_ap, ps, ps_ap):
        self.ACT(out_ap, ps_ap, AF.Sqrt, [ps, self.cst], [out_tl], bias=self.cst[:, 0:1])
        self.RCP(out_ap, out_ap, [out_tl], [out_tl])

    def wunit(self, wt, src_ap, a, b_):
        st = self.stg[self.stg_i % len(self.stg)]
        self.stg_i += 1
        sl = self.wsl[self.wsl_i % len(self.wsl)]
        self.wsl_i += 1
        sv = st[:, 0:a * b_].rearrange("p (a b) -> p a b", a=a)
        wv = sl[:, 0:a * b_].rearrange("p (a b) -> p a b", a=a)
        self.DMA(st[:, 0:a * b_], src_ap[:, 0:a * b_], [wt], [st])
        self.cast_i += 1
        self.CP("gpsimd", wv, sv, [st], [sl])
        return sl, wv

    def stream(self, units, consume, depth=3):
        n = len(units)
        got = []
        for k in range(min(depth, n)):
            got.append(self.wunit(*units[k]))
        for k in range(n):
            if k + depth < n:
                got.append(self.wunit(*units[k + depth]))
            consume(k, got[k][0], got[k][1])

    def build(self):
        nc = self.nc
        P = self.P
        L = self.n_layers
        es = self.es
        with es:
            self._build(nc, P, L, es)
        return nc

    def _build(self, nc, P, L, es):
        xT = self.dram_in("xT", [16, 128, TOK], F32)
        pos_own = self.dram_in("pos_own", [128, 16], I32)
        pos_all = self.dram_in("pos_all", [128, 64], I32)
        w_uq = self.dram_in("w_uq_my", [NL, 512, 384], F32)
        w_ukv = self.dram_in("w_ukv_my", [NL, 256, 512], F32)
        wspec = {"w_in": 40, "w_o": 16, "w_up": 64, "w_down": 64}
        wfl = {}
        for nm, nu in wspec.items():
            t_ = self.nc.dram_tensor(nm + "_t", [NL, nu * 128, 2048], F32, kind="ExternalInput").ap()
            wfl[nm] = [Tl(t_[i], f"{nm}_t{i}") for i in range(NL)]

        def bounce_weights(l_):
            pass

        def gather_weights(l_, names):
            pass
        gains = self.dram_in("gains", [128, 256], F32)
        cfs = self.dram_in("cfs", [128, 2048], F32)
        outT = self.dram_out("outT", [16, 128, TOK], F32)
        xres = self.dram_tmp("xres", [16, 128, TOK], F32)
        sc_q = self.dram_tmp("sc_q", [16, 128, 1024], BF16)
        sc_k = self.dram_tmp("sc_k", [16, 128, 1024], BF16)
        sc_v = self.dram_tmp("sc_v", [16, 128, 1024], BF16)
        sc_g = self.dram_tmp("sc_g", [16, 128, 1024], BF16)
        b_lat = [self.dram_tmp(f"b_lat{i}", [7 * 128, 512], BF16) for i in range(4)]
        g_lat = [self.dram_tmp(f"g_lat{i}", [4 * 7 * 128, 512], BF16) for i in range(4)]
        b_st = self.dram_tmp("b_st", [128, 1024], F32)
        g_st = self.dram_tmp("g_st", [512, 1024], F32)
        b_att = [self.dram_tmp(f"b_att{i}", [256, 1024], BF16) for i in range(8)]
        g_att_all = self.nc.dram_tensor("g_att", [8, 4 * 256, 1024], BF16, kind="Internal").ap()
        g_att = [Tl(g_att_all[i], f"g_att{i}") for i in range(8)]

        self.asize = 207 * 1024
        self.arena = es.enter_context(nc.sbuf_tensor("arena", [128, self.asize // 4], F32))
        self.aoff = 0
        self.apeak = 0
        banks = [Tl(es.enter_context(nc.psum_tensor(f"bank{i}", [128, 512], F32)), f"bank{i}") for i in range(8)]
        self.banks = banks

        gn = self.sb("gains", [128, 256], F32)
        cst = self.sb("cst", [128, 16], F32)
        self.cst = cst
        ident = self.sb("ident", [128, 128], BF16)
        self.ident = ident
        ones = self.sb("ones", [128, 4, 128], BF16)
        one1 = self.sb("one1", [128, 128], BF16)
        mask = self.sb("mask", [128, 128], BF16)
        wq = self.sb("wq", [128, 8], F32)
        wk = self.sb("wk", [128, 8], F32)
        gtab = self.sb("gtab", [128, 8, 128], F32)
        coef = self.sb("coef", [128, 4, 8], F32)
        cosR = self.sb("cosR", [128, 16, 64], F32)
        sinR = self.sb("sinR", [128, 16, 64], F32)
        cosK = self.sb("cosK", [128, 16, 32], F32)
        sinK = self.sb("sinK", [128, 16, 32], F32)
        cosQ = self.sb("cosQ", [128, 64, 32], BF16)
        sinQ = self.sb("sinQ", [128, 64, 32], BF16)
        Sst = self.sb("Sst", [128, 8, 128], F32)
        Sbf = self.sb("Sbf", [128, 8, 128], BF16)
        self.stg = [self.sb(f"stg{i}", [128, 2048], F32) for i in range(2)]
        self.wsl = [self.sb(f"wsl{i}", [128, 2048], BF16) for i in range(6)]
        self.stg_i = 0
        self.wsl_i = 0
        self.cast_i = 0
        base_mark = self.mark()

        self.DMA(gn[:], gains[:], [gains], [gn])
        G_ATT, G_MLP, G_QN, G_KVN, G_BA, G_BR, G_FIN = 0, 64, 128, 144, 152, 184, 216

        ctmp = self.sb("ctmp", [128, 2048], F32)
        self.DMA(ctmp[:], cfs[:], [cfs], [ctmp])
        self.CP("vector", ident[:], ctmp[:, 0:128], [ctmp], [ident])
        self.CP("vector", mask[:], ctmp[:, 128:256], [ctmp], [mask])
        self.CP("vector", wq[:], ctmp[:, 256:264], [ctmp], [wq])
        self.CP("vector", wk[:], ctmp[:, 264:272], [ctmp], [wk])
        self.CP("vector", gtab[:], ctmp[:, 272:1296].rearrange("p (a b) -> p a b", a=8), [ctmp], [gtab])
        self.CP("vector", coef[:], ctmp[:, 1296:1328].rearrange("p (a b) -> p a b", a=4), [ctmp], [coef])
        self.MEMSET("gpsimd", cst[:, 0:1], EPS, [cst])
        self.MEMSET("gpsimd", cst[:, 1:2], 0.0, [cst])
        for i, v in enumerate([1.0 / 2048, 1.0 / 512, 1.0 / 256, 1.0 / 1024]):
            self.MEMSET("gpsimd", ones[:, i, :], v, [ones])
        self.MEMSET("gpsimd", one1[:], 1.0, [one1])
        self.MEMSET("gpsimd", Sst[:], 0.0, [Sst])

        def rope_table(pos_dram, nt, invf_ap, nf, cos_t, sin_t):
            m = self.mark()
            pi_ = self.sb("pos_i", [128, nt], I32)
            pf = self.sb("pos_f", [128, nt], F32)
            ang = self.sb("ang", [128, nt, nf], F32)
            u = self.sb("u", [128, nt, nf], F32)
            r = self.sb("r", [128, nt, nf], F32)
            self.DMA(pi_[:], pos_dram[:], [pos_dram], [pi_])
            self.CP("vector", pf[:], pi_[:], [pi_], [pf])
            self.TT("vector", ang[:], invf_ap.unsqueeze(1).to_broadcast([128, nt, nf]),
                    pf[:].unsqueeze(2).to_broadcast([128, nt, nf]), ALU.mult, [ctmp, pf], [ang])
            for which, dst in ((0, sin_t), (1, cos_t)):
                if which == 1:
                    self.TS("vector", ang[:], ang[:], float(np.pi / 2), ALU.add, [ang], [ang])
                self.TS("vector", u[:], ang[:], float(1.0 / TWO_PI), ALU.mult, [ang], [u])
                self.TS("vector", u[:], u[:], MAGIC, ALU.add, [u], [u])
                self.TS("vector", u[:], u[:], MAGIC, ALU.subtract, [u], [u])
                self.STT("vector", r[:], u[:], -CW1, ang[:], ALU.mult, ALU.add, [u, ang], [r])
                self.STT("vector", r[:], u[:], -CW2, r[:], ALU.mult, ALU.add, [u, r], [r])
                self.TS("vector", r[:], r[:], -3.1415925, ALU.max, [r], [r], s2=3.1415925, op1=ALU.min)
                self.ACT(dst[:], r[:], AF.Sin, [r], [dst])
            self.release(m)

        rope_table(pos_own, 16, ctmp[:, 1360:1424], 64, cosR, sinR)
        rope_table(pos_own, 16, ctmp[:, 1328:1360], 32, cosK, sinK)
        rope_table(pos_all, 64, ctmp[:, 1328:1360], 32, cosQ, sinQ)

        bounce_weights(0)
        gather_weights(0, ["w_in", "w_o", "w_up", "w_down"])
        for kc in range(16):
            self.DMA(xres[kc], xT[kc], [xT], [xres])
        self.release(base_mark)
        if self.stage <= 0:
            for kc in range(16):
                self.DMA(outT[kc], xres[kc], [xres], [outT])
            P.emit(final_wait_bufs=[outT.b])
            return

        bank_i = [0]

        def nb():
            bk = banks[bank_i[0] % 8]
            bank_i[0] += 1
            return bk

        def rope_tm(src, nh, hd, cos_ap, sin_ap, out_tl, out_view, scale_ap, tmp):
            src_tl, src_ap = src
            h2 = hd // 2
            xs, t1, t2 = tmp
            xsv = xs[:, 0:nh * hd].rearrange("p (a b) -> p a b", a=nh)
            t1v = t1[:, 0:nh * h2].rearrange("p (a b) -> p a b", a=nh)
            t2v = t2[:, 0:nh * h2].rearrange("p (a b) -> p a b", a=nh)
            if scale_ap is not None:
                self.TT("vector", xsv, src_ap, scale_ap.unsqueeze(2).to_broadcast([128, nh, hd]), ALU.mult,
                        [src_tl, wq, wk], [xs])
            else:
                self.CP("scalar", xsv, src_ap, [src_tl], [xs])
            cb = cos_ap.unsqueeze(1).to_broadcast([128, nh, h2])
            sbb = sin_ap.unsqueeze(1).to_broadcast([128, nh, h2])
            tabs = [cosR, sinR, cosK, sinK, cosQ, sinQ]
            x1 = xsv[:, :, 0:h2]
            x2 = xsv[:, :, h2:hd]
            self.TT("vector", t1v, x1, cb, ALU.mult, [xs] + tabs, [t1])
            self.TT("gpsimd", t2v, x2, sbb, ALU.mult, [xs] + tabs, [t2])
            self.TT("vector", out_view[:, :, 0:h2], t1v, t2v, ALU.subtract, [t1, t2], [out_tl])
            self.TT("vector", t1v, x2, cb, ALU.mult, [xs] + tabs, [t1])
            self.TT("gpsimd", t2v, x1, sbb, ALU.mult, [xs] + tabs, [t2])
            self.TT("vector", out_view[:, :, h2:hd], t1v, t2v, ALU.add, [t1, t2], [out_tl])

        for l in range(L):
            lmark = self.mark()
            w_in, w_o, w_up, w_down = wfl["w_in"][l], wfl["w_o"][l], wfl["w_up"][l], wfl["w_down"][l]
            if l + 1 < L:
                bounce_weights(l + 1)
            def wu(wt_, u_):
                return wt_.ap[u_ * 128:(u_ + 1) * 128, :]
            self.MEMSET("gpsimd", Sst[:], 0.0, [Sst])
            for h in range(2):
                m = self.mark()
                tsl = slice(h * HALF, (h + 1) * HALF)
                xb = self.sb("xb", [128, 16, HALF], BF16)
                rstd = self.sb("rstd", [128, HALF], F32)
                xring = [self.sb(f"xr{i}", [128, HALF], F32) for i in range(3)]
                sqr = [self.sb(f"sq{i}", [128, HALF], BF16) for i in range(2)]
                bA, bB = banks[0], banks[1]
                for kc in range(16):
                    xr = xring[kc % 3]
                    sq = sqr[kc % 2]
                    self.DMA(xr[:], xres[kc][:, tsl], [xres], [xr])
                    self.ACT(sq[:], xr[:], AF.Square, [xr], [sq])
                    for tt, bk in ((0, bA), (1, bB)):
                        self.MM(bk, bk[:], ones[:, 0, :], sq[:, tt * 512:(tt + 1) * 512], kc == 0, kc == 15, [ones, sq])
                for tt, bk in ((0, bA), (1, bB)):
                    self.rstd_from(rstd, rstd[:, tt * 512:(tt + 1) * 512], bk, bk[:])
                for kc in range(16):
                    xr = xring[(kc + 1) % 3]
                    self.DMA(xr[:], xres[kc][:, tsl], [xres], [xr])
                    self.STT("vector", xb[:, kc, :], xr[:],
                             gn[:, G_ATT + l * 16 + kc:G_ATT + l * 16 + kc + 1], rstd[:], ALU.mult, ALU.mult,
                             [xr, gn, rstd], [xb])
                lat = self.sb("lat", [128, 4, HALF], F32)
                lsq = self.sb("lsq", [128, HALF], BF16)
                lrs = self.sb("lrs", [128, HALF], F32)
                bnc = self.sb("bnc", [128, 7, HALF], BF16)
                for (c0, nch, onei, gcol, boff) in ((0, 4, 1, G_QN + l * 4, 0), (512, 2, 2, G_KVN + l * 2, 4)):
                    units = [(w_in, wu(w_in, boff + j), 16, 128) for j in range(nch)]

                    def cons(k, sl, wv, nch=nch):
                        for tt in range(2):
                            bk = nb()
                            for kc in range(16):
                                self.MM(bk, bk[:], wv[:, kc, :], xb[:, kc, tt * 512:(tt + 1) * 512], kc == 0, kc == 15, [sl, xb])
                            self.CP("scalar", lat[:, k, tt * 512:(tt + 1) * 512], bk[:], [bk], [lat])
                    self.stream(units, cons)
                    for tt in range(2):
                        bk = nb()
                        for j in range(nch):
                            self.ACT(lsq[:, tt * 512:(tt + 1) * 512], lat[:, j, tt * 512:(tt + 1) * 512], AF.Square, [lat], [lsq])
                            self.MM(bk, bk[:], ones[:, onei, :], lsq[:, tt * 512:(tt + 1) * 512], j == 0, j == nch - 1, [ones, lsq])
                        self.rstd_from(lrs, lrs[:, tt * 512:(tt + 1) * 512], bk, bk[:])
                    for j in range(nch):
                        self.STT("vector", bnc[:, boff + j, :], lat[:, j, :], gn[:, gcol + j:gcol + j + 1], lrs[:],
                                 ALU.mult, ALU.mult, [lat, gn, lrs], [bnc])
                xs_t = self.sb("xs_t", [128, 512], F32)
                t1_t = self.sb("t1_t", [128, 256], F32)
                t2_t = self.sb("t2_t", [128, 256], F32)
                ktm = self.sb("ktm", [128, 8, 1024], BF16)
                orow = [self.sb(f"orow{i}", [128, 512], BF16) for i in range(2)]
                krt = self.sb("krt", [128, 128], BF16)
                oi = [0]
                gotk = self.wunit(w_in, wu(w_in, 6), 16, 64)
                for t in range(8):
                    bk = nb()
                    for kc in range(16):
                        self.MM(bk, bk[:, 0:64], xb[:, kc, t * 128:(t + 1) * 128], gotk[1][:, kc, :], kc == 0, kc == 15,
                                [xb, gotk[0]])
                    gt = h * 8 + t
                    rope_tm((bk, bk[:, 0:64].rearrange("p (a b) -> p a b", a=1)), 1, 64, cosK[:, gt, :], sinK[:, gt, :],
                            krt, krt[:, 0:64].rearrange("p (a b) -> p a b", a=1), None, (xs_t, t1_t, t2_t))
                    self.CP("gpsimd", krt[:, 64:128], krt[:, 0:64], [krt], [krt])
                    bk2 = nb()
                    bv = bk2[:, 0:64].bitcast(BF16)
                    self.TR(bk2, bv, krt[:], [krt])
                    self.CP("scalar", bnc[:, 6, t * 128:(t + 1) * 128], bv, [bk2], [bnc])
                for tt in range(2):
                    gtt = h * 2 + tt
                    for c_ in range(7):
                        self.DMA(b_lat[gtt][c_ * 128:(c_ + 1) * 128, :],
                                 bnc[:, c_, tt * 512:(tt + 1) * 512], [bnc], [b_lat[gtt]])
                for gi in range(8):
                    kind = gi // 2
                    hg = gi % 2
                    units = [(w_in, wu(w_in, 7 + gi * 4 + j), 4, 512) for j in range(4)]
                    got = [self.wunit(*u) for u in units]
                    for t in range(8):
                        gt = h * 8 + t
                        bk = nb()
                        for kc in range(16):
                            self.MM(bk, bk[:], xb[:, kc, t * 128:(t + 1) * 128], got[kc // 4][1][:, kc % 4, :], kc == 0, kc == 15,
                                    [xb, got[kc // 4][0]])
                        bk3 = bk[:].rearrange("p (a b) -> p a b", a=4)
                        if kind == 0:
                            o = orow[oi[0] % 2]
                            oi[0] += 1
                            rope_tm((bk, bk3), 4, 128, cosR[:, gt, :], sinR[:, gt, :], o,
                                    o[:].rearrange("p (a b) -> p a b", a=4), wq[:, hg * 4:hg * 4 + 4], (xs_t, t1_t, t2_t))
                            self.DMA(sc_q[gt][:, hg * 512:(hg + 1) * 512], o[:], [o], [sc_q])
                        elif kind == 1:
                            rope_tm((bk, bk3), 4, 128, cosR[:, gt, :], sinR[:, gt, :], ktm,
                                    ktm[:, t, hg * 512:(hg + 1) * 512].rearrange("p (a b) -> p a b", a=4),
                                    wk[:, hg * 4:hg * 4 + 4], (xs_t, t1_t, t2_t))
                            self.DMA(sc_k[gt][:, hg * 512:(hg + 1) * 512], ktm[:, t, hg * 512:(hg + 1) * 512], [ktm], [sc_k])
                        elif kind == 2:
                            o = orow[oi[0] % 2]
                            oi[0] += 1
                            self.CP("scalar", o[:], bk[:], [bk], [o])
                            self.DMA(sc_v[gt][:, hg * 512:(hg + 1) * 512], o[:], [o], [sc_v])
                            bm = nb()
                            for hh in range(4):
                                self.MM(bm, bm[:, hh * 128:(hh + 1) * 128], ktm[:, t, (hg * 4 + hh) * 128:(hg * 4 + hh + 1) * 128],
                                        o[:, hh * 128:(hh + 1) * 128], True, True, [ktm, o])
                            sv = Sst[:, hg * 4:hg * 4 + 4, :]
                            self.TT("vector", sv, sv, bm[:].rearrange("p (a b) -> p a b", a=4), ALU.add, [Sst, bm], [Sst])
                            self.TT("vector", sv, sv, gtab[:, hg * 4:hg * 4 + 4, :], ALU.mult, [Sst, gtab], [Sst])
                        else:
                            o = orow[oi[0] % 2]
                            oi[0] += 1
                            self.ACT(o[:], bk[:], AF.Silu, [bk], [o])
                            self.DMA(sc_g[gt][:, hg * 512:(hg + 1) * 512], o[:], [o], [sc_g])
                self.release(m)
            self.DMA(b_st[:], Sst[:].rearrange("p a b -> p (a b)"), [Sst], [b_st])
            for i_ in range(4):
                P.cc(lambda e, i_=i_: e.collective_compute("AllGather", ALU.bypass, replica_groups=GROUPS, ins=[b_lat[i_].ap], outs=[g_lat[i_].ap]),
                     [b_lat[i_].b], [g_lat[i_].b])
            P.cc(lambda e: e.collective_compute("AllGather", ALU.bypass, replica_groups=GROUPS, ins=[b_st.ap], outs=[g_st.ap]),
                 [b_st.b], [g_st.b])
            if l + 1 < L:
                gather_weights(l + 1, ["w_in", "w_o"])
            if self.stage == 1:
                for kc in range(16):
                    self.DMA(outT[kc], xres[kc], [xres], [outT])
                P.emit(final_wait_bufs=[outT.b, g_st.b] + [g_lat[i_].b for i_ in range(4)])
                return
            m = self.mark()
            wuq = self.sb("wuq", [128, 4, 384], BF16)
            wukv = self.sb("wukv", [128, 2, 512], BF16)
            wtmp = self.sb("wtmp", [128, 4, 384], F32)
            for kc in range(4):
                self.DMA(wtmp[:, kc, :], w_uq[l][kc * 128:(kc + 1) * 128, :], [w_uq], [wtmp])
            self.CP("gpsimd", wuq[:], wtmp[:], [wtmp], [wuq])
            wtmp2 = self.sb("wtmp2", [128, 2, 512], F32)
            for kc in range(2):
                self.DMA(wtmp2[:, kc, :], w_ukv[l][kc * 128:(kc + 1) * 128, :], [w_ukv], [wtmp2])
            self.CP("gpsimd", wukv[:], wtmp2[:], [wtmp2], [wukv])
            KT = self.sb("KT", [128, 2, S], BF16)
            KR = self.sb("KR", [128, S], BF16)
            VV = self.sb("VV", [128, 64, 256], BF16)
            latr = [self.sb(f"latr{i}", [128, 6, 512], BF16) for i in range(2)]
            QN = [self.sb(f"QN{i}", [128, 2, 512], BF16) for i in range(2)]
            QR = [self.sb(f"QR{i}", [128, 512], BF16) for i in range(2)]
            qrt = [self.sb(f"qrt{i}", [128, 128], BF16) for i in range(2)]
            PT = [self.sb(f"PT{i}", [128, 512], BF16) for i in range(3)]
            rden = self.sb("rden", [128, 512], F32)
            ao = [self.sb(f"ao{i}", [128, 512], BF16) for i in range(2)]
            xs_t = self.sb("xs_t", [128, 512], F32)
            t1_t = self.sb("t1_t", [128, 256], F32)
            t2_t = self.sb("t2_t", [128, 256], F32)
            pti = [0]
            aoi = [0]
            KTr = [Tl(KT.ap, f"KT{q}") for q in range(16)]
            KRr = [Tl(KR.ap, f"KR{q}") for q in range(16)]
            VVr = [Tl(VV.ap, f"VV{q}") for q in range(16)]
            g_lat_vs = [g_lat[i_].ap.rearrange("(r c p) t -> r p c t", c=7, p=128) for i_ in range(4)]
            for qt in range(16):
                lt = latr[qt % 2]
                qn = QN[qt % 2]
                qr = QR[qt % 2]
                glv = g_lat_vs[qt % 4][qt // 4]
                for c_ in range(6):
                    self.DMA(lt[:, c_, :], glv[:, c_, :], [g_lat[qt % 4]], [lt])
                self.DMA(KR[:, qt * 512:(qt + 1) * 512], glv[:, 6, :], [g_lat[qt % 4]], [KRr[qt]])
                for hh in range(2):
                    bk = nb()
                    for kc in range(2):
                        self.MM(bk, bk[:], wukv[:, kc, hh * 128:(hh + 1) * 128], lt[:, 4 + kc, :], kc == 0, kc == 1, [wukv, lt])
                    self.CP("scalar" if hh == 0 else "vector", KT[:, hh, qt * 512:(qt + 1) * 512], bk[:], [bk], [KTr[qt]])
                for j in range(4):
                    bk = nb()
                    for kc in range(2):
                        self.MM(bk, bk[:, 0:256], lt[:, 4 + kc, j * 128:(j + 1) * 128], wukv[:, kc, 256:512], kc == 0, kc == 1, [wukv, lt])
                    self.CP("vector" if j % 2 == 0 else "scalar", VV[:, qt * 4 + j, :], bk[:, 0:256], [bk], [VVr[qt]])
                for hh in range(2):
                    bk = nb()
                    for kc in range(4):
                        self.MM(bk, bk[:], wuq[:, kc, hh * 128:(hh + 1) * 128], lt[:, kc, :], kc == 0, kc == 3, [wuq, lt])
                    self.CP("scalar" if hh == 0 else "vector", qn[:, hh, :], bk[:], [bk], [qn])
                for j in range(4):
                    bk = nb()
                    for kc in range(4):
                        self.MM(bk, bk[:, 0:128], lt[:, kc, j * 128:(j + 1) * 128], wuq[:, kc, 256:384], kc == 0, kc == 3, [wuq, lt])
                    qq = qrt[j % 2]
                    rope_tm((bk, bk[:, 0:128].rearrange("p (a b) -> p a b", a=2)), 2, 64, cosQ[:, qt * 4 + j, :], sinQ[:, qt * 4 + j, :],
                            qq, qq[:].rearrange("p (a b) -> p a b", a=2), None, (xs_t, t1_t, t2_t))
                    bk2 = nb()
                    bv = bk2[:, 0:64].bitcast(BF16)
                    self.TR(bk2, bv, qq[:], [qq])
                    self.CP("scalar", qr[:, j * 128:(j + 1) * 128], bv, [bk2], [qr])
                nkt = 4 * qt + 4
                for hh in range(2):
                    OUT = banks[6] if hh == 0 else banks[4]
                    DEN = banks[7] if hh == 0 else banks[5]
                    for kt in range(nkt):
                        r_ = kt - 4 * qt
                        c0 = 0 if r_ <= 0 else r_ * 128
                        sbk = banks[kt % 4]
                        self.MM(sbk, sbk[:, c0:512], KT[:, hh, kt * 128:(kt + 1) * 128], qn[:, hh, c0:512], True, False, [KTr[kt // 4], qn])
                        self.MM(sbk, sbk[:, c0:512], KR[hh * 64:(hh + 1) * 64, kt * 128:(kt + 1) * 128], qr[hh * 64:(hh + 1) * 64, c0:512],
                                False, True, [KRr[kt // 4], qr])
                        pt = PT[pti[0] % 3]
                        pti[0] += 1
                        self.ACT(pt[:, c0:512], sbk[:, c0:512], AF.Exp, [sbk], [pt], scale=ATT_SCALE)
                        if r_ >= 0:
                            self.TT("gpsimd", pt[:, c0:c0 + 128], pt[:, c0:c0 + 128], mask[:], ALU.mult, [pt, mask], [pt])
                        self.MM(OUT, OUT[:, c0:512], VV[:, kt, hh * 128:(hh + 1) * 128], pt[:, c0:512], kt == 0, kt == nkt - 1, [VVr[kt // 4], pt])
                        self.MM(DEN, DEN[:, c0:512], one1[:], pt[:, c0:512], kt == 0, kt == nkt - 1, [one1, pt])
                    self.RCP(rden[:], DEN[:], [DEN], [rden])
                    a_ = ao[aoi[0] % 2]
                    aoi[0] += 1
                    self.TT("vector", a_[:], OUT[:], rden[:], ALU.mult, [OUT, rden], [a_])
                    blk = qt // 2
                    self.DMA(b_att[blk][hh * 128:(hh + 1) * 128, (qt % 2) * 512:(qt % 2 + 1) * 512], a_[:], [a_], [b_att[blk]])
                bank_i[0] = 0
            self.release(m)
            for i_ in range(8):
                P.cc(lambda e, i_=i_: e.collective_compute("AllGather", ALU.bypass, replica_groups=GROUPS, ins=[b_att[i_].ap], outs=[g_att[i_].ap]),
                     [b_att[i_].b], [g_att[i_].b])
            if l + 1 < L:
                gather_weights(l + 1, ["w_up", "w_down"])
            if self.stage == 2:
                for kc in range(16):
                    self.DMA(outT[kc], xres[kc], [xres], [outT])
                P.emit(final_wait_bufs=[outT.b] + [g_att[i_].b for i_ in range(8)])
                return
            m = self.mark()
            gs = self.sb("gs", [128, 4, 1024], F32)
            for r_ in range(4):
                self.DMA(gs[:, r_, :], g_st.ap[r_ * 128:(r_ + 1) * 128, :], [g_st], [gs])
            stmp = self.sb("stmp", [128, 8, 128], F32)
            for r_ in range(4):
                dst = Sst if r_ == 0 else stmp
                self.TT("vector", dst[:], gs[:, r_, :].rearrange("p (a b) -> p a b", a=8),
                        coef[:, r_, :].unsqueeze(2).to_broadcast([128, 8, 128]), ALU.mult, [gs, coef], [dst])
                if r_ > 0:
                    self.TT("vector", Sst[:], Sst[:], stmp[:], ALU.add, [Sst, stmp], [Sst])
            self.CP("vector", Sbf[:], Sst[:], [Sst], [Sbf])
            self.release(m)
            for h in range(2):
                m = self.mark()
                tsl = slice(h * HALF, (h + 1) * HALF)
                xacc = self.sb("xacc", [128, 16, HALF], F32)
                mixed = self.sb("mixed", [128, 16, HALF], BF16)
                for kc in range(16):
                    self.DMA(xacc[:, kc, :], xres[kc][:, tsl], [xres], [xacc])
                m2 = self.mark()
                rin = [[self.sb(f"rin{i}_{j}", [128, 1024], BF16) for j in range(4)] for i in range(2)]
                QTt = self.sb("QTt", [128, 8, 128], BF16)
                KTt = self.sb("KTt", [128, 8, 128], BF16)
                PTt = self.sb("PTt", [128, 8, 128], BF16)
                osb = self.sb("osb", [128, 8, 128], F32)
                osq = self.sb("osq", [128, 8, 128], F32)
                rr = self.sb("rr", [128, 8, 128], BF16)
                st8 = self.sb("st8", [128, 4, 8], F32)
                for t in range(8):
                    gt = h * 8 + t
                    q_, k_, v_, g_ = rin[t % 2]
                    self.DMA(q_[:], sc_q[gt], [sc_q], [q_])
                    self.DMA(k_[:], sc_k[gt], [sc_k], [k_])
                    self.DMA(v_[:], sc_v[gt], [sc_v], [v_])
                    self.DMA(g_[:], sc_g[gt], [sc_g], [g_])
                    bq, bkk = banks[0], banks[1]
                    bqv = bq[:].bitcast(BF16)
                    bkv = bkk[:].bitcast(BF16)
                    for hh in range(8):
                        self.TR(bq, bqv[:, hh * 128:(hh + 1) * 128], q_[:, hh * 128:(hh + 1) * 128], [q_])
                    for hh in range(8):
                        self.TR(bkk, bkv[:, hh * 128:(hh + 1) * 128], k_[:, hh * 128:(hh + 1) * 128], [k_])
                    self.CP("scalar", QTt[:].rearrange("p a b -> p (a b)"), bqv, [bq], [QTt])
                    self.CP("vector", KTt[:].rearrange("p a b -> p (a b)"), bkv, [bkk], [KTt])
                    for half8 in range(2):
                        bs = banks[2 + half8]
                        for hh in range(4):
                            hd_ = half8 * 4 + hh
                            self.MM(bs, bs[:, hh * 128:(hh + 1) * 128], KTt[:, hd_, :], QTt[:, hd_, :], True, True, [KTt, QTt])
                        self.TT("vector", PTt[:, half8 * 4:half8 * 4 + 4, :], bs[:].rearrange("p (a b) -> p a b", a=4),
                                mask[:].unsqueeze(1).to_broadcast([128, 4, 128]), ALU.mult, [bs, mask], [PTt])
                    for half8 in range(2):
                        bo = banks[4 + half8]
                        for hh in range(4):
                            hd_ = half8 * 4 + hh
                            self.MM(bo, bo[:, hh * 128:(hh + 1) * 128], PTt[:, hd_, :], v_[:, hd_ * 128:(hd_ + 1) * 128], True, False, [PTt, v_])
                            self.MM(bo, bo[:, hh * 128:(hh + 1) * 128], QTt[:, hd_, :], Sbf[:, hd_, :], False, True, [QTt, Sbf])
                        self.CP("scalar", osb[:, half8 * 4:half8 * 4 + 4, :], bo[:].rearrange("p (a b) -> p a b", a=4), [bo], [osb])
                    for half8 in range(2):
                        bm = banks[6 + half8]
                        for hh in range(4):
                            hd_ = half8 * 4 + hh
                            self.MM(bm, bm[:, hh * 128:(hh + 1) * 128], k_[:, hd_ * 128:(hd_ + 1) * 128], v_[:, hd_ * 128:(hd_ + 1) * 128],
                                    True, True, [k_, v_])
                        sv = Sst[:, half8 * 4:half8 * 4 + 4, :]
                        self.TT("vector", sv, sv, bm[:].rearrange("p (a b) -> p a b", a=4), ALU.add, [Sst, bm], [Sst])
                        self.TT("vector", sv, sv, gtab[:, half8 * 4:half8 * 4 + 4, :], ALU.mult, [Sst, gtab], [Sst])
                    self.CP("gpsimd", Sbf[:], Sst[:], [Sst], [Sbf])
                    self.RED("vector", st8[:, 0, :], osb[:], [osb], [st8])
                    self.ACT(osq[:], osb[:], AF.Square, [osb], [osq])
                    self.RED("vector", st8[:, 1, :], osq[:], [osq], [st8])
                    self.TS("vector", st8[:, 0, :], st8[:, 0, :], 1.0 / 128, ALU.mult, [st8], [st8])
                    self.TT("vector", st8[:, 2, :], st8[:, 0, :], st8[:, 0, :], ALU.mult, [st8], [st8])
                    self.STT("vector", st8[:, 1, :], st8[:, 1, :], 1.0 / 128, st8[:, 2, :], ALU.mult, ALU.subtract, [st8], [st8])
                    self.ACT(st8[:, 3, :], st8[:, 1, :], AF.Sqrt, [st8, cst], [st8], bias=cst[:, 0:1])
                    self.RCP(st8[:, 3, :], st8[:, 3, :], [st8], [st8])
                    self.TT("vector", osb[:], osb[:], st8[:, 0, :].unsqueeze(2).to_broadcast([128, 8, 128]), ALU.subtract, [osb, st8], [osb])
                    self.TT("gpsimd", osb[:], osb[:], st8[:, 3, :].unsqueeze(2).to_broadcast([128, 8, 128]), ALU.mult, [osb, st8], [osb])
                    self.TT("vector", rr[:].rearrange("p a b -> p (a b)"), osb[:].rearrange("p a b -> p (a b)"), g_[:], ALU.mult, [osb, g_], [rr])
                    br_ = banks[0]
                    brv = br_[:].bitcast(BF16)
                    for hh in range(8):
                        self.TR(br_, brv[:, hh * 128:(hh + 1) * 128], rr[:, hh, :], [rr])
                    self.TT("vector", mixed[:, 8:16, t * 128:(t + 1) * 128], brv.rearrange("p (a b) -> p a b", a=8),
                            gn[:, G_BR + l * 8:G_BR + l * 8 + 8].unsqueeze(2).to_broadcast([128, 8, 128]), ALU.mult, [br_, gn], [mixed])
                self.release(m2)
                m2 = self.mark()
                araw = self.sb("araw", [128, 8, HALF], BF16)
                asq = self.sb("asq", [128, 512], BF16)
                ars = self.sb("ars", [128, HALF], F32)
                g_att_v = g_att_all.rearrange("b (r c p) t -> b p (r c) t", r=4, c=2, p=128)
                for j_ in range(8):
                    def dyn(e, j_=j_, h=h):
                        blk = (P.pid % 4) * 2 + h
                        return e.dma_start(out=araw[:, j_, :], in_=g_att_v[bass.ds(blk, 1)].rearrange("o p j t -> p (o j) t")[:, j_, :])
                    P.dma("sync", dyn, [t_.b for t_ in g_att], [araw.b])
                for tt in range(2):
                    bk = nb()
                    for j in range(8):
                        self.ACT(asq[:], araw[:, j, tt * 512:(tt + 1) * 512], AF.Square, [araw], [asq])
                        self.MM(bk, bk[:], ones[:, 3, :], asq[:], j == 0, j == 7, [ones, asq])
                    self.rstd_from(ars, ars[:, tt * 512:(tt + 1) * 512], bk, bk[:])
                for j in range(8):
                    self.STT("vector", mixed[:, j, :], araw[:, j, :], gn[:, G_BA + l * 8 + j:G_BA + l * 8 + j + 1], ars[:],
                             ALU.mult, ALU.mult, [araw, gn, ars], [mixed])
                self.release(m2)
                m2 = self.mark()
                units = [(w_o, wu(w_o, oc), 16, 128) for oc in range(16)]

                def cons_o(k, sl, wv):
                    for tt in range(2):
                        bk = nb()
                        for mc in range(16):
                            self.MM(bk, bk[:], wv[:, mc, :], mixed[:, mc, tt * 512:(tt + 1) * 512], mc == 0, mc == 15, [sl, mixed])
                        xa = xacc[:, k, tt * 512:(tt + 1) * 512]
                        self.TT("vector", xa, xa, bk[:], ALU.add, [xacc, bk], [xacc])
                self.stream(units, cons_o)
                xb = self.sb("xb2", [128, 16, HALF], BF16) if False else None
                self.release(m2)
                m2 = self.mark()
                sq2 = [self.sb(f"sq2_{i}", [128, 512], BF16) for i in range(2)]
                rstd2 = self.sb("rstd2", [128, HALF], F32)
                xb = mixed
                for tt in range(2):
                    bk = nb()
                    for kc in range(16):
                        sq = sq2[kc % 2]
                        self.ACT(sq[:], xacc[:, kc, tt * 512:(tt + 1) * 512], AF.Square, [xacc], [sq])
                        self.MM(bk, bk[:], ones[:, 0, :], sq[:], kc == 0, kc == 15, [ones, sq])
                    self.rstd_from(rstd2, rstd2[:, tt * 512:(tt + 1) * 512], bk, bk[:])
                for kc in range(16):
                    self.STT("vector", xb[:, kc, :], xacc[:, kc, :],
                             gn[:, G_MLP + l * 16 + kc:G_MLP + l * 16 + kc + 1], rstd2[:], ALU.mult, ALU.mult,
                             [xacc, gn, rstd2], [xb])
                aT = [self.sb(f"aT{i}", [128, 4, HALF], BF16) for i in range(2)]
                rl = [self.sb(f"rl{i}", [128, 512], BF16) for i in range(2)]
                rli = [0]

                def up_group(grp):
                    a_ = aT[grp % 2]
                    units = [(w_up, wu(w_up, grp * 4 + j), 16, 128) for j in range(4)]

                    def cons_u(k, sl, wv):
                        for tt in range(2):
                            bk = nb()
                            for kc in range(16):
                                self.MM(bk, bk[:], wv[:, kc, :], xb[:, kc, tt * 512:(tt + 1) * 512], kc == 0, kc == 15, [sl, xb])
                            r1 = rl[rli[0] % 2]
                            rli[0] += 1
                            self.ACT(r1[:], bk[:], AF.Relu, [bk], [r1])
                            self.TT("vector", a_[:, k, tt * 512:(tt + 1) * 512], r1[:], r1[:], ALU.mult, [r1], [a_])
                    self.stream(units, cons_u, depth=2)

                def down_group(grp):
                    a_ = aT[grp % 2]
                    units = [(w_down, wu(w_down, grp * 4 + j), 16, 128) for j in range(4)]
                    got = [self.wunit(*u) for u in units]
                    for oc in range(16):
                        for tt in range(2):
                            bk = nb()
                            for j in range(4):
                                self.MM(bk, bk[:], got[j][1][:, oc, :], a_[:, j, tt * 512:(tt + 1) * 512], j == 0, j == 3, [got[j][0], a_])
                            xa = xacc[:, oc, tt * 512:(tt + 1) * 512]
                            self.TT("vector", xa, xa, bk[:], ALU.add, [xacc, bk], [xacc])

                up_group(0)
                for grp in range(16):
                    if grp + 1 < 16:
                        up_group(grp + 1)
                    down_group(grp)
                if l < L - 1:
                    for kc in range(16):
                        self.DMA(xres[kc][:, tsl], xacc[:, kc, :], [xacc], [xres])
                else:
                    for tt in range(2):
                        bk = nb()
                        for kc in range(16):
                            sq = sq2[kc % 2]
                            self.ACT(sq[:], xacc[:, kc, tt * 512:(tt + 1) * 512], AF.Square, [xacc], [sq])
                            self.MM(bk, bk[:], ones[:, 0, :], sq[:], kc == 0, kc == 15, [ones, sq])
                        self.rstd_from(rstd2, rstd2[:, tt * 512:(tt + 1) * 512], bk, bk[:])
                    for kc in range(16):
                        self.STT("vector", xacc[:, kc, :], xacc[:, kc, :],
                                 gn[:, G_FIN + kc:G_FIN + kc + 1], rstd2[:], ALU.mult, ALU.mult, [xacc, gn, rstd2], [xacc])
                        self.DMA(outT[kc][:, tsl], xacc[:, kc, :], [xacc], [outT])
                self.release(m2)
                self.release(m)
            self.release(lmark)
        P.emit(final_wait_bufs=[outT.b])


def _consts(g):
    c = np.zeros((128, 2048), np.float32)
    c[:, 0:128] = np.eye(128, dtype=np.float32)
    k = np.arange(128)[:, None]
    q = np.arange(128)[None, :]
    c[:, 128:256] = (q >= k).astype(np.float32)
    hh = np.arange(8, dtype=np.float64)
    gamma = 1.0 - np.exp2(-5.0 - hh)
    t = np.arange(128, dtype=np.float64)[:, None]
    c[:, 256:264] = (gamma[None, :] ** (t + 1.0)) * (128.0 ** -0.5)
    c[:, 264:272] = gamma[None, :] ** (-(t + 1.0))
    c[:, 272:1296] = np.repeat((gamma ** 128.0)[None, :], 128, axis=1).reshape(1, 1024).repeat(128, 0) if False else \
        np.broadcast_to(np.repeat(gamma ** 128.0, 128)[None, :], (128, 1024))
    coef = np.zeros((4, 8), np.float64)
    for i in range(4):
        if i < g:
            coef[i] = gamma ** (2048.0 * (g - 1 - i))
    c[:, 1296:1328] = np.broadcast_to(coef.reshape(1, 32), (128, 32))
    invf64 = (np.float32(10000.0) ** (-np.arange(0, 64, 2, dtype=np.float32) / np.float32(64))).astype(np.float32)
    invf128 = (np.float32(10000.0) ** (-np.arange(0, 128, 2, dtype=np.float32) / np.float32(128))).astype(np.float32)
    c[:, 1328:1360] = invf64[None, :]
    c[:, 1360:1424] = invf128[None, :]
    return c


def _gains(attn_norm, mlp_norm, q_norm, kv_norm, beta_attn, beta_ret, final_norm):
    gcols = np.zeros((128, 256), np.float32)
    for l in range(NL):
        gcols[:, 0 + l * 16:0 + (l + 1) * 16] = attn_norm[l].reshape(16, 128).T
        gcols[:, 64 + l * 16:64 + (l + 1) * 16] = mlp_norm[l].reshape(16, 128).T
        gcols[:, 128 + l * 4:128 + (l + 1) * 4] = q_norm[l].reshape(4, 128).T
        gcols[:, 144 + l * 2:144 + (l + 1) * 2] = kv_norm[l].reshape(2, 128).T
        gcols[:, 152 + l * 8:152 + (l + 1) * 8] = beta_attn[l].reshape(8, 128).T
        gcols[:, 184 + l * 8:184 + (l + 1) * 8] = beta_ret[l].reshape(8, 128).T
    gcols[:, 216:232] = final_norm.reshape(16, 128).T
    return gcols


_NC_CACHE = {}


def kernel(x, positions, attn_norm, w_in, q_norm, kv_norm, w_uq, w_ukv, beta_attn, beta_ret,
           w_o, mlp_norm, w_up, w_down, final_norm, _n_layers=NL, _stage=99):
    x = np.asarray(x, np.float32)
    positions = np.asarray(positions, np.int32)
    w_in = np.ascontiguousarray(np.asarray(w_in, np.float32))
    w_uq = np.asarray(w_uq, np.float32)
    w_ukv = np.asarray(w_ukv, np.float32)
    w_o = np.ascontiguousarray(np.asarray(w_o, np.float32))
    w_up = np.ascontiguousarray(np.asarray(w_up, np.float32))
    w_down = np.ascontiguousarray(np.asarray(w_down, np.float32))
    gains = _gains(*[np.asarray(a, np.float32) for a in (attn_norm, mlp_norm, q_norm, kv_norm, beta_attn, beta_ret, final_norm)])
    if (_n_layers, _stage) not in _NC_CACHE:
        _NC_CACHE[(_n_layers, _stage)] = Builder(_n_layers, _stage).build()
    nc = _NC_CACHE[(_n_layers, _stage)]
    def fm_units(w, ncolblk):
        nl, kdim, _ = w.shape
        kc_n = kdim // 128
        t = w[:, :, :ncolblk * 128].reshape(nl, kc_n, 128, ncolblk, 128).transpose(0, 3, 2, 1, 4)
        return t.reshape(nl, ncolblk, 128, kc_n * 128)
    w_in_t = np.zeros((NL, 40, 128, 2048), np.float32)
    w_in_t[:, 0:6] = fm_units(w_in[:, :, 0:768], 6)
    w_in_t[:, 6, :, 0:1024] = w_in[:, :, 768:832].reshape(NL, 16, 128, 64).transpose(0, 2, 1, 3).reshape(NL, 128, 1024)
    for gi in range(8):
        c0 = 832 + gi * 512
        blk = w_in[:, :, c0:c0 + 512].reshape(NL, 4, 4, 128, 512).transpose(0, 1, 3, 2, 4)
        w_in_t[:, 7 + gi * 4:7 + gi * 4 + 4] = blk.reshape(NL, 4, 128, 2048)
    w_in_t = w_in_t.reshape(NL, 40 * 128, 2048)
    w_o_t = np.ascontiguousarray(fm_units(w_o, 16)).reshape(NL, 16 * 128, 2048)
    w_up_t = np.ascontiguousarray(fm_units(w_up, 64)).reshape(NL, 64 * 128, 2048)
    in_maps = []
    for c in range(8):
        b, g = c // 4, c % 4
        xs = x[b, g * TOK:(g + 1) * TOK, :]
        xT = np.ascontiguousarray(xs.T).reshape(16, 128, TOK)
        po = np.ascontiguousarray(positions[b, g * TOK:(g + 1) * TOK].reshape(16, 128).T)
        pa = np.ascontiguousarray(positions[b].reshape(64, 128).T)
        h0, h1 = 2 * g, 2 * g + 1
        wuq_my = np.ascontiguousarray(np.concatenate(
            [w_uq[:, :, h0 * 192:h0 * 192 + 128], w_uq[:, :, h1 * 192:h1 * 192 + 128],
             w_uq[:, :, h0 * 192 + 128:(h0 + 1) * 192], w_uq[:, :, h1 * 192 + 128:(h1 + 1) * 192]], axis=2))
        wukv_my = np.ascontiguousarray(np.concatenate(
            [w_ukv[:, :, h0 * 256:h0 * 256 + 128], w_ukv[:, :, h1 * 256:h1 * 256 + 128],
             w_ukv[:, :, h0 * 256 + 128:(h0 + 1) * 256], w_ukv[:, :, h1 * 256 + 128:(h1 + 1) * 256]], axis=2))
        in_maps.append({
            "xT": xT, "pos_own": po, "pos_all": pa, "w_uq_my": wuq_my, "w_ukv_my": wukv_my,
            "w_in_t": w_in_t, "w_o_t": w_o_t, "w_up_t": w_up_t, "w_down_t": w_down,
            "gains": gains, "cfs": _consts(g),
        })
    res = run_bass_kernel_spmd(nc, in_maps, core_ids=list(range(8)))
    out = np.empty((2, S, D), np.float32)
    for c in range(8):
        b, g = c // 4, c % 4
        oT = np.asarray(res.results[c]["outT"]).reshape(D, TOK)
        out[b, g * TOK:(g + 1) * TOK, :] = oT.T
    return out
```

```python
import contextlib
import numpy as np
import concourse.bass as bass
import concourse.mybir as mybir
from concourse.bass_utils import run_bass_kernel_spmd

F32 = mybir.dt.float32
BF16 = mybir.dt.bfloat16
I32 = mybir.dt.int32
AF = mybir.ActivationFunctionType
ALU = mybir.AluOpType
AX = mybir.AxisListType

D = 2048
S = 8192
NL = 4
TOK = 2048
HALF = 1024
INW = 4928
DFF = 8192
GROUPS = [[0, 1, 2, 3], [4, 5, 6, 7]]
ATT_SCALE = 192.0 ** -0.5
EPS = 1e-6
MAGIC = 12582912.0
TWO_PI = 2.0 * np.pi
CW1 = 6.28125
CW2 = TWO_PI - 6.28125

ENGS = ("tensor", "vector", "scalar", "gpsimd", "sync")
SEM_LIMIT = 8000


class Buf:
    __slots__ = ("name", "last_w", "reads")

    def __init__(self, name):
        self.name = name
        self.last_w = None
        self.reads = []


class Op:
    __slots__ = ("eng", "fn", "deps", "kind", "sig", "has_dep", "dbuf")

    def __init__(self, eng, fn, kind):
        self.eng = eng
        self.fn = fn
        self.kind = kind
        self.deps = set()
        self.sig = None
        self.has_dep = False
        self.dbuf = None


class Prog:
    def __init__(self, nc):
        self.nc = nc
        self.ops = []
        self.by_eng = {e: [] for e in ENGS}
        self.last_of = {e: None for e in ENGS}
        self.pending = {e: set() for e in ENGS}
        self.dma_since_barrier = []

    def _add(self, eng, fn, reads, writes, kind):
        op = Op(eng, fn, kind)
        for b in reads:
            if b.last_w is not None:
                op.deps.add(b.last_w)
        for b in writes:
            if b.last_w is not None:
                op.deps.add(b.last_w)
            for r in b.reads:
                op.deps.add(r)
        for b in reads:
            b.reads.append(op)
        for b in writes:
            b.last_w = op
            b.reads = []
        op.deps.discard(op)
        if self.pending[eng]:
            op.deps |= self.pending[eng]
            self.pending[eng] = set()
        if eng == "tensor":
            op.deps = {d for d in op.deps if not (d.eng == "tensor" and d.kind == "c")}
        for d in op.deps:
            d.has_dep = True
        self.ops.append(op)
        self.by_eng[eng].append(op)
        if kind == "c":
            self.last_of[eng] = op
        else:
            self.dma_since_barrier.append(op)
        return op

    def c(self, eng, fn, reads=(), writes=()):
        return self._add(eng, fn, list(reads), list(writes), "c")

    def dma(self, eng, fn, reads=(), writes=()):
        op = self._add(eng, fn, list(reads), list(writes), "d")
        op.dbuf = writes[0]
        op.has_dep = True
        return op

    def cc(self, fn, reads=(), writes=()):
        op = self._add("gpsimd", fn, list(reads), list(writes), "cc")
        op.dbuf = writes[0]
        op.has_dep = True
        return op

    def barrier(self):
        deps = set(o for o in self.last_of.values() if o is not None) | set(self.dma_since_barrier)
        self.dma_since_barrier = []
        for e in ENGS:
            self.pending[e] |= deps

    def emit(self, final_wait_bufs=()):
        nc = self.nc
        eng_state = {e: [None, 0] for e in ENGS}
        sem_names = []

        def new_sem(tag):
            sem_names.append(tag)
            return len(sem_names) - 1

        dsem = {}
        for op in self.ops:
            if op.kind == "c":
                if op.has_dep:
                    st = eng_state[op.eng]
                    if st[0] is None or st[1] >= SEM_LIMIT:
                        st[0] = new_sem("e_" + op.eng)
                        st[1] = 0
                    st[1] += 1
                    op.sig = (st[0], st[1], 1)
            else:
                inc = 16 if op.kind == "d" else 1
                k = op.dbuf.name
                st = dsem.get(k)
                if st is None or st[1] >= SEM_LIMIT * 2:
                    st = [new_sem("d_" + op.dbuf.name), 0]
                    dsem[k] = st
                st[1] += inc
                op.sig = (st[0], st[1], inc)
        final = []
        for b in final_wait_bufs:
            st = dsem[b.name]
            final.append((st[0], st[1]))
        self.n_sems = len(sem_names)
        with contextlib.ExitStack() as es:
            handles = [es.enter_context(nc.semaphore(f"s{i}_{n}"[:40])) for i, n in enumerate(sem_names)]
            block = es.enter_context(nc.Block())

            def run(engname, eng):
                known = {}
                if engname == "sync":
                    self.pid = eng.partition_id()
                for op in self.by_eng[engname]:
                    need = {}
                    for d in op.deps:
                        s, v, _ = d.sig
                        if known.get(s, 0) >= v:
                            continue
                        if need.get(s, 0) < v:
                            need[s] = v
                    for s, v in need.items():
                        eng.wait_ge(handles[s], v)
                        known[s] = v
                    ins = op.fn(eng)
                    if op.sig is not None:
                        ins.then_inc(handles[op.sig[0]], op.sig[2])
                if engname == "sync":
                    for s, v in final:
                        eng.wait_ge(handles[s], v)

            @block.tensor
            def _(e):
                run("tensor", e)

            @block.vector
            def _(e):
                run("vector", e)

            @block.scalar
            def _(e):
                run("scalar", e)

            @block.gpsimd
            def _(e):
                run("gpsimd", e)

            @block.sync
            def _(e):
                run("sync", e)


class Tl:
    def __init__(self, ap, name):
        self.ap = ap
        self.b = Buf(name)

    def __getitem__(self, k):
        return self.ap[k]


DT_SIZE = {F32: 4, BF16: 2, I32: 4}


class Builder:
    def __init__(self, n_layers=NL, stage=99):
        self.n_layers = n_layers
        self.stage = stage
        nc = bass.Bass("TRN2", target_bir_lowering=False)
        self.nc = nc
        self.P = Prog(nc)
        self.es = contextlib.ExitStack()

    def dram_in(self, name, shape, dt):
        return Tl(self.nc.dram_tensor(name, list(shape), dt, kind="ExternalInput").ap(), name)

    def dram_out(self, name, shape, dt):
        return Tl(self.nc.dram_tensor(name, list(shape), dt, kind="ExternalOutput").ap(), name)

    def dram_tmp(self, name, shape, dt):
        return Tl(self.nc.dram_tensor(name, list(shape), dt, kind="Internal").ap(), name)

    def sb(self, name, shape, dt):
        n = 1
        for s_ in shape[1:]:
            n *= s_
        nbytes = n * DT_SIZE[dt]
        nbytes = (nbytes + 63) // 64 * 64
        off = self.aoff
        self.aoff += nbytes
        assert self.aoff <= self.asize, (name, self.aoff, self.asize)
        self.apeak = max(self.apeak, self.aoff)
        w = self.arena[0:shape[0], off // 4:(off + nbytes) // 4]
        if dt != F32:
            w = w.bitcast(dt)
        w = w[:, 0:n]
        if len(shape) == 3:
            w = w.rearrange("p (a b) -> p a b", a=shape[1])
        elif len(shape) == 4:
            w = w.rearrange("p (a b c) -> p a b c", a=shape[1], b=shape[2])
        return Tl(w, name)

    def mark(self):
        return self.aoff

    def regions(self, tl, names):
        return [Tl(tl.ap, n) for n in names]

    def release(self, mark):
        self.P.barrier()
        self.aoff = mark

    def MM(self, ps, out_ap, lhsT, rhs, start, stop, reads):
        self.P.c("tensor", lambda e: e.matmul(out_ap, lhsT=lhsT, rhs=rhs, start=start, stop=stop),
                 [t.b for t in reads], [ps.b])

    def TR(self, ps, out_ap, in_ap, reads):
        ident = self.ident
        self.P.c("tensor", lambda e: e.transpose(out_ap, in_ap, ident[:]),
                 [t.b for t in reads] + [ident.b], [ps.b])

    def ACT(self, out_ap, in_ap, func, reads, writes, bias=None, scale=1.0):
        if bias is None:
            fn = lambda e: e.activation(out=out_ap, in_=in_ap, func=func, scale=scale)
        else:
            fn = lambda e: e.activation(out=out_ap, in_=in_ap, func=func, bias=bias, scale=scale)
        self.P.c("scalar", fn, [t.b for t in reads], [t.b for t in writes])

    def TT(self, eng, out_ap, in0, in1, op, reads, writes):
        self.P.c(eng, lambda e: e.tensor_tensor(out=out_ap, in0=in0, in1=in1, op=op),
                 [t.b for t in reads], [t.b for t in writes])

    def TS(self, eng, out_ap, in0, s1, op0, reads, writes, s2=None, op1=None):
        if op1 is None:
            fn = lambda e: e.tensor_scalar(out=out_ap, in0=in0, scalar1=s1, scalar2=None, op0=op0)
        else:
            fn = lambda e: e.tensor_scalar(out=out_ap, in0=in0, scalar1=s1, scalar2=s2, op0=op0, op1=op1)
        self.P.c(eng, fn, [t.b for t in reads], [t.b for t in writes])

    def STT(self, eng, out_ap, in0, scalar, in1, op0, op1, reads, writes):
        self.P.c(eng, lambda e: e.scalar_tensor_tensor(out=out_ap, in0=in0, scalar=scalar, in1=in1, op0=op0, op1=op1),
                 [t.b for t in reads], [t.b for t in writes])

    def CP(self, eng, out_ap, in_ap, reads, writes):
        if eng == "scalar":
            self.ACT(out_ap, in_ap, AF.Copy, reads, writes)
        else:
            self.P.c(eng, lambda e: e.tensor_copy(out=out_ap, in_=in_ap), [t.b for t in reads], [t.b for t in writes])

    def RED(self, eng, out_ap, in_ap, reads, writes):
        self.P.c(eng, lambda e: e.tensor_reduce(out=out_ap, in_=in_ap, axis=AX.X, op=ALU.add),
                 [t.b for t in reads], [t.b for t in writes])

    def RCP(self, out_ap, in_ap, reads, writes):
        self.P.c("vector", lambda e: e.reciprocal(out=out_ap, in_=in_ap), [t.b for t in reads], [t.b for t in writes])

    def MEMSET(self, eng, ap, val, writes):
        self.P.c(eng, lambda e: e.memset(ap, val), [], [t.b for t in writes])

    def DMA(self, out_ap, in_ap, reads, writes, eng="sync"):
        self.P.dma(eng, lambda e: e.dma_start(out=out_ap, in_=in_ap), [t.b for t in reads], [t.b for t in writes])

    def rstd_from(self, out_tl, out_ap, ps, ps_ap):
        self.ACT(out_ap, ps_ap, AF.Sqrt, [ps, self.cst], [out_tl], bias=self.cst[:, 0:1])
        self.RCP(out_ap, out_ap, [out_tl], [out_tl])

    def wunit(self, wt, src_ap, a, b_):
        st = self.stg[self.stg_i % len(self.stg)]
        self.stg_i += 1
        sl = self.wsl[self.wsl_i % len(self.wsl)]
        self.wsl_i += 1
        sv = st[:, 0:a * b_].rearrange("p (a b) -> p a b", a=a)
        wv = sl[:, 0:a * b_].rearrange("p (a b) -> p a b", a=a)
        self.DMA(st[:, 0:a * b_], src_ap[:, 0:a * b_], [wt], [st])
        self.cast_i += 1
        self.CP("gpsimd" if self.cast_i % 3 else "scalar", wv, sv, [st], [sl])
        return sl, wv

    def stream(self, units, consume, depth=3):
        n = len(units)
        got = []
        for k in range(min(depth, n)):
            got.append(self.wunit(*units[k]))
        for k in range(n):
            if k + depth < n:
                got.append(self.wunit(*units[k + depth]))
            consume(k, got[k][0], got[k][1])

    def build(self):
        nc = self.nc
        P = self.P
        L = self.n_layers
        es = self.es
        with es:
            self._build(nc, P, L, es)
        return nc

    def _build(self, nc, P, L, es):
        xT = self.dram_in("xT", [16, 128, TOK], F32)
        pos_own = self.dram_in("pos_own", [128, 16], I32)
        pos_all = self.dram_in("pos_all", [128, 64], I32)
        w_uq = self.dram_in("w_uq_my", [NL, 512, 384], F32)
        w_ukv = self.dram_in("w_ukv_my", [NL, 256, 512], F32)
        wspec = {"w_in": 40, "w_o": 16, "w_up": 64, "w_down": 64}
        wfl = {}
        for nm, nu in wspec.items():
            t_ = self.nc.dram_tensor(nm + "_t", [NL, nu * 128, 2048], F32, kind="ExternalInput").ap()
            wfl[nm] = [Tl(t_[i], f"{nm}_t{i}") for i in range(NL)]

        def bounce_weights(l_):
            pass

        def gather_weights(l_, names):
            pass
        gains = self.dram_in("gains", [128, 256], F32)
        cfs = self.dram_in("cfs", [128, 2048], F32)
        outT = self.dram_out("outT", [16, 128, TOK], F32)
        xres = self.dram_tmp("xres", [16, 128, TOK], F32)
        sc_q = self.dram_tmp("sc_q", [16, 128, 1024], BF16)
        sc_k = self.dram_tmp("sc_k", [16, 128, 1024], BF16)
        sc_v = self.dram_tmp("sc_v", [16, 128, 1024], BF16)
        sc_g = self.dram_tmp("sc_g", [16, 128, 1024], BF16)
        b_lat = [self.dram_tmp(f"b_lat{i}", [7 * 128, 512], BF16) for i in range(4)]
        g_lat = [self.dram_tmp(f"g_lat{i}", [4 * 7 * 128, 512], BF16) for i in range(4)]
        b_st = self.dram_tmp("b_st", [128, 1024], F32)
        g_st = self.dram_tmp("g_st", [512, 1024], F32)
        b_att = [self.dram_tmp(f"b_att{i}", [256, 1024], BF16) for i in range(8)]
        g_att_all = self.nc.dram_tensor("g_att", [8, 4 * 256, 1024], BF16, kind="Internal").ap()
        g_att = [Tl(g_att_all[i], f"g_att{i}") for i in range(8)]

        self.asize = 207 * 1024
        self.arena = es.enter_context(nc.sbuf_tensor("arena", [128, self.asize // 4], F32))
        self.aoff = 0
        self.apeak = 0
        banks = [Tl(es.enter_context(nc.psum_tensor(f"bank{i}", [128, 512], F32)), f"bank{i}") for i in range(8)]
        self.banks = banks

        gn = self.sb("gains", [128, 256], F32)
        cst = self.sb("cst", [128, 16], F32)
        self.cst = cst
        ident = self.sb("ident", [128, 128], BF16)
        self.ident = ident
        ones = self.sb("ones", [128, 4, 128], BF16)
        one1 = self.sb("one1", [128, 128], BF16)
        mask = self.sb("mask", [128, 128], BF16)
        wq = self.sb("wq", [128, 8], F32)
        wk = self.sb("wk", [128, 8], F32)
        gtab = self.sb("gtab", [128, 8, 128], F32)
        coef = self.sb("coef", [128, 4, 8], F32)
        cosR = self.sb("cosR", [128, 16, 64], F32)
        sinR = self.sb("sinR", [128, 16, 64], F32)
        cosK = self.sb("cosK", [128, 16, 32], F32)
        sinK = self.sb("sinK", [128, 16, 32], F32)
        cosQ = self.sb("cosQ", [128, 64, 32], BF16)
        sinQ = self.sb("sinQ", [128, 64, 32], BF16)
        Sst = self.sb("Sst", [128, 8, 128], F32)
        Sbf = self.sb("Sbf", [128, 8, 128], BF16)
        self.stg = [self.sb(f"stg{i}", [128, 2048], F32) for i in range(2)]
        self.wsl = [self.sb(f"wsl{i}", [128, 2048], BF16) for i in range(6)]
        self.stg_i = 0
        self.wsl_i = 0
        self.cast_i = 0
        base_mark = self.mark()

        self.DMA(gn[:], gains[:], [gains], [gn])
        G_ATT, G_MLP, G_QN, G_KVN, G_BA, G_BR, G_FIN = 0, 64, 128, 144, 152, 184, 216

        ctmp = self.sb("ctmp", [128, 2048], F32)
        self.DMA(ctmp[:], cfs[:], [cfs], [ctmp])
        self.CP("vector", ident[:], ctmp[:, 0:128], [ctmp], [ident])
        self.CP("vector", mask[:], ctmp[:, 128:256], [ctmp], [mask])
        self.CP("vector", wq[:], ctmp[:, 256:264], [ctmp], [wq])
        self.CP("vector", wk[:], ctmp[:, 264:272], [ctmp], [wk])
        self.CP("vector", gtab[:], ctmp[:, 272:1296].rearrange("p (a b) -> p a b", a=8), [ctmp], [gtab])
        self.CP("vector", coef[:], ctmp[:, 1296:1328].rearrange("p (a b) -> p a b", a=4), [ctmp], [coef])
        self.MEMSET("gpsimd", cst[:, 0:1], EPS, [cst])
        self.MEMSET("gpsimd", cst[:, 1:2], 0.0, [cst])
        for i, v in enumerate([1.0 / 2048, 1.0 / 512, 1.0 / 256, 1.0 / 1024]):
            self.MEMSET("gpsimd", ones[:, i, :], v, [ones])
        self.MEMSET("gpsimd", one1[:], 1.0, [one1])
        self.MEMSET("gpsimd", Sst[:], 0.0, [Sst])

        def rope_table(pos_dram, nt, invf_ap, nf, cos_t, sin_t):
            m = self.mark()
            pi_ = self.sb("pos_i", [128, nt], I32)
            pf = self.sb("pos_f", [128, nt], F32)
            ang = self.sb("ang", [128, nt, nf], F32)
            u = self.sb("u", [128, nt, nf], F32)
            r = self.sb("r", [128, nt, nf], F32)
            self.DMA(pi_[:], pos_dram[:], [pos_dram], [pi_])
            self.CP("vector", pf[:], pi_[:], [pi_], [pf])
            self.TT("vector", ang[:], invf_ap.unsqueeze(1).to_broadcast([128, nt, nf]),
                    pf[:].unsqueeze(2).to_broadcast([128, nt, nf]), ALU.mult, [ctmp, pf], [ang])
            for which, dst in ((0, sin_t), (1, cos_t)):
                if which == 1:
                    self.TS("vector", ang[:], ang[:], float(np.pi / 2), ALU.add, [ang], [ang])
                self.TS("vector", u[:], ang[:], float(1.0 / TWO_PI), ALU.mult, [ang], [u])
                self.TS("vector", u[:], u[:], MAGIC, ALU.add, [u], [u])
                self.TS("vector", u[:], u[:], MAGIC, ALU.subtract, [u], [u])
                self.STT("vector", r[:], u[:], -CW1, ang[:], ALU.mult, ALU.add, [u, ang], [r])
                self.STT("vector", r[:], u[:], -CW2, r[:], ALU.mult, ALU.add, [u, r], [r])
                self.TS("vector", r[:], r[:], -3.1415925, ALU.max, [r], [r], s2=3.1415925, op1=ALU.min)
                self.ACT(dst[:], r[:], AF.Sin, [r], [dst])
            self.release(m)

        rope_table(pos_own, 16, ctmp[:, 1360:1424], 64, cosR, sinR)
        rope_table(pos_own, 16, ctmp[:, 1328:1360], 32, cosK, sinK)
        rope_table(pos_all, 64, ctmp[:, 1328:1360], 32, cosQ, sinQ)

        bounce_weights(0)
        gather_weights(0, ["w_in", "w_o", "w_up", "w_down"])
        for kc in range(16):
            self.DMA(xres[kc], xT[kc], [xT], [xres])
        self.release(base_mark)
        if self.stage <= 0:
            for kc in range(16):
                self.DMA(outT[kc], xres[kc], [xres], [outT])
            P.emit(final_wait_bufs=[outT.b])
            return

        bank_i = [0]

        def nb():
            bk = banks[bank_i[0] % 8]
            bank_i[0] += 1
            return bk

        def rope_tm(src, nh, hd, cos_ap, sin_ap, out_tl, out_view, scale_ap, tmp):
            src_tl, src_ap = src
            h2 = hd // 2
            xs, t1, t2 = tmp
            xsv = xs[:, 0:nh * hd].rearrange("p (a b) -> p a b", a=nh)
            t1v = t1[:, 0:nh * h2].rearrange("p (a b) -> p a b", a=nh)
            t2v = t2[:, 0:nh * h2].rearrange("p (a b) -> p a b", a=nh)
            if scale_ap is not None:
                self.TT("vector", xsv, src_ap, scale_ap.unsqueeze(2).to_broadcast([128, nh, hd]), ALU.mult,
                        [src_tl, wq, wk], [xs])
            else:
                self.CP("scalar", xsv, src_ap, [src_tl], [xs])
            cb = cos_ap.unsqueeze(1).to_broadcast([128, nh, h2])
            sbb = sin_ap.unsqueeze(1).to_broadcast([128, nh, h2])
            tabs = [cosR, sinR, cosK, sinK, cosQ, sinQ]
            x1 = xsv[:, :, 0:h2]
            x2 = xsv[:, :, h2:hd]
            self.TT("vector", t1v, x1, cb, ALU.mult, [xs] + tabs, [t1])
            self.TT("gpsimd", t2v, x2, sbb, ALU.mult, [xs] + tabs, [t2])
            self.TT("vector", out_view[:, :, 0:h2], t1v, t2v, ALU.subtract, [t1, t2], [out_tl])
            self.TT("vector", t1v, x2, cb, ALU.mult, [xs] + tabs, [t1])
            self.TT("gpsimd", t2v, x1, sbb, ALU.mult, [xs] + tabs, [t2])
            self.TT("vector", out_view[:, :, h2:hd], t1v, t2v, ALU.add, [t1, t2], [out_tl])

        for l in range(L):
            lmark = self.mark()
            w_in, w_o, w_up, w_down = wfl["w_in"][l], wfl["w_o"][l], wfl["w_up"][l], wfl["w_down"][l]
            if l + 1 < L:
                bounce_weights(l + 1)
            def wu(wt_, u_):
                return wt_.ap[u_ * 128:(u_ + 1) * 128, :]
            self.MEMSET("gpsimd", Sst[:], 0.0, [Sst])
            for h in range(2):
                m = self.mark()
                tsl = slice(h * HALF, (h + 1) * HALF)
                xb = self.sb("xb", [128, 16, HALF], BF16)
                rstd = self.sb("rstd", [128, HALF], F32)
                xring = [self.sb(f"xr{i}", [128, HALF], F32) for i in range(3)]
                sqr = [self.sb(f"sq{i}", [128, HALF], BF16) for i in range(2)]
                bA, bB = banks[0], banks[1]
                for kc in range(16):
                    xr = xring[kc % 3]
                    sq = sqr[kc % 2]
                    self.DMA(xr[:], xres[kc][:, tsl], [xres], [xr])
                    self.ACT(sq[:], xr[:], AF.Square, [xr], [sq])
                    for tt, bk in ((0, bA), (1, bB)):
                        self.MM(bk, bk[:], ones[:, 0, :], sq[:, tt * 512:(tt + 1) * 512], kc == 0, kc == 15, [ones, sq])
                for tt, bk in ((0, bA), (1, bB)):
                    self.rstd_from(rstd, rstd[:, tt * 512:(tt + 1) * 512], bk, bk[:])
                for kc in range(16):
                    xr = xring[(kc + 1) % 3]
                    self.DMA(xr[:], xres[kc][:, tsl], [xres], [xr])
                    self.STT("vector", xb[:, kc, :], xr[:],
                             gn[:, G_ATT + l * 16 + kc:G_ATT + l * 16 + kc + 1], rstd[:], ALU.mult, ALU.mult,
                             [xr, gn, rstd], [xb])
                lat = self.sb("lat", [128, 4, HALF], F32)
                lsq = self.sb("lsq", [128, HALF], BF16)
                lrs = self.sb("lrs", [128, HALF], F32)
                bnc = self.sb("bnc", [128, 7, HALF], BF16)
                for (c0, nch, onei, gcol, boff) in ((0, 4, 1, G_QN + l * 4, 0), (512, 2, 2, G_KVN + l * 2, 4)):
                    units = [(w_in, wu(w_in, boff + j), 16, 128) for j in range(nch)]

                    def cons(k, sl, wv, nch=nch):
                        for tt in range(2):
                            bk = nb()
                            for kc in range(16):
                                self.MM(bk, bk[:], wv[:, kc, :], xb[:, kc, tt * 512:(tt + 1) * 512], kc == 0, kc == 15, [sl, xb])
                            self.CP("scalar", lat[:, k, tt * 512:(tt + 1) * 512], bk[:], [bk], [lat])
                    self.stream(units, cons)
                    for tt in range(2):
                        bk = nb()
                        for j in range(nch):
                            self.ACT(lsq[:, tt * 512:(tt + 1) * 512], lat[:, j, tt * 512:(tt + 1) * 512], AF.Square, [lat], [lsq])
                            self.MM(bk, bk[:], ones[:, onei, :], lsq[:, tt * 512:(tt + 1) * 512], j == 0, j == nch - 1, [ones, lsq])
                        self.rstd_from(lrs, lrs[:, tt * 512:(tt + 1) * 512], bk, bk[:])
                    for j in range(nch):
                        self.STT("vector", bnc[:, boff + j, :], lat[:, j, :], gn[:, gcol + j:gcol + j + 1], lrs[:],
                                 ALU.mult, ALU.mult, [lat, gn, lrs], [bnc])
                rtmp = [(self.sb(f"xs_t{i}", [128, 512], F32), self.sb(f"t1_t{i}", [128, 256], F32), self.sb(f"t2_t{i}", [128, 256], F32))
                        for i in range(2)]
                rti = [0]

                def rt():
                    rti[0] += 1
                    return rtmp[rti[0] % 2]
                ktm = self.sb("ktm", [128, 8, 1024], BF16)
                orow = [self.sb(f"orow{i}", [128, 512], BF16) for i in range(2)]
                krt = self.sb("krt", [128, 128], BF16)
                oi = [0]
                gotk = self.wunit(w_in, wu(w_in, 6), 16, 64)
                for t in range(8):
                    bk = nb()
                    for kc in range(16):
                        self.MM(bk, bk[:, 0:64], xb[:, kc, t * 128:(t + 1) * 128], gotk[1][:, kc, :], kc == 0, kc == 15,
                                [xb, gotk[0]])
                    gt = h * 8 + t
                    rope_tm((bk, bk[:, 0:64].rearrange("p (a b) -> p a b", a=1)), 1, 64, cosK[:, gt, :], sinK[:, gt, :],
                            krt, krt[:, 0:64].rearrange("p (a b) -> p a b", a=1), None, rt())
                    self.CP("gpsimd", krt[:, 64:128], krt[:, 0:64], [krt], [krt])
                    bk2 = nb()
                    bv = bk2[:, 0:64].bitcast(BF16)
                    self.TR(bk2, bv, krt[:], [krt])
                    self.CP("scalar", bnc[:, 6, t * 128:(t + 1) * 128], bv, [bk2], [bnc])
                for tt in range(2):
                    gtt = h * 2 + tt
                    for c_ in range(7):
                        self.DMA(b_lat[gtt][c_ * 128:(c_ + 1) * 128, :],
                                 bnc[:, c_, tt * 512:(tt + 1) * 512], [bnc], [b_lat[gtt]])
                for gi in range(8):
                    kind = gi // 2
                    hg = gi % 2
                    units = [(w_in, wu(w_in, 7 + gi * 4 + j), 4, 512) for j in range(4)]
                    got = [self.wunit(*u) for u in units]
                    for t in range(8):
                        gt = h * 8 + t
                        bk = nb()
                        for kc in range(16):
                            self.MM(bk, bk[:], xb[:, kc, t * 128:(t + 1) * 128], got[kc // 4][1][:, kc % 4, :], kc == 0, kc == 15,
                                    [xb, got[kc // 4][0]])
                        bk3 = bk[:].rearrange("p (a b) -> p a b", a=4)
                        if kind == 0:
                            o = orow[oi[0] % 2]
                            oi[0] += 1
                            rope_tm((bk, bk3), 4, 128, cosR[:, gt, :], sinR[:, gt, :], o,
                                    o[:].rearrange("p (a b) -> p a b", a=4), wq[:, hg * 4:hg * 4 + 4], rt())
                            self.DMA(sc_q[gt][:, hg * 512:(hg + 1) * 512], o[:], [o], [sc_q])
                        elif kind == 1:
                            rope_tm((bk, bk3), 4, 128, cosR[:, gt, :], sinR[:, gt, :], ktm,
                                    ktm[:, t, hg * 512:(hg + 1) * 512].rearrange("p (a b) -> p a b", a=4),
                                    wk[:, hg * 4:hg * 4 + 4], rt())
                            self.DMA(sc_k[gt][:, hg * 512:(hg + 1) * 512], ktm[:, t, hg * 512:(hg + 1) * 512], [ktm], [sc_k])
                        elif kind == 2:
                            o = orow[oi[0] % 2]
                            oi[0] += 1
                            self.CP("scalar", o[:], bk[:], [bk], [o])
                            self.DMA(sc_v[gt][:, hg * 512:(hg + 1) * 512], o[:], [o], [sc_v])
                            bm = nb()
                            for hh in range(4):
                                self.MM(bm, bm[:, hh * 128:(hh + 1) * 128], ktm[:, t, (hg * 4 + hh) * 128:(hg * 4 + hh + 1) * 128],
                                        o[:, hh * 128:(hh + 1) * 128], True, True, [ktm, o])
                            sv = Sst[:, hg * 4:hg * 4 + 4, :]
                            self.TT("vector", sv, sv, bm[:].rearrange("p (a b) -> p a b", a=4), ALU.add, [Sst, bm], [Sst])
                            self.TT("vector", sv, sv, gtab[:, hg * 4:hg * 4 + 4, :], ALU.mult, [Sst, gtab], [Sst])
                        else:
                            o = orow[oi[0] % 2]
                            oi[0] += 1
                            self.ACT(o[:], bk[:], AF.Silu, [bk], [o])
                            self.DMA(sc_g[gt][:, hg * 512:(hg + 1) * 512], o[:], [o], [sc_g])
                self.release(m)
            self.DMA(b_st[:], Sst[:].rearrange("p a b -> p (a b)"), [Sst], [b_st])
            for i_ in range(4):
                P.cc(lambda e, i_=i_: e.collective_compute("AllGather", ALU.bypass, replica_groups=GROUPS, ins=[b_lat[i_].ap], outs=[g_lat[i_].ap]),
                     [b_lat[i_].b], [g_lat[i_].b])
            P.cc(lambda e: e.collective_compute("AllGather", ALU.bypass, replica_groups=GROUPS, ins=[b_st.ap], outs=[g_st.ap]),
                 [b_st.b], [g_st.b])
            if l + 1 < L:
                gather_weights(l + 1, ["w_in", "w_o"])
            if self.stage == 1:
                for kc in range(16):
                    self.DMA(outT[kc], xres[kc], [xres], [outT])
                P.emit(final_wait_bufs=[outT.b, g_st.b] + [g_lat[i_].b for i_ in range(4)])
                return
            m = self.mark()
            wuq = self.sb("wuq", [128, 4, 384], BF16)
            wukv = self.sb("wukv", [128, 2, 512], BF16)
            wtmp = self.sb("wtmp", [128, 4, 384], F32)
            for kc in range(4):
                self.DMA(wtmp[:, kc, :], w_uq[l][kc * 128:(kc + 1) * 128, :], [w_uq], [wtmp])
            self.CP("gpsimd", wuq[:], wtmp[:], [wtmp], [wuq])
            wtmp2 = self.sb("wtmp2", [128, 2, 512], F32)
            for kc in range(2):
                self.DMA(wtmp2[:, kc, :], w_ukv[l][kc * 128:(kc + 1) * 128, :], [w_ukv], [wtmp2])
            self.CP("gpsimd", wukv[:], wtmp2[:], [wtmp2], [wukv])
            KT = self.sb("KT", [128, 2, S], BF16)
            KR = self.sb("KR", [128, S], BF16)
            VV = self.sb("VV", [128, 64, 256], BF16)
            latr = [self.sb(f"latr{i}", [128, 6, 512], BF16) for i in range(2)]
            QN = [self.sb(f"QN{i}", [128, 2, 512], BF16) for i in range(2)]
            QR = [self.sb(f"QR{i}", [128, 512], BF16) for i in range(2)]
            qrt = [self.sb(f"qrt{i}", [128, 128], BF16) for i in range(2)]
            PT = [self.sb(f"PT{i}", [128, 512], BF16) for i in range(3)]
            rden = self.sb("rden", [128, 512], F32)
            ao = [self.sb(f"ao{i}", [128, 512], BF16) for i in range(2)]
            xs_t = self.sb("xs_t", [128, 512], F32)
            t1_t = self.sb("t1_t", [128, 256], F32)
            t2_t = self.sb("t2_t", [128, 256], F32)
            pti = [0]
            aoi = [0]
            KTr = [Tl(KT.ap, f"KT{q}") for q in range(16)]
            KRr = [Tl(KR.ap, f"KR{q}") for q in range(16)]
            VVr = [Tl(VV.ap, f"VV{q}") for q in range(16)]
            g_lat_vs = [g_lat[i_].ap.rearrange("(r c p) t -> r p c t", c=7, p=128) for i_ in range(4)]
            for qt in range(16):
                lt = latr[qt % 2]
                qn = QN[qt % 2]
                qr = QR[qt % 2]
                glv = g_lat_vs[qt % 4][qt // 4]
                for c_ in range(6):
                    self.DMA(lt[:, c_, :], glv[:, c_, :], [g_lat[qt % 4]], [lt])
                self.DMA(KR[:, qt * 512:(qt + 1) * 512], glv[:, 6, :], [g_lat[qt % 4]], [KRr[qt]])
                for hh in range(2):
                    bk = nb()
                    for kc in range(2):
                        self.MM(bk, bk[:], wukv[:, kc, hh * 128:(hh + 1) * 128], lt[:, 4 + kc, :], kc == 0, kc == 1, [wukv, lt])
                    self.CP("scalar" if hh == 0 else "vector", KT[:, hh, qt * 512:(qt + 1) * 512], bk[:], [bk], [KTr[qt]])
                for j in range(4):
                    bk = nb()
                    for kc in range(2):
                        self.MM(bk, bk[:, 0:256], lt[:, 4 + kc, j * 128:(j + 1) * 128], wukv[:, kc, 256:512], kc == 0, kc == 1, [wukv, lt])
                    self.CP("vector" if j % 2 == 0 else "scalar", VV[:, qt * 4 + j, :], bk[:, 0:256], [bk], [VVr[qt]])
                for hh in range(2):
                    bk = nb()
                    for kc in range(4):
                        self.MM(bk, bk[:], wuq[:, kc, hh * 128:(hh + 1) * 128], lt[:, kc, :], kc == 0, kc == 3, [wuq, lt])
                    self.CP("scalar" if hh == 0 else "vector", qn[:, hh, :], bk[:], [bk], [qn])
                for j in range(4):
                    bk = nb()
                    for kc in range(4):
                        self.MM(bk, bk[:, 0:128], lt[:, kc, j * 128:(j + 1) * 128], wuq[:, kc, 256:384], kc == 0, kc == 3, [wuq, lt])
                    qq = qrt[j % 2]
                    rope_tm((bk, bk[:, 0:128].rearrange("p (a b) -> p a b", a=2)), 2, 64, cosQ[:, qt * 4 + j, :], sinQ[:, qt * 4 + j, :],
                            qq, qq[:].rearrange("p (a b) -> p a b", a=2), None, (xs_t, t1_t, t2_t))
                    bk2 = nb()
                    bv = bk2[:, 0:64].bitcast(BF16)
                    self.TR(bk2, bv, qq[:], [qq])
                    self.CP("scalar", qr[:, j * 128:(j + 1) * 128], bv, [bk2], [qr])
                nkt = 4 * qt + 4
                for hh in range(2):
                    OUT = banks[6] if hh == 0 else banks[4]
                    DEN = banks[7] if hh == 0 else banks[5]
                    for kt in range(nkt):
                        r_ = kt - 4 * qt
                        c0 = 0 if r_ <= 0 else r_ * 128
                        sbk = banks[kt % 4]
                        self.MM(sbk, sbk[:, c0:512], KT[:, hh, kt * 128:(kt + 1) * 128], qn[:, hh, c0:512], True, False, [KTr[kt // 4], qn])
                        self.MM(sbk, sbk[:, c0:512], KR[hh * 64:(hh + 1) * 64, kt * 128:(kt + 1) * 128], qr[hh * 64:(hh + 1) * 64, c0:512],
                                False, True, [KRr[kt // 4], qr])
                        pt = PT[pti[0] % 3]
                        pti[0] += 1
                        self.ACT(pt[:, c0:512], sbk[:, c0:512], AF.Exp, [sbk], [pt], scale=ATT_SCALE)
                        if r_ >= 0:
                            self.TT("gpsimd", pt[:, c0:c0 + 128], pt[:, c0:c0 + 128], mask[:], ALU.mult, [pt, mask], [pt])
                        self.MM(OUT, OUT[:, c0:512], VV[:, kt, hh * 128:(hh + 1) * 128], pt[:, c0:512], kt == 0, kt == nkt - 1, [VVr[kt // 4], pt])
                        self.MM(DEN, DEN[:, c0:512], one1[:], pt[:, c0:512], kt == 0, kt == nkt - 1, [one1, pt])
                    self.RCP(rden[:], DEN[:], [DEN], [rden])
                    a_ = ao[aoi[0] % 2]
                    aoi[0] += 1
                    self.TT("vector", a_[:], OUT[:], rden[:], ALU.mult, [OUT, rden], [a_])
                    blk = qt // 2
                    self.DMA(b_att[blk][hh * 128:(hh + 1) * 128, (qt % 2) * 512:(qt % 2 + 1) * 512], a_[:], [a_], [b_att[blk]])
                bank_i[0] = 0
            self.release(m)
            for i_ in range(8):
                P.cc(lambda e, i_=i_: e.collective_compute("AllGather", ALU.bypass, replica_groups=GROUPS, ins=[b_att[i_].ap], outs=[g_att[i_].ap]),
                     [b_att[i_].b], [g_att[i_].b])
            if l + 1 < L:
                gather_weights(l + 1, ["w_up", "w_down"])
            if self.stage == 2:
                for kc in range(16):
                    self.DMA(outT[kc], xres[kc], [xres], [outT])
                P.emit(final_wait_bufs=[outT.b] + [g_att[i_].b for i_ in range(8)])
                return
            m = self.mark()
            gs = self.sb("gs", [128, 4, 1024], F32)
            for r_ in range(4):
                self.DMA(gs[:, r_, :], g_st.ap[r_ * 128:(r_ + 1) * 128, :], [g_st], [gs])
            stmp = self.sb("stmp", [128, 8, 128], F32)
            for r_ in range(4):
                dst = Sst if r_ == 0 else stmp
                self.TT("vector", dst[:], gs[:, r_, :].rearrange("p (a b) -> p a b", a=8),
                        coef[:, r_, :].unsqueeze(2).to_broadcast([128, 8, 128]), ALU.mult, [gs, coef], [dst])
                if r_ > 0:
                    self.TT("vector", Sst[:], Sst[:], stmp[:], ALU.add, [Sst, stmp], [Sst])
            self.CP("vector", Sbf[:], Sst[:], [Sst], [Sbf])
            self.release(m)
            for h in range(2):
                m = self.mark()
                tsl = slice(h * HALF, (h + 1) * HALF)
                xacc = self.sb("xacc", [128, 16, HALF], F32)
                mixed = self.sb("mixed", [128, 16, HALF], BF16)
                xar = [self.regions(xacc, [f"xacc_{kc}_0", f"xacc_{kc}_1"]) for kc in range(16)]
                for kc in range(16):
                    self.DMA(xacc[:, kc, :], xres[kc][:, tsl], [xres], xar[kc])
                m2 = self.mark()
                rin = [[self.sb(f"rin{i}_{j}", [128, 1024], BF16) for j in range(4)] for i in range(2)]
                QTt = self.sb("QTt", [128, 8, 128], BF16)
                KTt = self.sb("KTt", [128, 8, 128], BF16)
                PTt = self.sb("PTt", [128, 8, 128], BF16)
                osb = self.sb("osb", [128, 8, 128], F32)
                osq = self.sb("osq", [128, 8, 128], F32)
                rr = self.sb("rr", [128, 8, 128], BF16)
                st8 = self.sb("st8", [128, 4, 8], F32)
                for t in range(8):
                    gt = h * 8 + t
                    q_, k_, v_, g_ = rin[t % 2]
                    self.DMA(q_[:], sc_q[gt], [sc_q], [q_])
                    self.DMA(k_[:], sc_k[gt], [sc_k], [k_])
                    self.DMA(v_[:], sc_v[gt], [sc_v], [v_])
                    self.DMA(g_[:], sc_g[gt], [sc_g], [g_])
                    bq, bkk = banks[0], banks[1]
                    bqv = bq[:].bitcast(BF16)
                    bkv = bkk[:].bitcast(BF16)
                    for hh in range(8):
                        self.TR(bq, bqv[:, hh * 128:(hh + 1) * 128], q_[:, hh * 128:(hh + 1) * 128], [q_])
                    for hh in range(8):
                        self.TR(bkk, bkv[:, hh * 128:(hh + 1) * 128], k_[:, hh * 128:(hh + 1) * 128], [k_])
                    self.CP("scalar", QTt[:].rearrange("p a b -> p (a b)"), bqv, [bq], [QTt])
                    self.CP("vector", KTt[:].rearrange("p a b -> p (a b)"), bkv, [bkk], [KTt])
                    for half8 in range(2):
                        bs = banks[2 + half8]
                        for hh in range(4):
                            hd_ = half8 * 4 + hh
                            self.MM(bs, bs[:, hh * 128:(hh + 1) * 128], KTt[:, hd_, :], QTt[:, hd_, :], True, True, [KTt, QTt])
                        self.TT("vector", PTt[:, half8 * 4:half8 * 4 + 4, :], bs[:].rearrange("p (a b) -> p a b", a=4),
                                mask[:].unsqueeze(1).to_broadcast([128, 4, 128]), ALU.mult, [bs, mask], [PTt])
                    for half8 in range(2):
                        bo = banks[4 + half8]
                        for hh in range(4):
                            hd_ = half8 * 4 + hh
                            self.MM(bo, bo[:, hh * 128:(hh + 1) * 128], PTt[:, hd_, :], v_[:, hd_ * 128:(hd_ + 1) * 128], True, False, [PTt, v_])
                            self.MM(bo, bo[:, hh * 128:(hh + 1) * 128], QTt[:, hd_, :], Sbf[:, hd_, :], False, True, [QTt, Sbf])
                        self.CP("scalar", osb[:, half8 * 4:half8 * 4 + 4, :], bo[:].rearrange("p (a b) -> p a b", a=4), [bo], [osb])
                    for half8 in range(2):
                        bm = banks[6 + half8]
                        for hh in range(4):
                            hd_ = half8 * 4 + hh
                            self.MM(bm, bm[:, hh * 128:(hh + 1) * 128], k_[:, hd_ * 128:(hd_ + 1) * 128], v_[:, hd_ * 128:(hd_ + 1) * 128],
                                    True, True, [k_, v_])
                        sv = Sst[:, half8 * 4:half8 * 4 + 4, :]
                        self.TT("vector", sv, sv, bm[:].rearrange("p (a b) -> p a b", a=4), ALU.add, [Sst, bm], [Sst])
                        self.TT("vector", sv, sv, gtab[:, half8 * 4:half8 * 4 + 4, :], ALU.mult, [Sst, gtab], [Sst])
                    self.CP("gpsimd", Sbf[:], Sst[:], [Sst], [Sbf])
                    self.RED("vector", st8[:, 0, :], osb[:], [osb], [st8])
                    self.ACT(osq[:], osb[:], AF.Square, [osb], [osq])
                    self.RED("vector", st8[:, 1, :], osq[:], [osq], [st8])
                    self.TS("vector", st8[:, 0, :], st8[:, 0, :], 1.0 / 128, ALU.mult, [st8], [st8])
                    self.TT("vector", st8[:, 2, :], st8[:, 0, :], st8[:, 0, :], ALU.mult, [st8], [st8])
                    self.STT("vector", st8[:, 1, :], st8[:, 1, :], 1.0 / 128, st8[:, 2, :], ALU.mult, ALU.subtract, [st8], [st8])
                    self.ACT(st8[:, 3, :], st8[:, 1, :], AF.Sqrt, [st8, cst], [st8], bias=cst[:, 0:1])
                    self.RCP(st8[:, 3, :], st8[:, 3, :], [st8], [st8])
                    self.TT("vector", osb[:], osb[:], st8[:, 0, :].unsqueeze(2).to_broadcast([128, 8, 128]), ALU.subtract, [osb, st8], [osb])
                    self.TT("gpsimd", osb[:], osb[:], st8[:, 3, :].unsqueeze(2).to_broadcast([128, 8, 128]), ALU.mult, [osb, st8], [osb])
                    self.TT("vector", rr[:].rearrange("p a b -> p (a b)"), osb[:].rearrange("p a b -> p (a b)"), g_[:], ALU.mult, [osb, g_], [rr])
                    br_ = banks[0]
                    brv = br_[:].bitcast(BF16)
                    for hh in range(8):
                        self.TR(br_, brv[:, hh * 128:(hh + 1) * 128], rr[:, hh, :], [rr])
                    self.TT("vector", mixed[:, 8:16, t * 128:(t + 1) * 128], brv.rearrange("p (a b) -> p a b", a=8),
                            gn[:, G_BR + l * 8:G_BR + l * 8 + 8].unsqueeze(2).to_broadcast([128, 8, 128]), ALU.mult, [br_, gn], [mixed])
                self.release(m2)
                m2 = self.mark()
                araw = self.sb("araw", [128, 8, HALF], BF16)
                asq = self.sb("asq", [128, 512], BF16)
                ars = self.sb("ars", [128, HALF], F32)
                g_att_v = g_att_all.rearrange("b (r c p) t -> b p (r c) t", r=4, c=2, p=128)
                for j_ in range(8):
                    def dyn(e, j_=j_, h=h):
                        blk = (P.pid % 4) * 2 + h
                        return e.dma_start(out=araw[:, j_, :], in_=g_att_v[bass.ds(blk, 1)].rearrange("o p j t -> p (o j) t")[:, j_, :])
                    P.dma("sync", dyn, [t_.b for t_ in g_att], [araw.b])
                for tt in range(2):
                    bk = nb()
                    for j in range(8):
                        self.ACT(asq[:], araw[:, j, tt * 512:(tt + 1) * 512], AF.Square, [araw], [asq])
                        self.MM(bk, bk[:], ones[:, 3, :], asq[:], j == 0, j == 7, [ones, asq])
                    self.rstd_from(ars, ars[:, tt * 512:(tt + 1) * 512], bk, bk[:])
                for j in range(8):
                    self.STT("vector", mixed[:, j, :], araw[:, j, :], gn[:, G_BA + l * 8 + j:G_BA + l * 8 + j + 1], ars[:],
                             ALU.mult, ALU.mult, [araw, gn, ars], [mixed])
                self.release(m2)
                m2 = self.mark()
                units = [(w_o, wu(w_o, oc), 16, 128) for oc in range(16)]

                def cons_o(k, sl, wv):
                    for tt in range(2):
                        bk = nb()
                        for mc in range(16):
                            self.MM(bk, bk[:], wv[:, mc, :], mixed[:, mc, tt * 512:(tt + 1) * 512], mc == 0, mc == 15, [sl, mixed])
                        xa = xacc[:, k, tt * 512:(tt + 1) * 512]
                        self.TT("vector", xa, xa, bk[:], ALU.add, [xar[k][tt], bk], [xar[k][tt]])
                self.stream(units, cons_o)
                xb = self.sb("xb2", [128, 16, HALF], BF16) if False else None
                self.release(m2)
                m2 = self.mark()
                sq2 = [self.sb(f"sq2_{i}", [128, 512], BF16) for i in range(2)]
                rstd2 = self.sb("rstd2", [128, HALF], F32)
                xb = mixed
                for tt in range(2):
                    bk = nb()
                    for kc in range(16):
                        sq = sq2[kc % 2]
                        self.ACT(sq[:], xacc[:, kc, tt * 512:(tt + 1) * 512], AF.Square, [xar[kc][tt]], [sq])
                        self.MM(bk, bk[:], ones[:, 0, :], sq[:], kc == 0, kc == 15, [ones, sq])
                    self.rstd_from(rstd2, rstd2[:, tt * 512:(tt + 1) * 512], bk, bk[:])
                xbr = self.regions(xb, [f"xbr{kc}" for kc in range(16)])
                for kc in range(16):
                    self.STT("vector", xb[:, kc, :], xacc[:, kc, :],
                             gn[:, G_MLP + l * 16 + kc:G_MLP + l * 16 + kc + 1], rstd2[:], ALU.mult, ALU.mult,
                             [xar[kc][0], xar[kc][1], gn, rstd2], [xbr[kc]])
                aT = [self.sb(f"aT{i}", [128, 4, HALF], BF16) for i in range(2)]
                aTr = [[self.regions(aT[i], [f"aT{i}_{j}_0", f"aT{i}_{j}_1"]) for j in range(4)] for i in range(2)]
                rl = [self.sb(f"rl{i}", [128, 512], BF16) for i in range(2)]
                rli = [0]

                def up_group(grp):
                    a_ = aT[grp % 2]
                    units = [(w_up, wu(w_up, grp * 4 + j), 16, 128) for j in range(4)]

                    def cons_u(k, sl, wv):
                        for tt in range(2):
                            bk = nb()
                            for kc in range(16):
                                self.MM(bk, bk[:], wv[:, kc, :], xb[:, kc, tt * 512:(tt + 1) * 512], kc == 0, kc == 15, [sl, xbr[kc]])
                            r1 = rl[rli[0] % 2]
                            rli[0] += 1
                            self.ACT(r1[:], bk[:], AF.Relu, [bk], [r1])
                            self.ACT(a_[:, k, tt * 512:(tt + 1) * 512], r1[:], AF.Square, [r1], [aTr[grp % 2][k][tt]])
                    self.stream(units, cons_u, depth=2)

                def down_group(grp):
                    a_ = aT[grp % 2]
                    units = [(w_down, wu(w_down, grp * 4 + j), 16, 128) for j in range(4)]
                    got = [self.wunit(*u) for u in units]
                    for oc in range(16):
                        for tt in range(2):
                            bk = nb()
                            for j in range(4):
                                self.MM(bk, bk[:], got[j][1][:, oc, :], a_[:, j, tt * 512:(tt + 1) * 512], j == 0, j == 3,
                                        [got[j][0], aTr[grp % 2][j][tt]])
                            xa = xacc[:, oc, tt * 512:(tt + 1) * 512]
                            self.TT("vector", xa, xa, bk[:], ALU.add, [xar[oc][tt], bk], [xar[oc][tt]])

                up_group(0)
                for grp in range(16):
                    if grp + 1 < 16:
                        up_group(grp + 1)
                    down_group(grp)
                if l < L - 1:
                    for kc in range(16):
                        self.DMA(xres[kc][:, tsl], xacc[:, kc, :], xar[kc], [xres])
                else:
                    for tt in range(2):
                        bk = nb()
                        for kc in range(16):
                            sq = sq2[kc % 2]
                            self.ACT(sq[:], xacc[:, kc, tt * 512:(tt + 1) * 512], AF.Square, [xar[kc][tt]], [sq])
                            self.MM(bk, bk[:], ones[:, 0, :], sq[:], kc == 0, kc == 15, [ones, sq])
                        self.rstd_from(rstd2, rstd2[:, tt * 512:(tt + 1) * 512], bk, bk[:])
                    for kc in range(16):
                        self.STT("vector", xacc[:, kc, :], xacc[:, kc, :],
                                 gn[:, G_FIN + kc:G_FIN + kc + 1], rstd2[:], ALU.mult, ALU.mult, xar[kc] + [gn, rstd2], xar[kc])
                        self.DMA(outT[kc][:, tsl], xacc[:, kc, :], xar[kc], [outT])
                self.release(m2)
                self.release(m)
            self.release(lmark)
        P.emit(final_wait_bufs=[outT.b])


def _consts(g):
    c = np.zeros((128, 2048), np.float32)
    c[:, 0:128] = np.eye(128, dtype=np.float32)
    k = np.arange(128)[:, None]
    q = np.arange(128)[None, :]
    c[:, 128:256] = (q >= k).astype(np.float32)
    hh = np.arange(8, dtype=np.float64)
    gamma = 1.0 - np.exp2(-5.0 - hh)
    t = np.arange(128, dtype=np.float64)[:, None]
    c[:, 256:264] = (gamma[None, :] ** (t + 1.0)) * (128.0 ** -0.5)
    c[:, 264:272] = gamma[None, :] ** (-(t + 1.0))
    c[:, 272:1296] = np.repeat((gamma ** 128.0)[None, :], 128, axis=1).reshape(1, 1024).repeat(128, 0) if False else \
        np.broadcast_to(np.repeat(gamma ** 128.0, 128)[None, :], (128, 1024))
    coef = np.zeros((4, 8), np.float64)
    for i in range(4):
        if i < g:
            coef[i] = gamma ** (2048.0 * (g - 1 - i))
    c[:, 1296:1328] = np.broadcast_to(coef.reshape(1, 32), (128, 32))
    invf64 = (np.float32(10000.0) ** (-np.arange(0, 64, 2, dtype=np.float32) / np.float32(64))).astype(np.float32)
    invf128 = (np.float32(10000.0) ** (-np.arange(0, 128, 2, dtype=np.float32) / np.float32(128))).astype(np.float32)
    c[:, 1328:1360] = invf64[None, :]
    c[:, 1360:1424] = invf128[None, :]
    return c


def _gains(attn_norm, mlp_norm, q_norm, kv_norm, beta_attn, beta_ret, final_norm):
    gcols = np.zeros((128, 256), np.float32)
    for l in range(NL):
        gcols[:, 0 + l * 16:0 + (l + 1) * 16] = attn_norm[l].reshape(16, 128).T
        gcols[:, 64 + l * 16:64 + (l + 1) * 16] = mlp_norm[l].reshape(16, 128).T
        gcols[:, 128 + l * 4:128 + (l + 1) * 4] = q_norm[l].reshape(4, 128).T
        gcols[:, 144 + l * 2:144 + (l + 1) * 2] = kv_norm[l].reshape(2, 128).T
        gcols[:, 152 + l * 8:152 + (l + 1) * 8] = beta_attn[l].reshape(8, 128).T
        gcols[:, 184 + l * 8:184 + (l + 1) * 8] = beta_ret[l].reshape(8, 128).T
    gcols[:, 216:232] = final_norm.reshape(16, 128).T
    return gcols


_NC_CACHE = {}


def kernel(x, positions, attn_norm, w_in, q_norm, kv_norm, w_uq, w_ukv, beta_attn, beta_ret,
           w_o, mlp_norm, w_up, w_down, final_norm, _n_layers=NL, _stage=99):
    x = np.asarray(x, np.float32)
    positions = np.asarray(positions, np.int32)
    w_in = np.ascontiguousarray(np.asarray(w_in, np.float32))
    w_uq = np.asarray(w_uq, np.float32)
    w_ukv = np.asarray(w_ukv, np.float32)
    w_o = np.ascontiguousarray(np.asarray(w_o, np.float32))
    w_up = np.ascontiguousarray(np.asarray(w_up, np.float32))
    w_down = np.ascontiguousarray(np.asarray(w_down, np.float32))
    gains = _gains(*[np.asarray(a, np.float32) for a in (attn_norm, mlp_norm, q_norm, kv_norm, beta_attn, beta_ret, final_norm)])
    if (_n_layers, _stage) not in _NC_CACHE:
        _NC_CACHE[(_n_layers, _stage)] = Builder(_n_layers, _stage).build()
    nc = _NC_CACHE[(_n_layers, _stage)]
    def fm_units(w, ncolblk):
        nl, kdim, _ = w.shape
        kc_n = kdim // 128
        t = w[:, :, :ncolblk * 128].reshape(nl, kc_n, 128, ncolblk, 128).transpose(0, 3, 2, 1, 4)
        return t.reshape(nl, ncolblk, 128, kc_n * 128)
    w_in_t = np.zeros((NL, 40, 128, 2048), np.float32)
    w_in_t[:, 0:6] = fm_units(w_in[:, :, 0:768], 6)
    w_in_t[:, 6, :, 0:1024] = w_in[:, :, 768:832].reshape(NL, 16, 128, 64).transpose(0, 2, 1, 3).reshape(NL, 128, 1024)
    for gi in range(8):
        c0 = 832 + gi * 512
        blk = w_in[:, :, c0:c0 + 512].reshape(NL, 4, 4, 128, 512).transpose(0, 1, 3, 2, 4)
        w_in_t[:, 7 + gi * 4:7 + gi * 4 + 4] = blk.reshape(NL, 4, 128, 2048)
    w_in_t = w_in_t.reshape(NL, 40 * 128, 2048)
    w_o_t = np.ascontiguousarray(fm_units(w_o, 16)).reshape(NL, 16 * 128, 2048)
    w_up_t = np.ascontiguousarray(fm_units(w_up, 64)).reshape(NL, 64 * 128, 2048)
    in_maps = []
    for c in range(8):
        b, g = c // 4, c % 4
        xs = x[b, g * TOK:(g + 1) * TOK, :]
        xT = np.ascontiguousarray(xs.T).reshape(16, 128, TOK)
        po = np.ascontiguousarray(positions[b, g * TOK:(g + 1) * TOK].reshape(16, 128).T)
        pa = np.ascontiguousarray(positions[b].reshape(64, 128).T)
        h0, h1 = 2 * g, 2 * g + 1
        wuq_my = np.ascontiguousarray(np.concatenate(
            [w_uq[:, :, h0 * 192:h0 * 192 + 128], w_uq[:, :, h1 * 192:h1 * 192 + 128],
             w_uq[:, :, h0 * 192 + 128:(h0 + 1) * 192], w_uq[:, :, h1 * 192 + 128:(h1 + 1) * 192]], axis=2))
        wukv_my = np.ascontiguousarray(np.concatenate(
            [w_ukv[:, :, h0 * 256:h0 * 256 + 128], w_ukv[:, :, h1 * 256:h1 * 256 + 128],
             w_ukv[:, :, h0 * 256 + 128:(h0 + 1) * 256], w_ukv[:, :, h1 * 256 + 128:(h1 + 1) * 256]], axis=2))
        in_maps.append({
            "xT": xT, "pos_own": po, "pos_all": pa, "w_uq_my": wuq_my, "w_ukv_my": wukv_my,
            "w_in_t": w_in_t, "w_o_t": w_o_t, "w_up_t": w_up_t, "w_down_t": w_down,
            "gains": gains, "cfs": _consts(g),
        })
    res = run_bass_kernel_spmd(nc, in_maps, core_ids=list(range(8)))
    out = np.empty((2, S, D), np.float32)
    for c in range(8):
        b, g = c // 4, c % 4
        oT = np.asarray(res.results[c]["outT"]).reshape(D, TOK)
        out[b, g * TOK:(g + 1) * TOK, :] = oT.T
    return out
```

```python
import contextlib
import numpy as np
import concourse.bass as bass
import concourse.mybir as mybir
from concourse.bass_utils import run_bass_kernel_spmd

F32 = mybir.dt.float32
BF16 = mybir.dt.bfloat16
I32 = mybir.dt.int32
AF = mybir.ActivationFunctionType
ALU = mybir.AluOpType
AX = mybir.AxisListType

D = 2048
S = 8192
NL = 4
TOK = 2048
HALF = 1024
INW = 4928
DFF = 8192
GROUPS = [[0, 1, 2, 3], [4, 5, 6, 7]]
ATT_SCALE = 192.0 ** -0.5
EPS = 1e-6
MAGIC = 12582912.0
TWO_PI = 2.0 * np.pi
CW1 = 6.28125
CW2 = TWO_PI - 6.28125

ENGS = ("tensor", "vector", "scalar", "gpsimd", "sync")
SEM_LIMIT = 8000


class Buf:
    __slots__ = ("name", "last_w", "reads")

    def __init__(self, name):
        self.name = name
        self.last_w = None
        self.reads = []


class Op:
    __slots__ = ("eng", "fn", "deps", "kind", "sig", "has_dep", "dbuf")

    def __init__(self, eng, fn, kind):
        self.eng = eng
        self.fn = fn
        self.kind = kind
        self.deps = set()
        self.sig = None
        self.has_dep = False
        self.dbuf = None


class Prog:
    def __init__(self, nc):
        self.nc = nc
        self.ops = []
        self.by_eng = {e: [] for e in ENGS}
        self.last_of = {e: None for e in ENGS}
        self.pending = {e: set() for e in ENGS}
        self.dma_since_barrier = []

    def _add(self, eng, fn, reads, writes, kind):
        op = Op(eng, fn, kind)
        for b in reads:
            if b.last_w is not None:
                op.deps.add(b.last_w)
        for b in writes:
            if b.last_w is not None:
                op.deps.add(b.last_w)
            for r in b.reads:
                op.deps.add(r)
        for b in reads:
            b.reads.append(op)
        for b in writes:
            b.last_w = op
            b.reads = []
        op.deps.discard(op)
        if self.pending[eng]:
            op.deps |= self.pending[eng]
            self.pending[eng] = set()
        if eng == "tensor":
            op.deps = {d for d in op.deps if not (d.eng == "tensor" and d.kind == "c")}
        for d in op.deps:
            d.has_dep = True
        self.ops.append(op)
        self.by_eng[eng].append(op)
        if kind == "c":
            self.last_of[eng] = op
        else:
            self.dma_since_barrier.append(op)
        return op

    def c(self, eng, fn, reads=(), writes=()):
        return self._add(eng, fn, list(reads), list(writes), "c")

    def dma(self, eng, fn, reads=(), writes=()):
        op = self._add(eng, fn, list(reads), list(writes), "d")
        op.dbuf = writes[0]
        op.has_dep = True
        return op

    def cc(self, fn, reads=(), writes=()):
        op = self._add("gpsimd", fn, list(reads), list(writes), "cc")
        op.dbuf = writes[0]
        op.has_dep = True
        return op

    def barrier(self):
        deps = set(o for o in self.last_of.values() if o is not None) | set(self.dma_since_barrier)
        self.dma_since_barrier = []
        for e in ENGS:
            self.pending[e] |= deps

    def emit(self, final_wait_bufs=()):
        nc = self.nc
        eng_state = {e: [None, 0] for e in ENGS}
        sem_names = []

        def new_sem(tag):
            sem_names.append(tag)
            return len(sem_names) - 1

        dsem = {}
        for op in self.ops:
            if op.kind == "c":
                if op.has_dep:
                    st = eng_state[op.eng]
                    if st[0] is None or st[1] >= SEM_LIMIT:
                        st[0] = new_sem("e_" + op.eng)
                        st[1] = 0
                    st[1] += 1
                    op.sig = (st[0], st[1], 1)
            else:
                inc = 16 if op.kind == "d" else 1
                k = op.dbuf.name
                st = dsem.get(k)
                if st is None or st[1] >= SEM_LIMIT * 2:
                    st = [new_sem("d_" + op.dbuf.name), 0]
                    dsem[k] = st
                st[1] += inc
                op.sig = (st[0], st[1], inc)
        final = []
        for b in final_wait_bufs:
            st = dsem[b.name]
            final.append((st[0], st[1]))
        self.n_sems = len(sem_names)
        with contextlib.ExitStack() as es:
            handles = [es.enter_context(nc.semaphore(f"s{i}_{n}"[:40])) for i, n in enumerate(sem_names)]
            block = es.enter_context(nc.Block())

            def run(engname, eng):
                known = {}
                if engname == "sync":
                    self.pid = eng.partition_id()
                for op in self.by_eng[engname]:
                    need = {}
                    for d in op.deps:
                        s, v, _ = d.sig
                        if known.get(s, 0) >= v:
                            continue
                        if need.get(s, 0) < v:
                            need[s] = v
                    for s, v in need.items():
                        eng.wait_ge(handles[s], v)
                        known[s] = v
                    ins = op.fn(eng)
                    if op.sig is not None:
                        ins.then_inc(handles[op.sig[0]], op.sig[2])
                if engname == "sync":
                    for s, v in final:
                        eng.wait_ge(handles[s], v)

            @block.tensor
            def _(e):
                run("tensor", e)

            @block.vector
            def _(e):
                run("vector", e)

            @block.scalar
            def _(e):
                run("scalar", e)

            @block.gpsimd
            def _(e):
                run("gpsimd", e)

            @block.sync
            def _(e):
                run("sync", e)


class Tl:
    def __init__(self, ap, name):
        self.ap = ap
        self.b = Buf(name)

    def __getitem__(self, k):
        return self.ap[k]


DT_SIZE = {F32: 4, BF16: 2, I32: 4}


class Builder:
    def __init__(self, n_layers=NL, stage=99):
        self.n_layers = n_layers
        self.stage = stage
        nc = bass.Bass("TRN2", target_bir_lowering=False)
        self.nc = nc
        self.P = Prog(nc)
        self.es = contextlib.ExitStack()

    def dram_in(self, name, shape, dt):
        return Tl(self.nc.dram_tensor(name, list(shape), dt, kind="ExternalInput").ap(), name)

    def dram_out(self, name, shape, dt):
        return Tl(self.nc.dram_tensor(name, list(shape), dt, kind="ExternalOutput").ap(), name)

    def dram_tmp(self, name, shape, dt):
        return Tl(self.nc.dram_tensor(name, list(shape), dt, kind="Internal").ap(), name)

    def sb(self, name, shape, dt):
        n = 1
        for s_ in shape[1:]:
            n *= s_
        nbytes = n * DT_SIZE[dt]
        nbytes = (nbytes + 63) // 64 * 64
        off = self.aoff
        self.aoff += nbytes
        assert self.aoff <= self.asize, (name, self.aoff, self.asize)
        self.apeak = max(self.apeak, self.aoff)
        w = self.arena[0:shape[0], off // 4:(off + nbytes) // 4]
        if dt != F32:
            w = w.bitcast(dt)
        w = w[:, 0:n]
        if len(shape) == 3:
            w = w.rearrange("p (a b) -> p a b", a=shape[1])
        elif len(shape) == 4:
            w = w.rearrange("p (a b c) -> p a b c", a=shape[1], b=shape[2])
        return Tl(w, name)

    def mark(self):
        return self.aoff

    def regions(self, tl, names):
        return [Tl(tl.ap, n) for n in names]

    def release(self, mark):
        self.P.barrier()
        self.aoff = mark

    def MM(self, ps, out_ap, lhsT, rhs, start, stop, reads):
        self.P.c("tensor", lambda e: e.matmul(out_ap, lhsT=lhsT, rhs=rhs, start=start, stop=stop),
                 [t.b for t in reads], [ps.b])

    def TR(self, ps, out_ap, in_ap, reads):
        ident = self.ident
        self.P.c("tensor", lambda e: e.transpose(out_ap, in_ap, ident[:]),
                 [t.b for t in reads] + [ident.b], [ps.b])

    def ACT(self, out_ap, in_ap, func, reads, writes, bias=None, scale=1.0):
        if bias is None:
            fn = lambda e: e.activation(out=out_ap, in_=in_ap, func=func, scale=scale)
        else:
            fn = lambda e: e.activation(out=out_ap, in_=in_ap, func=func, bias=bias, scale=scale)
        self.P.c("scalar", fn, [t.b for t in reads], [t.b for t in writes])

    def TT(self, eng, out_ap, in0, in1, op, reads, writes):
        self.P.c(eng, lambda e: e.tensor_tensor(out=out_ap, in0=in0, in1=in1, op=op),
                 [t.b for t in reads], [t.b for t in writes])

    def TS(self, eng, out_ap, in0, s1, op0, reads, writes, s2=None, op1=None):
        if op1 is None:
            fn = lambda e: e.tensor_scalar(out=out_ap, in0=in0, scalar1=s1, scalar2=None, op0=op0)
        else:
            fn = lambda e: e.tensor_scalar(out=out_ap, in0=in0, scalar1=s1, scalar2=s2, op0=op0, op1=op1)
        self.P.c(eng, fn, [t.b for t in reads], [t.b for t in writes])

    def STT(self, eng, out_ap, in0, scalar, in1, op0, op1, reads, writes):
        self.P.c(eng, lambda e: e.scalar_tensor_tensor(out=out_ap, in0=in0, scalar=scalar, in1=in1, op0=op0, op1=op1),
                 [t.b for t in reads], [t.b for t in writes])

    def CP(self, eng, out_ap, in_ap, reads, writes):
        if eng == "scalar":
            self.ACT(out_ap, in_ap, AF.Copy, reads, writes)
        else:
            self.P.c(eng, lambda e: e.tensor_copy(out=out_ap, in_=in_ap), [t.b for t in reads], [t.b for t in writes])

    def RED(self, eng, out_ap, in_ap, reads, writes):
        self.P.c(eng, lambda e: e.tensor_reduce(out=out_ap, in_=in_ap, axis=AX.X, op=ALU.add),
                 [t.b for t in reads], [t.b for t in writes])

    def RCP(self, out_ap, in_ap, reads, writes):
        self.P.c("vector", lambda e: e.reciprocal(out=out_ap, in_=in_ap), [t.b for t in reads], [t.b for t in writes])

    def MEMSET(self, eng, ap, val, writes):
        self.P.c(eng, lambda e: e.memset(ap, val), [], [t.b for t in writes])

    def DMA(self, out_ap, in_ap, reads, writes, eng="sync"):
        self.P.dma(eng, lambda e: e.dma_start(out=out_ap, in_=in_ap), [t.b for t in reads], [t.b for t in writes])

    def rstd_from(self, out_tl, out_ap, ps, ps_ap):
        self.ACT(out_ap, ps_ap, AF.Sqrt, [ps, self.cst], [out_tl], bias=self.cst[:, 0:1])
        self.RCP(out_ap, out_ap, [out_tl], [out_tl])

    def wunit(self, wt, src_ap, a, b_):
        st = self.stg[self.stg_i % len(self.stg)]
        self.stg_i += 1
        sl = self.wsl[self.wsl_i % len(self.wsl)]
        self.wsl_i += 1
        sv = st[:, 0:a * b_].rearrange("p (a b) -> p a b", a=a)
        wv = sl[:, 0:a * b_].rearrange("p (a b) -> p a b", a=a)
        self.DMA(st[:, 0:a * b_], src_ap[:, 0:a * b_], [wt], [st])
        self.cast_i += 1
        self.CP(("scalar", "vector", "scalar", "gpsimd")[self.cast_i % 4], wv, sv, [st], [sl])
        return sl, wv

    def wunit_to(self, wt, src_ap, nelem, a, sl, sl_ap):
        st = self.stg[self.stg_i % len(self.stg)]
        self.stg_i += 1
        self.DMA(st[:, 0:nelem], src_ap, [wt], [st])
        self.cast_i += 1
        self.CP(("scalar", "vector", "scalar", "gpsimd")[self.cast_i % 4], sl_ap, st[:, 0:nelem], [st], [sl])
        return sl, sl_ap.rearrange("p (a b) -> p a b", a=a)

    def stream(self, units, consume, depth=3):
        n = len(units)
        got = []
        for k in range(min(depth, n)):
            got.append(self.wunit(*units[k]))
        for k in range(n):
            if k + depth < n:
                got.append(self.wunit(*units[k + depth]))
            consume(k, got[k][0], got[k][1])

    def build(self):
        nc = self.nc
        P = self.P
        L = self.n_layers
        es = self.es
        with es:
            self._build(nc, P, L, es)
        return nc

    def _build(self, nc, P, L, es):
        xT = self.dram_in("xT", [16, 128, TOK], F32)
        pos_own = self.dram_in("pos_own", [128, 16], I32)
        pos_all = self.dram_in("pos_all", [128, 64], I32)
        w_uq = self.dram_in("w_uq_my", [NL, 512, 384], F32)
        w_ukv = self.dram_in("w_ukv_my", [NL, 256, 512], F32)
        wspec = {"w_in": 40, "w_o": 16, "w_up": 64, "w_down": 64}
        wfl = {}
        for nm, nu in wspec.items():
            t_ = self.nc.dram_tensor(nm + "_t", [NL, nu * 128, 2048], F32, kind="ExternalInput").ap()
            wfl[nm] = [Tl(t_[i], f"{nm}_t{i}") for i in range(NL)]

        def bounce_weights(l_):
            pass

        def gather_weights(l_, names):
            pass
        gains = self.dram_in("gains", [128, 256], F32)
        cfs = self.dram_in("cfs", [128, 2048], F32)
        outT = self.dram_out("outT", [16, 128, TOK], F32)
        xres = self.dram_tmp("xres", [16, 128, TOK], F32)
        sc_q = self.dram_tmp("sc_q", [16, 128, 1024], BF16)
        sc_k = self.dram_tmp("sc_k", [16, 128, 1024], BF16)
        sc_v = self.dram_tmp("sc_v", [16, 128, 1024], BF16)
        sc_g = self.dram_tmp("sc_g", [16, 128, 1024], BF16)
        b_lat = [self.dram_tmp(f"b_lat{i}", [7 * 128, 512], BF16) for i in range(4)]
        g_lat = [self.dram_tmp(f"g_lat{i}", [4 * 7 * 128, 512], BF16) for i in range(4)]
        b_st = self.dram_tmp("b_st", [128, 1024], F32)
        g_st = self.dram_tmp("g_st", [512, 1024], F32)
        b_att = [self.dram_tmp(f"b_att{i}", [256, 1024], BF16) for i in range(8)]
        g_att_all = self.nc.dram_tensor("g_att", [8, 4 * 256, 1024], BF16, kind="Internal").ap()
        g_att = [Tl(g_att_all[i], f"g_att{i}") for i in range(8)]

        self.asize = 207 * 1024
        self.arena = es.enter_context(nc.sbuf_tensor("arena", [128, self.asize // 4], F32))
        self.aoff = 0
        self.apeak = 0
        banks = [Tl(es.enter_context(nc.psum_tensor(f"bank{i}", [128, 512], F32)), f"bank{i}") for i in range(8)]
        self.banks = banks

        gn = self.sb("gains", [128, 256], F32)
        cst = self.sb("cst", [128, 16], F32)
        self.cst = cst
        ident = self.sb("ident", [128, 128], BF16)
        self.ident = ident
        ones = self.sb("ones", [128, 4, 128], BF16)
        one1 = self.sb("one1", [128, 128], BF16)
        mask = self.sb("mask", [128, 128], BF16)
        wq = self.sb("wq", [128, 8], F32)
        wk = self.sb("wk", [128, 8], F32)
        gtab = self.sb("gtab", [128, 8, 128], F32)
        coef = self.sb("coef", [128, 4, 8], F32)
        cosR = self.sb("cosR", [128, 16, 64], F32)
        sinR = self.sb("sinR", [128, 16, 64], F32)
        cosK = self.sb("cosK", [128, 16, 32], F32)
        sinK = self.sb("sinK", [128, 16, 32], F32)
        cosQ = self.sb("cosQ", [128, 64, 32], BF16)
        sinQ = self.sb("sinQ", [128, 64, 32], BF16)
        Sst = self.sb("Sst", [128, 8, 128], F32)
        Sbf = self.sb("Sbf", [128, 8, 128], BF16)
        self.stg = [self.sb(f"stg{i}", [128, 2048], F32) for i in range(2)]
        self.wsl = [self.sb(f"wsl{i}", [128, 2048], BF16) for i in range(6)]
        self.stg_i = 0
        self.wsl_i = 0
        self.cast_i = 0
        base_mark = self.mark()

        self.DMA(gn[:], gains[:], [gains], [gn])
        G_ATT, G_MLP, G_QN, G_KVN, G_BA, G_BR, G_FIN = 0, 64, 128, 144, 152, 184, 216

        ctmp = self.sb("ctmp", [128, 2048], F32)
        self.DMA(ctmp[:], cfs[:], [cfs], [ctmp])
        self.CP("vector", ident[:], ctmp[:, 0:128], [ctmp], [ident])
        self.CP("vector", mask[:], ctmp[:, 128:256], [ctmp], [mask])
        self.CP("vector", wq[:], ctmp[:, 256:264], [ctmp], [wq])
        self.CP("vector", wk[:], ctmp[:, 264:272], [ctmp], [wk])
        self.CP("vector", gtab[:], ctmp[:, 272:1296].rearrange("p (a b) -> p a b", a=8), [ctmp], [gtab])
        self.CP("vector", coef[:], ctmp[:, 1296:1328].rearrange("p (a b) -> p a b", a=4), [ctmp], [coef])
        self.MEMSET("gpsimd", cst[:, 0:1], EPS, [cst])
        self.MEMSET("gpsimd", cst[:, 1:2], 0.0, [cst])
        for i, v in enumerate([1.0 / 2048, 1.0 / 512, 1.0 / 256, 1.0 / 1024]):
            self.MEMSET("gpsimd", ones[:, i, :], v, [ones])
        self.MEMSET("gpsimd", one1[:], 1.0, [one1])
        self.MEMSET("gpsimd", Sst[:], 0.0, [Sst])

        def rope_table(pos_dram, nt, invf_ap, nf, cos_t, sin_t):
            m = self.mark()
            pi_ = self.sb("pos_i", [128, nt], I32)
            pf = self.sb("pos_f", [128, nt], F32)
            ang = self.sb("ang", [128, nt, nf], F32)
            u = self.sb("u", [128, nt, nf], F32)
            r = self.sb("r", [128, nt, nf], F32)
            self.DMA(pi_[:], pos_dram[:], [pos_dram], [pi_])
            self.CP("vector", pf[:], pi_[:], [pi_], [pf])
            self.TT("vector", ang[:], invf_ap.unsqueeze(1).to_broadcast([128, nt, nf]),
                    pf[:].unsqueeze(2).to_broadcast([128, nt, nf]), ALU.mult, [ctmp, pf], [ang])
            for which, dst in ((0, sin_t), (1, cos_t)):
                if which == 1:
                    self.TS("vector", ang[:], ang[:], float(np.pi / 2), ALU.add, [ang], [ang])
                self.TS("vector", u[:], ang[:], float(1.0 / TWO_PI), ALU.mult, [ang], [u])
                self.TS("vector", u[:], u[:], MAGIC, ALU.add, [u], [u])
                self.TS("vector", u[:], u[:], MAGIC, ALU.subtract, [u], [u])
                self.STT("vector", r[:], u[:], -CW1, ang[:], ALU.mult, ALU.add, [u, ang], [r])
                self.STT("vector", r[:], u[:], -CW2, r[:], ALU.mult, ALU.add, [u, r], [r])
                self.TS("vector", r[:], r[:], -3.1415925, ALU.max, [r], [r], s2=3.1415925, op1=ALU.min)
                self.ACT(dst[:], r[:], AF.Sin, [r], [dst])
            self.release(m)

        rope_table(pos_own, 16, ctmp[:, 1360:1424], 64, cosR, sinR)
        rope_table(pos_own, 16, ctmp[:, 1328:1360], 32, cosK, sinK)
        rope_table(pos_all, 64, ctmp[:, 1328:1360], 32, cosQ, sinQ)

        bounce_weights(0)
        gather_weights(0, ["w_in", "w_o", "w_up", "w_down"])
        for kc in range(16):
            self.DMA(xres[kc], xT[kc], [xT], [xres])
        self.release(base_mark)
        if self.stage <= 0:
            for kc in range(16):
                self.DMA(outT[kc], xres[kc], [xres], [outT])
            P.emit(final_wait_bufs=[outT.b])
            return

        bank_i = [0]

        def nb():
            bk = banks[bank_i[0] % 8]
            bank_i[0] += 1
            return bk

        def rope_tm(src, nh, hd, cos_ap, sin_ap, out_tl, out_view, scale_ap, tmp):
            src_tl, src_ap = src
            h2 = hd // 2
            xs, t1, t2 = tmp
            xsv = xs[:, 0:nh * hd].rearrange("p (a b) -> p a b", a=nh)
            t1v = t1[:, 0:nh * h2].rearrange("p (a b) -> p a b", a=nh)
            t2v = t2[:, 0:nh * h2].rearrange("p (a b) -> p a b", a=nh)
            if scale_ap is not None:
                self.TT("vector", xsv, src_ap, scale_ap.unsqueeze(2).to_broadcast([128, nh, hd]), ALU.mult,
                        [src_tl, wq, wk], [xs])
            else:
                self.CP("scalar", xsv, src_ap, [src_tl], [xs])
            cb = cos_ap.unsqueeze(1).to_broadcast([128, nh, h2])
            sbb = sin_ap.unsqueeze(1).to_broadcast([128, nh, h2])
            tabs = [cosR, sinR, cosK, sinK, cosQ, sinQ]
            x1 = xsv[:, :, 0:h2]
            x2 = xsv[:, :, h2:hd]
            self.TT("vector", t1v, x1, cb, ALU.mult, [xs] + tabs, [t1])
            self.TT("gpsimd", t2v, x2, sbb, ALU.mult, [xs] + tabs, [t2])
            self.TT("vector", out_view[:, :, 0:h2], t1v, t2v, ALU.subtract, [t1, t2], [out_tl])
            self.TT("vector", t1v, x2, cb, ALU.mult, [xs] + tabs, [t1])
            self.TT("gpsimd", t2v, x1, sbb, ALU.mult, [xs] + tabs, [t2])
            self.TT("vector", out_view[:, :, h2:hd], t1v, t2v, ALU.add, [t1, t2], [out_tl])

        for l in range(L):
            lmark = self.mark()
            w_in, w_o, w_up, w_down = wfl["w_in"][l], wfl["w_o"][l], wfl["w_up"][l], wfl["w_down"][l]
            if l + 1 < L:
                bounce_weights(l + 1)
            def wu(wt_, u_):
                return wt_.ap[u_ * 128:(u_ + 1) * 128, :]
            self.MEMSET("gpsimd", Sst[:], 0.0, [Sst])
            for h in range(2):
                m = self.mark()
                tsl = slice(h * HALF, (h + 1) * HALF)
                xb = self.sb("xb", [128, 16, HALF], BF16)
                rstd = self.sb("rstd", [128, HALF], F32)
                xring = [self.sb(f"xr{i}", [128, HALF], F32) for i in range(3)]
                sqr = [self.sb(f"sq{i}", [128, HALF], BF16) for i in range(2)]
                bA, bB = banks[0], banks[1]
                for kc in range(16):
                    xr = xring[kc % 3]
                    sq = sqr[kc % 2]
                    self.DMA(xr[:], xres[kc][:, tsl], [xres], [xr])
                    self.ACT(sq[:], xr[:], AF.Square, [xr], [sq])
                    for tt, bk in ((0, bA), (1, bB)):
                        self.MM(bk, bk[:], ones[:, 0, :], sq[:, tt * 512:(tt + 1) * 512], kc == 0, kc == 15, [ones, sq])
                for tt, bk in ((0, bA), (1, bB)):
                    self.rstd_from(rstd, rstd[:, tt * 512:(tt + 1) * 512], bk, bk[:])
                for kc in range(16):
                    xr = xring[(kc + 1) % 3]
                    self.DMA(xr[:], xres[kc][:, tsl], [xres], [xr])
                    self.STT("vector", xb[:, kc, :], xr[:],
                             gn[:, G_ATT + l * 16 + kc:G_ATT + l * 16 + kc + 1], rstd[:], ALU.mult, ALU.mult,
                             [xr, gn, rstd], [xb])
                lat = self.sb("lat", [128, 4, HALF], F32)
                lsq = self.sb("lsq", [128, HALF], BF16)
                lrs = self.sb("lrs", [128, HALF], F32)
                bnc = self.sb("bnc", [128, 7, HALF], BF16)
                for (c0, nch, onei, gcol, boff) in ((0, 4, 1, G_QN + l * 4, 0), (512, 2, 2, G_KVN + l * 2, 4)):
                    units = [(w_in, wu(w_in, boff + j), 16, 128) for j in range(nch)]

                    def cons(k, sl, wv, nch=nch):
                        for tt in range(2):
                            bk = nb()
                            for kc in range(16):
                                self.MM(bk, bk[:], wv[:, kc, :], xb[:, kc, tt * 512:(tt + 1) * 512], kc == 0, kc == 15, [sl, xb])
                            self.CP("scalar", lat[:, k, tt * 512:(tt + 1) * 512], bk[:], [bk], [lat])
                    self.stream(units, cons)
                    for tt in range(2):
                        bk = nb()
                        for j in range(nch):
                            self.ACT(lsq[:, tt * 512:(tt + 1) * 512], lat[:, j, tt * 512:(tt + 1) * 512], AF.Square, [lat], [lsq])
                            self.MM(bk, bk[:], ones[:, onei, :], lsq[:, tt * 512:(tt + 1) * 512], j == 0, j == nch - 1, [ones, lsq])
                        self.rstd_from(lrs, lrs[:, tt * 512:(tt + 1) * 512], bk, bk[:])
                    for j in range(nch):
                        self.STT("vector", bnc[:, boff + j, :], lat[:, j, :], gn[:, gcol + j:gcol + j + 1], lrs[:],
                                 ALU.mult, ALU.mult, [lat, gn, lrs], [bnc])
                rtmp = [(self.sb(f"xs_t{i}", [128, 512], F32), self.sb(f"t1_t{i}", [128, 256], F32), self.sb(f"t2_t{i}", [128, 256], F32))
                        for i in range(2)]
                rti = [0]

                def rt():
                    rti[0] += 1
                    return rtmp[rti[0] % 2]
                ktm = self.sb("ktm", [128, 8, 1024], BF16)
                orow = [self.sb(f"orow{i}", [128, 512], BF16) for i in range(2)]
                krt = self.sb("krt", [128, 128], BF16)
                oi = [0]
                gotk = self.wunit(w_in, wu(w_in, 6), 16, 64)
                for t in range(8):
                    bk = nb()
                    for kc in range(16):
                        self.MM(bk, bk[:, 0:64], xb[:, kc, t * 128:(t + 1) * 128], gotk[1][:, kc, :], kc == 0, kc == 15,
                                [xb, gotk[0]])
                    gt = h * 8 + t
                    rope_tm((bk, bk[:, 0:64].rearrange("p (a b) -> p a b", a=1)), 1, 64, cosK[:, gt, :], sinK[:, gt, :],
                            krt, krt[:, 0:64].rearrange("p (a b) -> p a b", a=1), None, rt())
                    self.CP("gpsimd", krt[:, 64:128], krt[:, 0:64], [krt], [krt])
                    bk2 = nb()
                    bv = bk2[:, 0:64].bitcast(BF16)
                    self.TR(bk2, bv, krt[:], [krt])
                    self.CP("scalar", bnc[:, 6, t * 128:(t + 1) * 128], bv, [bk2], [bnc])
                for tt in range(2):
                    gtt = h * 2 + tt
                    for c_ in range(7):
                        self.DMA(b_lat[gtt][c_ * 128:(c_ + 1) * 128, :],
                                 bnc[:, c_, tt * 512:(tt + 1) * 512], [bnc], [b_lat[gtt]])
                for gi in range(8):
                    kind = gi // 2
                    hg = gi % 2
                    units = [(w_in, wu(w_in, 7 + gi * 4 + j), 4, 512) for j in range(4)]
                    got = [self.wunit(*u) for u in units]
                    for t in range(8):
                        gt = h * 8 + t
                        bk = nb()
                        for kc in range(16):
                            self.MM(bk, bk[:], xb[:, kc, t * 128:(t + 1) * 128], got[kc // 4][1][:, kc % 4, :], kc == 0, kc == 15,
                                    [xb, got[kc // 4][0]])
                        bk3 = bk[:].rearrange("p (a b) -> p a b", a=4)
                        if kind == 0:
                            o = orow[oi[0] % 2]
                            oi[0] += 1
                            rope_tm((bk, bk3), 4, 128, cosR[:, gt, :], sinR[:, gt, :], o,
                                    o[:].rearrange("p (a b) -> p a b", a=4), wq[:, hg * 4:hg * 4 + 4], rt())
                            self.DMA(sc_q[gt][:, hg * 512:(hg + 1) * 512], o[:], [o], [sc_q])
                        elif kind == 1:
                            rope_tm((bk, bk3), 4, 128, cosR[:, gt, :], sinR[:, gt, :], ktm,
                                    ktm[:, t, hg * 512:(hg + 1) * 512].rearrange("p (a b) -> p a b", a=4),
                                    wk[:, hg * 4:hg * 4 + 4], rt())
                            self.DMA(sc_k[gt][:, hg * 512:(hg + 1) * 512], ktm[:, t, hg * 512:(hg + 1) * 512], [ktm], [sc_k])
                        elif kind == 2:
                            o = orow[oi[0] % 2]
                            oi[0] += 1
                            self.CP("scalar", o[:], bk[:], [bk], [o])
                            self.DMA(sc_v[gt][:, hg * 512:(hg + 1) * 512], o[:], [o], [sc_v])
                            bm = nb()
                            for hh in range(4):
                                self.MM(bm, bm[:, hh * 128:(hh + 1) * 128], ktm[:, t, (hg * 4 + hh) * 128:(hg * 4 + hh + 1) * 128],
                                        o[:, hh * 128:(hh + 1) * 128], True, True, [ktm, o])
                            sv = Sst[:, hg * 4:hg * 4 + 4, :]
                            self.TT("vector", sv, sv, bm[:].rearrange("p (a b) -> p a b", a=4), ALU.add, [Sst, bm], [Sst])
                            self.TT("vector", sv, sv, gtab[:, hg * 4:hg * 4 + 4, :], ALU.mult, [Sst, gtab], [Sst])
                        else:
                            o = orow[oi[0] % 2]
                            oi[0] += 1
                            self.ACT(o[:], bk[:], AF.Silu, [bk], [o])
                            self.DMA(sc_g[gt][:, hg * 512:(hg + 1) * 512], o[:], [o], [sc_g])
                self.release(m)
            self.DMA(b_st[:], Sst[:].rearrange("p a b -> p (a b)"), [Sst], [b_st])
            for i_ in range(4):
                P.cc(lambda e, i_=i_: e.collective_compute("AllGather", ALU.bypass, replica_groups=GROUPS, ins=[b_lat[i_].ap], outs=[g_lat[i_].ap]),
                     [b_lat[i_].b], [g_lat[i_].b])
            P.cc(lambda e: e.collective_compute("AllGather", ALU.bypass, replica_groups=GROUPS, ins=[b_st.ap], outs=[g_st.ap]),
                 [b_st.b], [g_st.b])
            if l + 1 < L:
                gather_weights(l + 1, ["w_in", "w_o"])
            if self.stage == 1:
                for kc in range(16):
                    self.DMA(outT[kc], xres[kc], [xres], [outT])
                P.emit(final_wait_bufs=[outT.b, g_st.b] + [g_lat[i_].b for i_ in range(4)])
                return
            m = self.mark()
            wuq = self.sb("wuq", [128, 4, 384], BF16)
            wukv = self.sb("wukv", [128, 2, 512], BF16)
            wtmp = self.sb("wtmp", [128, 4, 384], F32)
            for kc in range(4):
                self.DMA(wtmp[:, kc, :], w_uq[l][kc * 128:(kc + 1) * 128, :], [w_uq], [wtmp])
            self.CP("gpsimd", wuq[:], wtmp[:], [wtmp], [wuq])
            wtmp2 = self.sb("wtmp2", [128, 2, 512], F32)
            for kc in range(2):
                self.DMA(wtmp2[:, kc, :], w_ukv[l][kc * 128:(kc + 1) * 128, :], [w_ukv], [wtmp2])
            self.CP("gpsimd", wukv[:], wtmp2[:], [wtmp2], [wukv])
            KT = self.sb("KT", [128, 2, S], BF16)
            KR = self.sb("KR", [128, S], BF16)
            VV = self.sb("VV", [128, 64, 256], BF16)
            latr = [self.sb(f"latr{i}", [128, 6, 512], BF16) for i in range(2)]
            QN = [self.sb(f"QN{i}", [128, 2, 512], BF16) for i in range(2)]
            QR = [self.sb(f"QR{i}", [128, 512], BF16) for i in range(2)]
            qrt = [self.sb(f"qrt{i}", [128, 128], BF16) for i in range(2)]
            PT = [self.sb(f"PT{i}", [128, 512], BF16) for i in range(3)]
            rden = self.sb("rden", [128, 512], F32)
            ao = [self.sb(f"ao{i}", [128, 512], BF16) for i in range(2)]
            xs_t = self.sb("xs_t", [128, 512], F32)
            t1_t = self.sb("t1_t", [128, 256], F32)
            t2_t = self.sb("t2_t", [128, 256], F32)
            pti = [0]
            aoi = [0]
            KTr = [Tl(KT.ap, f"KT{q}") for q in range(16)]
            KRr = [Tl(KR.ap, f"KR{q}") for q in range(16)]
            VVr = [Tl(VV.ap, f"VV{q}") for q in range(16)]
            g_lat_vs = [g_lat[i_].ap.rearrange("(r c p) t -> r p c t", c=7, p=128) for i_ in range(4)]
            for qt in range(16):
                lt = latr[qt % 2]
                qn = QN[qt % 2]
                qr = QR[qt % 2]
                glv = g_lat_vs[qt % 4][qt // 4]
                for c_ in range(6):
                    self.DMA(lt[:, c_, :], glv[:, c_, :], [g_lat[qt % 4]], [lt])
                self.DMA(KR[:, qt * 512:(qt + 1) * 512], glv[:, 6, :], [g_lat[qt % 4]], [KRr[qt]])
                for hh in range(2):
                    bk = nb()
                    for kc in range(2):
                        self.MM(bk, bk[:], wukv[:, kc, hh * 128:(hh + 1) * 128], lt[:, 4 + kc, :], kc == 0, kc == 1, [wukv, lt])
                    self.CP("scalar" if hh == 0 else "vector", KT[:, hh, qt * 512:(qt + 1) * 512], bk[:], [bk], [KTr[qt]])
                for j in range(4):
                    bk = nb()
                    for kc in range(2):
                        self.MM(bk, bk[:, 0:256], lt[:, 4 + kc, j * 128:(j + 1) * 128], wukv[:, kc, 256:512], kc == 0, kc == 1, [wukv, lt])
                    self.CP("vector" if j % 2 == 0 else "scalar", VV[:, qt * 4 + j, :], bk[:, 0:256], [bk], [VVr[qt]])
                for hh in range(2):
                    bk = nb()
                    for kc in range(4):
                        self.MM(bk, bk[:], wuq[:, kc, hh * 128:(hh + 1) * 128], lt[:, kc, :], kc == 0, kc == 3, [wuq, lt])
                    self.CP("scalar" if hh == 0 else "vector", qn[:, hh, :], bk[:], [bk], [qn])
                for j in range(4):
                    bk = nb()
                    for kc in range(4):
                        self.MM(bk, bk[:, 0:128], lt[:, kc, j * 128:(j + 1) * 128], wuq[:, kc, 256:384], kc == 0, kc == 3, [wuq, lt])
                    qq = qrt[j % 2]
                    rope_tm((bk, bk[:, 0:128].rearrange("p (a b) -> p a b", a=2)), 2, 64, cosQ[:, qt * 4 + j, :], sinQ[:, qt * 4 + j, :],
                            qq, qq[:].rearrange("p (a b) -> p a b", a=2), None, (xs_t, t1_t, t2_t))
                    bk2 = nb()
                    bv = bk2[:, 0:64].bitcast(BF16)
                    self.TR(bk2, bv, qq[:], [qq])
                    self.CP("scalar", qr[:, j * 128:(j + 1) * 128], bv, [bk2], [qr])
                nkt = 4 * qt + 4
                for hh in range(2):
                    OUT = banks[6] if hh == 0 else banks[4]
                    DEN = banks[7] if hh == 0 else banks[5]
                    for kt in range(nkt):
                        r_ = kt - 4 * qt
                        c0 = 0 if r_ <= 0 else r_ * 128
                        sbk = banks[kt % 4]
                        self.MM(sbk, sbk[:, c0:512], KT[:, hh, kt * 128:(kt + 1) * 128], qn[:, hh, c0:512], True, False, [KTr[kt // 4], qn])
                        self.MM(sbk, sbk[:, c0:512], KR[hh * 64:(hh + 1) * 64, kt * 128:(kt + 1) * 128], qr[hh * 64:(hh + 1) * 64, c0:512],
                                False, True, [KRr[kt // 4], qr])
                        pt = PT[pti[0] % 3]
                        pti[0] += 1
                        self.ACT(pt[:, c0:512], sbk[:, c0:512], AF.Exp, [sbk], [pt], scale=ATT_SCALE)
                        if r_ >= 0:
                            self.TT("gpsimd", pt[:, c0:c0 + 128], pt[:, c0:c0 + 128], mask[:], ALU.mult, [pt, mask], [pt])
                        self.MM(OUT, OUT[:, c0:512], VV[:, kt, hh * 128:(hh + 1) * 128], pt[:, c0:512], kt == 0, kt == nkt - 1, [VVr[kt // 4], pt])
                        self.MM(DEN, DEN[:, c0:512], one1[:], pt[:, c0:512], kt == 0, kt == nkt - 1, [one1, pt])
                    self.RCP(rden[:], DEN[:], [DEN], [rden])
                    a_ = ao[aoi[0] % 2]
                    aoi[0] += 1
                    self.TT("vector", a_[:], OUT[:], rden[:], ALU.mult, [OUT, rden], [a_])
                    blk = qt // 2
                    self.DMA(b_att[blk][hh * 128:(hh + 1) * 128, (qt % 2) * 512:(qt % 2 + 1) * 512], a_[:], [a_], [b_att[blk]])
                bank_i[0] = 0
            self.release(m)
            for i_ in range(8):
                P.cc(lambda e, i_=i_: e.collective_compute("AllGather", ALU.bypass, replica_groups=GROUPS, ins=[b_att[i_].ap], outs=[g_att[i_].ap]),
                     [b_att[i_].b], [g_att[i_].b])
            if l + 1 < L:
                gather_weights(l + 1, ["w_up", "w_down"])
            if self.stage == 2:
                for kc in range(16):
                    self.DMA(outT[kc], xres[kc], [xres], [outT])
                P.emit(final_wait_bufs=[outT.b] + [g_att[i_].b for i_ in range(8)])
                return
            m = self.mark()
            gs = self.sb("gs", [128, 4, 1024], F32)
            for r_ in range(4):
                self.DMA(gs[:, r_, :], g_st.ap[r_ * 128:(r_ + 1) * 128, :], [g_st], [gs])
            stmp = self.sb("stmp", [128, 8, 128], F32)
            for r_ in range(4):
                dst = Sst if r_ == 0 else stmp
                self.TT("vector", dst[:], gs[:, r_, :].rearrange("p (a b) -> p a b", a=8),
                        coef[:, r_, :].unsqueeze(2).to_broadcast([128, 8, 128]), ALU.mult, [gs, coef], [dst])
                if r_ > 0:
                    self.TT("vector", Sst[:], Sst[:], stmp[:], ALU.add, [Sst, stmp], [Sst])
            self.CP("vector", Sbf[:], Sst[:], [Sst], [Sbf])
            self.release(m)
            for h in range(2):
                m = self.mark()
                tsl = slice(h * HALF, (h + 1) * HALF)
                xacc = self.sb("xacc", [128, 16, HALF], F32)
                mixed = self.sb("mixed", [128, 16, HALF], BF16)
                xar = [self.regions(xacc, [f"xacc_{kc}_0", f"xacc_{kc}_1"]) for kc in range(16)]
                for kc in range(16):
                    self.DMA(xacc[:, kc, :], xres[kc][:, tsl], [xres], xar[kc])
                m2 = self.mark()
                rin = [[self.sb(f"rin{i}_{j}", [128, 1024], BF16) for j in range(4)] for i in range(2)]
                QTt = self.sb("QTt", [128, 8, 128], BF16)
                KTt = self.sb("KTt", [128, 8, 128], BF16)
                PTt = self.sb("PTt", [128, 8, 128], BF16)
                osb = self.sb("osb", [128, 8, 128], F32)
                osq = self.sb("osq", [128, 8, 128], F32)
                rr = self.sb("rr", [128, 8, 128], BF16)
                st8 = self.sb("st8", [128, 4, 8], F32)
                for t in range(8):
                    gt = h * 8 + t
                    q_, k_, v_, g_ = rin[t % 2]
                    self.DMA(q_[:], sc_q[gt], [sc_q], [q_])
                    self.DMA(k_[:], sc_k[gt], [sc_k], [k_])
                    self.DMA(v_[:], sc_v[gt], [sc_v], [v_])
                    self.DMA(g_[:], sc_g[gt], [sc_g], [g_])
                    bq, bkk = banks[0], banks[1]
                    bqv = bq[:].bitcast(BF16)
                    bkv = bkk[:].bitcast(BF16)
                    for hh in range(8):
                        self.TR(bq, bqv[:, hh * 128:(hh + 1) * 128], q_[:, hh * 128:(hh + 1) * 128], [q_])
                    for hh in range(8):
                        self.TR(bkk, bkv[:, hh * 128:(hh + 1) * 128], k_[:, hh * 128:(hh + 1) * 128], [k_])
                    self.CP("scalar", QTt[:].rearrange("p a b -> p (a b)"), bqv, [bq], [QTt])
                    self.CP("vector", KTt[:].rearrange("p a b -> p (a b)"), bkv, [bkk], [KTt])
                    for half8 in range(2):
                        bs = banks[2 + half8]
                        for hh in range(4):
                            hd_ = half8 * 4 + hh
                            self.MM(bs, bs[:, hh * 128:(hh + 1) * 128], KTt[:, hd_, :], QTt[:, hd_, :], True, True, [KTt, QTt])
                        self.TT("vector", PTt[:, half8 * 4:half8 * 4 + 4, :], bs[:].rearrange("p (a b) -> p a b", a=4),
                                mask[:].unsqueeze(1).to_broadcast([128, 4, 128]), ALU.mult, [bs, mask], [PTt])
                    for half8 in range(2):
                        bo = banks[4 + half8]
                        for hh in range(4):
                            hd_ = half8 * 4 + hh
                            self.MM(bo, bo[:, hh * 128:(hh + 1) * 128], PTt[:, hd_, :], v_[:, hd_ * 128:(hd_ + 1) * 128], True, False, [PTt, v_])
                            self.MM(bo, bo[:, hh * 128:(hh + 1) * 128], QTt[:, hd_, :], Sbf[:, hd_, :], False, True, [QTt, Sbf])
                        self.CP("scalar", osb[:, half8 * 4:half8 * 4 + 4, :], bo[:].rearrange("p (a b) -> p a b", a=4), [bo], [osb])
                    for half8 in range(2):
                        bm = banks[6 + half8]
                        for hh in range(4):
                            hd_ = half8 * 4 + hh
                            self.MM(bm, bm[:, hh * 128:(hh + 1) * 128], k_[:, hd_ * 128:(hd_ + 1) * 128], v_[:, hd_ * 128:(hd_ + 1) * 128],
                                    True, True, [k_, v_])
                        sv = Sst[:, half8 * 4:half8 * 4 + 4, :]
                        self.TT("vector", sv, sv, bm[:].rearrange("p (a b) -> p a b", a=4), ALU.add, [Sst, bm], [Sst])
                        self.TT("vector", sv, sv, gtab[:, half8 * 4:half8 * 4 + 4, :], ALU.mult, [Sst, gtab], [Sst])
                    self.CP("gpsimd", Sbf[:], Sst[:], [Sst], [Sbf])
                    self.RED("vector", st8[:, 0, :], osb[:], [osb], [st8])
                    self.ACT(osq[:], osb[:], AF.Square, [osb], [osq])
                    self.RED("vector", st8[:, 1, :], osq[:], [osq], [st8])
                    self.TS("vector", st8[:, 0, :], st8[:, 0, :], 1.0 / 128, ALU.mult, [st8], [st8])
                    self.TT("vector", st8[:, 2, :], st8[:, 0, :], st8[:, 0, :], ALU.mult, [st8], [st8])
                    self.STT("vector", st8[:, 1, :], st8[:, 1, :], 1.0 / 128, st8[:, 2, :], ALU.mult, ALU.subtract, [st8], [st8])
                    self.ACT(st8[:, 3, :], st8[:, 1, :], AF.Sqrt, [st8, cst], [st8], bias=cst[:, 0:1])
                    self.RCP(st8[:, 3, :], st8[:, 3, :], [st8], [st8])
                    self.TT("vector", osb[:], osb[:], st8[:, 0, :].unsqueeze(2).to_broadcast([128, 8, 128]), ALU.subtract, [osb, st8], [osb])
                    self.TT("gpsimd", osb[:], osb[:], st8[:, 3, :].unsqueeze(2).to_broadcast([128, 8, 128]), ALU.mult, [osb, st8], [osb])
                    self.TT("vector", rr[:].rearrange("p a b -> p (a b)"), osb[:].rearrange("p a b -> p (a b)"), g_[:], ALU.mult, [osb, g_], [rr])
                    br_ = banks[0]
                    brv = br_[:].bitcast(BF16)
                    for hh in range(8):
                        self.TR(br_, brv[:, hh * 128:(hh + 1) * 128], rr[:, hh, :], [rr])
                    self.TT("vector", mixed[:, 8:16, t * 128:(t + 1) * 128], brv.rearrange("p (a b) -> p a b", a=8),
                            gn[:, G_BR + l * 8:G_BR + l * 8 + 8].unsqueeze(2).to_broadcast([128, 8, 128]), ALU.mult, [br_, gn], [mixed])
                self.release(m2)
                m2 = self.mark()
                araw = self.sb("araw", [128, 8, HALF], BF16)
                asq = self.sb("asq", [128, 512], BF16)
                ars = self.sb("ars", [128, HALF], F32)
                g_att_v = g_att_all.rearrange("b (r c p) t -> b p (r c) t", r=4, c=2, p=128)
                for j_ in range(8):
                    def dyn(e, j_=j_, h=h):
                        blk = (P.pid % 4) * 2 + h
                        return e.dma_start(out=araw[:, j_, :], in_=g_att_v[bass.ds(blk, 1)].rearrange("o p j t -> p (o j) t")[:, j_, :])
                    P.dma("sync", dyn, [t_.b for t_ in g_att], [araw.b])
                for tt in range(2):
                    bk = nb()
                    for j in range(8):
                        self.ACT(asq[:], araw[:, j, tt * 512:(tt + 1) * 512], AF.Square, [araw], [asq])
                        self.MM(bk, bk[:], ones[:, 3, :], asq[:], j == 0, j == 7, [ones, asq])
                    self.rstd_from(ars, ars[:, tt * 512:(tt + 1) * 512], bk, bk[:])
                for j in range(8):
                    self.STT("vector", mixed[:, j, :], araw[:, j, :], gn[:, G_BA + l * 8 + j:G_BA + l * 8 + j + 1], ars[:],
                             ALU.mult, ALU.mult, [araw, gn, ars], [mixed])
                self.release(m2)
                m2 = self.mark()
                units = [(w_o, wu(w_o, oc), 16, 128) for oc in range(16)]

                def cons_o(k, sl, wv):
                    for tt in range(2):
                        bk = nb()
                        for mc in range(16):
                            self.MM(bk, bk[:], wv[:, mc, :], mixed[:, mc, tt * 512:(tt + 1) * 512], mc == 0, mc == 15, [sl, mixed])
                        xa = xacc[:, k, tt * 512:(tt + 1) * 512]
                        self.TT("vector", xa, xa, bk[:], ALU.add, [xar[k][tt], bk], [xar[k][tt]])
                self.stream(units, cons_o)
                xb = self.sb("xb2", [128, 16, HALF], BF16) if False else None
                self.release(m2)
                m2 = self.mark()
                sq2 = [self.sb(f"sq2_{i}", [128, 512], BF16) for i in range(2)]
                rstd2 = self.sb("rstd2", [128, HALF], F32)
                xb = mixed
                for tt in range(2):
                    bk = nb()
                    for kc in range(16):
                        sq = sq2[kc % 2]
                        self.ACT(sq[:], xacc[:, kc, tt * 512:(tt + 1) * 512], AF.Square, [xar[kc][tt]], [sq])
                        self.MM(bk, bk[:], ones[:, 0, :], sq[:], kc == 0, kc == 15, [ones, sq])
                    self.rstd_from(rstd2, rstd2[:, tt * 512:(tt + 1) * 512], bk, bk[:])
                xbr = self.regions(xb, [f"xbr{kc}" for kc in range(16)])
                for kc in range(16):
                    self.STT("vector", xb[:, kc, :], xacc[:, kc, :],
                             gn[:, G_MLP + l * 16 + kc:G_MLP + l * 16 + kc + 1], rstd2[:], ALU.mult, ALU.mult,
                             [xar[kc][0], xar[kc][1], gn, rstd2], [xbr[kc]])
                aT = [self.sb(f"aT{i}", [128, 4, HALF], BF16) for i in range(2)]
                aTr = [[self.regions(aT[i], [f"aT{i}_{j}_0", f"aT{i}_{j}_1"]) for j in range(4)] for i in range(2)]
                rl = [self.sb(f"rl{i}", [128, 512], BF16) for i in range(2)]
                rli = [0]

                upring = [Tl(self.wsl[i].ap, f"upr{i}") for i in range(2)]
                dnring = [Tl(self.wsl[2 + i // 2].ap[:, (i % 2) * 1024:(i % 2 + 1) * 1024], f"dnr{i}") for i in range(8)]
                up_h = {}
                upi = [0]

                def load_up(g_, j_):
                    if (g_, j_) in up_h or g_ >= 16:
                        return
                    sl = upring[upi[0] % 2]
                    upi[0] += 1
                    up_h[(g_, j_)] = self.wunit_to(w_up, wu(w_up, g_ * 4 + j_), 2048, 16, sl, sl.ap)

                dn_h = {}

                def load_dn(g_, ch_, j_):
                    sl = dnring[ch_ * 4 + j_]
                    dn_h[(g_, ch_, j_)] = self.wunit_to(w_down, wu(w_down, g_ * 4 + j_)[:, ch_ * 1024:(ch_ + 1) * 1024], 1024, 8, sl, sl.ap)

                def consume_up(grp):
                    a_ = aT[grp % 2]
                    for k in range(4):
                        load_up(grp, k)
                        if k < 3:
                            load_up(grp, k + 1)
                        if grp >= 1:
                            load_dn(grp - 1, 0, k)
                        sl, wv = up_h.pop((grp, k))
                        for tt in range(2):
                            bk = nb()
                            for kc in range(16):
                                self.MM(bk, bk[:], wv[:, kc, :], xb[:, kc, tt * 512:(tt + 1) * 512], kc == 0, kc == 15, [sl, xbr[kc]])
                            r1 = rl[rli[0] % 2]
                            rli[0] += 1
                            self.ACT(r1[:], bk[:], AF.Relu, [bk], [r1])
                            self.ACT(a_[:, k, tt * 512:(tt + 1) * 512], r1[:], AF.Square, [r1], [aTr[grp % 2][k][tt]])

                def consume_down(grp):
                    a_ = aT[grp % 2]
                    for j in range(4):
                        load_dn(grp, 1, j)
                    for ch in range(2):
                        if ch == 1:
                            load_up(grp + 2, 0)
                            load_up(grp + 2, 1)
                        got = [dn_h.pop((grp, ch, j)) for j in range(4)]
                        for o8 in range(8):
                            oc = ch * 8 + o8
                            for tt in range(2):
                                bk = nb()
                                for j in range(4):
                                    self.MM(bk, bk[:], got[j][1][:, o8, :], a_[:, j, tt * 512:(tt + 1) * 512], j == 0, j == 3,
                                            [got[j][0], aTr[grp % 2][j][tt]])
                                xa = xacc[:, oc, tt * 512:(tt + 1) * 512]
                                self.TT("vector", xa, xa, bk[:], ALU.add, [xar[oc][tt], bk], [xar[oc][tt]])

                consume_up(0)
                for grp in range(16):
                    if grp + 1 < 16:
                        consume_up(grp + 1)
                    else:
                        for j in range(4):
                            load_dn(grp, 0, j)
                    consume_down(grp)
                if l < L - 1:
                    for kc in range(16):
                        self.DMA(xres[kc][:, tsl], xacc[:, kc, :], xar[kc], [xres])
                else:
                    for tt in range(2):
                        bk = nb()
                        for kc in range(16):
                            sq = sq2[kc % 2]
                            self.ACT(sq[:], xacc[:, kc, tt * 512:(tt + 1) * 512], AF.Square, [xar[kc][tt]], [sq])
                            self.MM(bk, bk[:], ones[:, 0, :], sq[:], kc == 0, kc == 15, [ones, sq])
                        self.rstd_from(rstd2, rstd2[:, tt * 512:(tt + 1) * 512], bk, bk[:])
                    for kc in range(16):
                        self.STT("vector", xacc[:, kc, :], xacc[:, kc, :],
                                 gn[:, G_FIN + kc:G_FIN + kc + 1], rstd2[:], ALU.mult, ALU.mult, xar[kc] + [gn, rstd2], xar[kc])
                        self.DMA(outT[kc][:, tsl], xacc[:, kc, :], xar[kc], [outT])
                self.release(m2)
                self.release(m)
            self.release(lmark)
        P.emit(final_wait_bufs=[outT.b])


def _consts(g):
    c = np.zeros((128, 2048), np.float32)
    c[:, 0:128] = np.eye(128, dtype=np.float32)
    k = np.arange(128)[:, None]
    q = np.arange(128)[None, :]
    c[:, 128:256] = (q >= k).astype(np.float32)
    hh = np.arange(8, dtype=np.float64)
    gamma = 1.0 - np.exp2(-5.0 - hh)
    t = np.arange(128, dtype=np.float64)[:, None]
    c[:, 256:264] = (gamma[None, :] ** (t + 1.0)) * (128.0 ** -0.5)
    c[:, 264:272] = gamma[None, :] ** (-(t + 1.0))
    c[:, 272:1296] = np.repeat((gamma ** 128.0)[None, :], 128, axis=1).reshape(1, 1024).repeat(128, 0) if False else \
        np.broadcast_to(np.repeat(gamma ** 128.0, 128)[None, :], (128, 1024))
    coef = np.zeros((4, 8), np.float64)
    for i in range(4):
        if i < g:
            coef[i] = gamma ** (2048.0 * (g - 1 - i))
    c[:, 1296:1328] = np.broadcast_to(coef.reshape(1, 32), (128, 32))
    invf64 = (np.float32(10000.0) ** (-np.arange(0, 64, 2, dtype=np.float32) / np.float32(64))).astype(np.float32)
    invf128 = (np.float32(10000.0) ** (-np.arange(0, 128, 2, dtype=np.float32) / np.float32(128))).astype(np.float32)
    c[:, 1328:1360] = invf64[None, :]
    c[:, 1360:1424] = invf128[None, :]
    return c


def _gains(attn_norm, mlp_norm, q_norm, kv_norm, beta_attn, beta_ret, final_norm):
    gcols = np.zeros((128, 256), np.float32)
    for l in range(NL):
        gcols[:, 0 + l * 16:0 + (l + 1) * 16] = attn_norm[l].reshape(16, 128).T
        gcols[:, 64 + l * 16:64 + (l + 1) * 16] = mlp_norm[l].reshape(16, 128).T
        gcols[:, 128 + l * 4:128 + (l + 1) * 4] = q_norm[l].reshape(4, 128).T
        gcols[:, 144 + l * 2:144 + (l + 1) * 2] = kv_norm[l].reshape(2, 128).T
        gcols[:, 152 + l * 8:152 + (l + 1) * 8] = beta_attn[l].reshape(8, 128).T
        gcols[:, 184 + l * 8:184 + (l + 1) * 8] = beta_ret[l].reshape(8, 128).T
    gcols[:, 216:232] = final_norm.reshape(16, 128).T
    return gcols


_NC_CACHE = {}


def kernel(x, positions, attn_norm, w_in, q_norm, kv_norm, w_uq, w_ukv, beta_attn, beta_ret,
           w_o, mlp_norm, w_up, w_down, final_norm, _n_layers=NL, _stage=99):
    x = np.asarray(x, np.float32)
    positions = np.asarray(positions, np.int32)
    w_in = np.ascontiguousarray(np.asarray(w_in, np.float32))
    w_uq = np.asarray(w_uq, np.float32)
    w_ukv = np.asarray(w_ukv, np.float32)
    w_o = np.ascontiguousarray(np.asarray(w_o, np.float32))
    w_up = np.ascontiguousarray(np.asarray(w_up, np.float32))
    w_down = np.ascontiguousarray(np.asarray(w_down, np.float32))
    gains = _gains(*[np.asarray(a, np.float32) for a in (attn_norm, mlp_norm, q_norm, kv_norm, beta_attn, beta_ret, final_norm)])
    if (_n_layers, _stage) not in _NC_CACHE:
        _NC_CACHE[(_n_layers, _stage)] = Builder(_n_layers, _stage).build()
    nc = _NC_CACHE[(_n_layers, _stage)]
    def fm_units(w, ncolblk):
        nl, kdim, _ = w.shape
        kc_n = kdim // 128
        t = w[:, :, :ncolblk * 128].reshape(nl, kc_n, 128, ncolblk, 128).transpose(0, 3, 2, 1, 4)
        return t.reshape(nl, ncolblk, 128, kc_n * 128)
    w_in_t = np.zeros((NL, 40, 128, 2048), np.float32)
    w_in_t[:, 0:6] = fm_units(w_in[:, :, 0:768], 6)
    w_in_t[:, 6, :, 0:1024] = w_in[:, :, 768:832].reshape(NL, 16, 128, 64).transpose(0, 2, 1, 3).reshape(NL, 128, 1024)
    for gi in range(8):
        c0 = 832 + gi * 512
        blk = w_in[:, :, c0:c0 + 512].reshape(NL, 4, 4, 128, 512).transpose(0, 1, 3, 2, 4)
        w_in_t[:, 7 + gi * 4:7 + gi * 4 + 4] = blk.reshape(NL, 4, 128, 2048)
    w_in_t = w_in_t.reshape(NL, 40 * 128, 2048)
    w_o_t = np.ascontiguousarray(fm_units(w_o, 16)).reshape(NL, 16 * 128, 2048)
    w_up_t = np.ascontiguousarray(fm_units(w_up, 64)).reshape(NL, 64 * 128, 2048)
    in_maps = []
    for c in range(8):
        b, g = c // 4, c % 4
        xs = x[b, g * TOK:(g + 1) * TOK, :]
        xT = np.ascontiguousarray(xs.T).reshape(16, 128, TOK)
        po = np.ascontiguousarray(positions[b, g * TOK:(g + 1) * TOK].reshape(16, 128).T)
        pa = np.ascontiguousarray(positions[b].reshape(64, 128).T)
        h0, h1 = 2 * g, 2 * g + 1
        wuq_my = np.ascontiguousarray(np.concatenate(
            [w_uq[:, :, h0 * 192:h0 * 192 + 128], w_uq[:, :, h1 * 192:h1 * 192 + 128],
             w_uq[:, :, h0 * 192 + 128:(h0 + 1) * 192], w_uq[:, :, h1 * 192 + 128:(h1 + 1) * 192]], axis=2))
        wukv_my = np.ascontiguousarray(np.concatenate(
            [w_ukv[:, :, h0 * 256:h0 * 256 + 128], w_ukv[:, :, h1 * 256:h1 * 256 + 128],
             w_ukv[:, :, h0 * 256 + 128:(h0 + 1) * 256], w_ukv[:, :, h1 * 256 + 128:(h1 + 1) * 256]], axis=2))
        in_maps.append({
            "xT": xT, "pos_own": po, "pos_all": pa, "w_uq_my": wuq_my, "w_ukv_my": wukv_my,
            "w_in_t": w_in_t, "w_o_t": w_o_t, "w_up_t": w_up_t, "w_down_t": w_down,
            "gains": gains, "cfs": _consts(g),
        })
    res = run_bass_kernel_spmd(nc, in_maps, core_ids=list(range(8)))
    out = np.empty((2, S, D), np.float32)
    for c in range(8):
        b, g = c // 4, c % 4
        oT = np.asarray(res.results[c]["outT"]).reshape(D, TOK)
        out[b, g * TOK:(g + 1) * TOK, :] = oT.T
    return out
```

```python
import contextlib
import numpy as np
import concourse.bass as bass
import concourse.mybir as mybir
from concourse.bass_utils import run_bass_kernel_spmd

F32 = mybir.dt.float32
BF16 = mybir.dt.bfloat16
I32 = mybir.dt.int32
AF = mybir.ActivationFunctionType
ALU = mybir.AluOpType
AX = mybir.AxisListType

D = 2048
S = 8192
NL = 4
TOK = 2048
HALF = 1024
INW = 4928
DFF = 8192
GROUPS = [[0, 1, 2, 3], [4, 5, 6, 7]]
ATT_SCALE = 192.0 ** -0.5
EPS = 1e-6
MAGIC = 12582912.0
TWO_PI = 2.0 * np.pi
CW1 = 6.28125
CW2 = TWO_PI - 6.28125

ENGS = ("tensor", "vector", "scalar", "gpsimd", "sync")
SEM_LIMIT = 8000


class Buf:
    __slots__ = ("name", "last_w", "reads")

    def __init__(self, name):
        self.name = name
        self.last_w = None
        self.reads = []


class Op:
    __slots__ = ("eng", "fn", "deps", "kind", "sig", "has_dep", "dbuf")

    def __init__(self, eng, fn, kind):
        self.eng = eng
        self.fn = fn
        self.kind = kind
        self.deps = set()
        self.sig = None
        self.has_dep = False
        self.dbuf = None


class Prog:
    def __init__(self, nc):
        self.nc = nc
        self.ops = []
        self.by_eng = {e: [] for e in ENGS}
        self.last_of = {e: None for e in ENGS}
        self.pending = {e: set() for e in ENGS}
        self.dma_since_barrier = []

    def _add(self, eng, fn, reads, writes, kind):
        op = Op(eng, fn, kind)
        for b in reads:
            if b.last_w is not None:
                op.deps.add(b.last_w)
        for b in writes:
            if b.last_w is not None:
                op.deps.add(b.last_w)
            for r in b.reads:
                op.deps.add(r)
        for b in reads:
            b.reads.append(op)
        for b in writes:
            b.last_w = op
            b.reads = []
        op.deps.discard(op)
        if self.pending[eng]:
            op.deps |= self.pending[eng]
            self.pending[eng] = set()
        if eng == "tensor":
            op.deps = {d for d in op.deps if not (d.eng == "tensor" and d.kind == "c")}
        for d in op.deps:
            d.has_dep = True
        self.ops.append(op)
        self.by_eng[eng].append(op)
        if kind == "c":
            self.last_of[eng] = op
        else:
            self.dma_since_barrier.append(op)
        return op

    def c(self, eng, fn, reads=(), writes=()):
        return self._add(eng, fn, list(reads), list(writes), "c")

    def dma(self, eng, fn, reads=(), writes=()):
        op = self._add(eng, fn, list(reads), list(writes), "d")
        op.dbuf = writes[0]
        op.has_dep = True
        return op

    def cc(self, fn, reads=(), writes=()):
        op = self._add("gpsimd", fn, list(reads), list(writes), "cc")
        op.dbuf = writes[0]
        op.has_dep = True
        return op

    def barrier(self):
        deps = set(o for o in self.last_of.values() if o is not None) | set(self.dma_since_barrier)
        self.dma_since_barrier = []
        for e in ENGS:
            self.pending[e] |= deps

    def emit(self, final_wait_bufs=()):
        nc = self.nc
        eng_state = {e: [None, 0] for e in ENGS}
        sem_names = []

        def new_sem(tag):
            sem_names.append(tag)
            return len(sem_names) - 1

        dsem = {}
        for op in self.ops:
            if op.kind == "c":
                if op.has_dep:
                    st = eng_state[op.eng]
                    if st[0] is None or st[1] >= SEM_LIMIT:
                        st[0] = new_sem("e_" + op.eng)
                        st[1] = 0
                    st[1] += 1
                    op.sig = (st[0], st[1], 1)
            else:
                inc = 16 if op.kind == "d" else 1
                k = op.dbuf.name
                st = dsem.get(k)
                if st is None or st[1] >= SEM_LIMIT * 2:
                    st = [new_sem("d_" + op.dbuf.name), 0]
                    dsem[k] = st
                st[1] += inc
                op.sig = (st[0], st[1], inc)
        final = []
        for b in final_wait_bufs:
            st = dsem[b.name]
            final.append((st[0], st[1]))
        self.n_sems = len(sem_names)
        with contextlib.ExitStack() as es:
            handles = [es.enter_context(nc.semaphore(f"s{i}_{n}"[:40])) for i, n in enumerate(sem_names)]
            block = es.enter_context(nc.Block())

            def run(engname, eng):
                known = {}
                if engname == "sync":
                    self.pid = eng.partition_id()
                for op in self.by_eng[engname]:
                    need = {}
                    for d in op.deps:
                        s, v, _ = d.sig
                        if known.get(s, 0) >= v:
                            continue
                        if need.get(s, 0) < v:
                            need[s] = v
                    for s, v in need.items():
                        eng.wait_ge(handles[s], v)
                        known[s] = v
                    ins = op.fn(eng)
                    if op.sig is not None:
                        ins.then_inc(handles[op.sig[0]], op.sig[2])
                if engname == "sync":
                    for s, v in final:
                        eng.wait_ge(handles[s], v)

            @block.tensor
            def _(e):
                run("tensor", e)

            @block.vector
            def _(e):
                run("vector", e)

            @block.scalar
            def _(e):
                run("scalar", e)

            @block.gpsimd
            def _(e):
                run("gpsimd", e)

            @block.sync
            def _(e):
                run("sync", e)


class Tl:
    def __init__(self, ap, name):
        self.ap = ap
        self.b = Buf(name)

    def __getitem__(self, k):
        return self.ap[k]


DT_SIZE = {F32: 4, BF16: 2, I32: 4}


class Builder:
    def __init__(self, n_layers=NL, stage=99):
        self.n_layers = n_layers
        self.stage = stage
        nc = bass.Bass("TRN2", target_bir_lowering=False)
        self.nc = nc
        self.P = Prog(nc)
        self.es = contextlib.ExitStack()

    def dram_in(self, name, shape, dt):
        return Tl(self.nc.dram_tensor(name, list(shape), dt, kind="ExternalInput").ap(), name)

    def dram_out(self, name, shape, dt):
        return Tl(self.nc.dram_tensor(name, list(shape), dt, kind="ExternalOutput").ap(), name)

    def dram_tmp(self, name, shape, dt):
        return Tl(self.nc.dram_tensor(name, list(shape), dt, kind="Internal").ap(), name)

    def sb(self, name, shape, dt):
        n = 1
        for s_ in shape[1:]:
            n *= s_
        nbytes = n * DT_SIZE[dt]
        nbytes = (nbytes + 63) // 64 * 64
        off = self.aoff
        self.aoff += nbytes
        assert self.aoff <= self.asize, (name, self.aoff, self.asize)
        self.apeak = max(self.apeak, self.aoff)
        w = self.arena[0:shape[0], off // 4:(off + nbytes) // 4]
        if dt != F32:
            w = w.bitcast(dt)
        w = w[:, 0:n]
        if len(shape) == 3:
            w = w.rearrange("p (a b) -> p a b", a=shape[1])
        elif len(shape) == 4:
            w = w.rearrange("p (a b c) -> p a b c", a=shape[1], b=shape[2])
        return Tl(w, name)

    def mark(self):
        return self.aoff

    def regions(self, tl, names):
        return [Tl(tl.ap, n) for n in names]

    def release(self, mark):
        self.P.barrier()
        self.aoff = mark

    def MM(self, ps, out_ap, lhsT, rhs, start, stop, reads):
        self.P.c("tensor", lambda e: e.matmul(out_ap, lhsT=lhsT, rhs=rhs, start=start, stop=stop),
                 [t.b for t in reads], [ps.b])

    def TR(self, ps, out_ap, in_ap, reads):
        ident = self.ident
        self.P.c("tensor", lambda e: e.transpose(out_ap, in_ap, ident[:]),
                 [t.b for t in reads] + [ident.b], [ps.b])

    def ACT(self, out_ap, in_ap, func, reads, writes, bias=None, scale=1.0):
        if bias is None:
            fn = lambda e: e.activation(out=out_ap, in_=in_ap, func=func, scale=scale)
        else:
            fn = lambda e: e.activation(out=out_ap, in_=in_ap, func=func, bias=bias, scale=scale)
        self.P.c("scalar", fn, [t.b for t in reads], [t.b for t in writes])

    def TT(self, eng, out_ap, in0, in1, op, reads, writes):
        self.P.c(eng, lambda e: e.tensor_tensor(out=out_ap, in0=in0, in1=in1, op=op),
                 [t.b for t in reads], [t.b for t in writes])

    def TS(self, eng, out_ap, in0, s1, op0, reads, writes, s2=None, op1=None):
        if op1 is None:
            fn = lambda e: e.tensor_scalar(out=out_ap, in0=in0, scalar1=s1, scalar2=None, op0=op0)
        else:
            fn = lambda e: e.tensor_scalar(out=out_ap, in0=in0, scalar1=s1, scalar2=s2, op0=op0, op1=op1)
        self.P.c(eng, fn, [t.b for t in reads], [t.b for t in writes])

    def STT(self, eng, out_ap, in0, scalar, in1, op0, op1, reads, writes):
        self.P.c(eng, lambda e: e.scalar_tensor_tensor(out=out_ap, in0=in0, scalar=scalar, in1=in1, op0=op0, op1=op1),
                 [t.b for t in reads], [t.b for t in writes])

    def CP(self, eng, out_ap, in_ap, reads, writes):
        if eng == "scalar":
            self.ACT(out_ap, in_ap, AF.Copy, reads, writes)
        else:
            self.P.c(eng, lambda e: e.tensor_copy(out=out_ap, in_=in_ap), [t.b for t in reads], [t.b for t in writes])

    def RED(self, eng, out_ap, in_ap, reads, writes):
        self.P.c(eng, lambda e: e.tensor_reduce(out=out_ap, in_=in_ap, axis=AX.X, op=ALU.add),
                 [t.b for t in reads], [t.b for t in writes])

    def RCP(self, out_ap, in_ap, reads, writes):
        self.P.c("vector", lambda e: e.reciprocal(out=out_ap, in_=in_ap), [t.b for t in reads], [t.b for t in writes])

    def MEMSET(self, eng, ap, val, writes):
        self.P.c(eng, lambda e: e.memset(ap, val), [], [t.b for t in writes])

    def DMA(self, out_ap, in_ap, reads, writes, eng="sync"):
        self.P.dma(eng, lambda e: e.dma_start(out=out_ap, in_=in_ap), [t.b for t in reads], [t.b for t in writes])

    def rstd_from(self, out_tl, out_ap, ps, ps_ap):
        self.ACT(out_ap, ps_ap, AF.Sqrt, [ps, self.cst], [out_tl], bias=self.cst[:, 0:1])
        self.RCP(out_ap, out_ap, [out_tl], [out_tl])

    def wunit(self, wt, src_ap, a, b_):
        st = self.stg[self.stg_i % len(self.stg)]
        self.stg_i += 1
        sl = self.wsl[self.wsl_i % len(self.wsl)]
        self.wsl_i += 1
        sv = st[:, 0:a * b_].rearrange("p (a b) -> p a b", a=a)
        wv = sl[:, 0:a * b_].rearrange("p (a b) -> p a b", a=a)
        self.DMA(st[:, 0:a * b_], src_ap[:, 0:a * b_], [wt], [st])
        self.cast_i += 1
        self.CP(("scalar", "vector", "scalar", "gpsimd")[self.cast_i % 4], wv, sv, [st], [sl])
        return sl, wv

    def wunit_to(self, wt, src_ap, nelem, a, sl, sl_ap):
        st = self.stg[self.stg_i % len(self.stg)]
        self.stg_i += 1
        self.DMA(st[:, 0:nelem], src_ap, [wt], [st])
        self.cast_i += 1
        self.CP(("scalar", "vector", "scalar", "gpsimd")[self.cast_i % 4], sl_ap, st[:, 0:nelem], [st], [sl])
        return sl, sl_ap.rearrange("p (a b) -> p a b", a=a)

    def stream(self, units, consume, depth=3):
        n = len(units)
        got = []
        for k in range(min(depth, n)):
            got.append(self.wunit(*units[k]))
        for k in range(n):
            if k + depth < n:
                got.append(self.wunit(*units[k + depth]))
            consume(k, got[k][0], got[k][1])

    def build(self):
        nc = self.nc
        P = self.P
        L = self.n_layers
        es = self.es
        with es:
            self._build(nc, P, L, es)
        return nc

    def _build(self, nc, P, L, es):
        xT = self.dram_in("xT", [16, 128, TOK], F32)
        pos_own = self.dram_in("pos_own", [128, 16], I32)
        pos_all = self.dram_in("pos_all", [128, 64], I32)
        w_uq = self.dram_in("w_uq_my", [NL, 512, 384], F32)
        w_ukv = self.dram_in("w_ukv_my", [NL, 256, 512], F32)
        wspec = {"w_in": 40, "w_o": 16, "w_up": 64, "w_down": 64}
        wfl = {}
        for nm, nu in wspec.items():
            t_ = self.nc.dram_tensor(nm + "_t", [NL, nu * 128, 2048], F32, kind="ExternalInput").ap()
            wfl[nm] = [Tl(t_[i], f"{nm}_t{i}") for i in range(NL)]

        def bounce_weights(l_):
            pass

        def gather_weights(l_, names):
            pass
        gains = self.dram_in("gains", [128, 256], F32)
        cfs = self.dram_in("cfs", [128, 2048], F32)
        outT = self.dram_out("outT", [16, 128, TOK], F32)
        xres = self.dram_tmp("xres", [16, 128, TOK], F32)
        sc_q = self.dram_tmp("sc_q", [16, 128, 1024], BF16)
        sc_k = self.dram_tmp("sc_k", [16, 128, 1024], BF16)
        sc_v = self.dram_tmp("sc_v", [16, 128, 1024], BF16)
        sc_g = self.dram_tmp("sc_g", [16, 128, 1024], BF16)
        b_lat = [self.dram_tmp(f"b_lat{i}", [7 * 128, 512], BF16) for i in range(4)]
        g_lat = [self.dram_tmp(f"g_lat{i}", [4 * 7 * 128, 512], BF16) for i in range(4)]
        b_st = self.dram_tmp("b_st", [128, 1024], F32)
        g_st = self.dram_tmp("g_st", [512, 1024], F32)
        b_att = [self.dram_tmp(f"b_att{i}", [256, 1024], BF16) for i in range(8)]
        g_att_all = self.nc.dram_tensor("g_att", [8, 4 * 256, 1024], BF16, kind="Internal").ap()
        g_att = [Tl(g_att_all[i], f"g_att{i}") for i in range(8)]

        self.asize = 207 * 1024
        self.arena = es.enter_context(nc.sbuf_tensor("arena", [128, self.asize // 4], F32))
        self.aoff = 0
        self.apeak = 0
        banks = [Tl(es.enter_context(nc.psum_tensor(f"bank{i}", [128, 512], F32)), f"bank{i}") for i in range(8)]
        self.banks = banks

        gn = self.sb("gains", [128, 256], F32)
        cst = self.sb("cst", [128, 16], F32)
        self.cst = cst
        ident = self.sb("ident", [128, 128], BF16)
        self.ident = ident
        ones = self.sb("ones", [128, 4, 128], BF16)
        one1 = self.sb("one1", [128, 128], BF16)
        mask = self.sb("mask", [128, 128], BF16)
        wq = self.sb("wq", [128, 8], F32)
        wk = self.sb("wk", [128, 8], F32)
        gtab = self.sb("gtab", [128, 8, 128], F32)
        coef = self.sb("coef", [128, 4, 8], F32)
        cosR = self.sb("cosR", [128, 16, 64], F32)
        sinR = self.sb("sinR", [128, 16, 64], F32)
        cosK = self.sb("cosK", [128, 16, 32], F32)
        sinK = self.sb("sinK", [128, 16, 32], F32)
        cosQ = self.sb("cosQ", [128, 64, 32], BF16)
        sinQ = self.sb("sinQ", [128, 64, 32], BF16)
        Sst = self.sb("Sst", [128, 8, 128], F32)
        Sbf = self.sb("Sbf", [128, 8, 128], BF16)
        self.stg = [self.sb(f"stg{i}", [128, 2048], F32) for i in range(2)]
        self.wsl = [self.sb(f"wsl{i}", [128, 2048], BF16) for i in range(6)]
        self.stg_i = 0
        self.wsl_i = 0
        self.cast_i = 0
        base_mark = self.mark()

        self.DMA(gn[:], gains[:], [gains], [gn])
        G_ATT, G_MLP, G_QN, G_KVN, G_BA, G_BR, G_FIN = 0, 64, 128, 144, 152, 184, 216

        ctmp = self.sb("ctmp", [128, 2048], F32)
        self.DMA(ctmp[:], cfs[:], [cfs], [ctmp])
        self.CP("vector", ident[:], ctmp[:, 0:128], [ctmp], [ident])
        self.CP("vector", mask[:], ctmp[:, 128:256], [ctmp], [mask])
        self.CP("vector", wq[:], ctmp[:, 256:264], [ctmp], [wq])
        self.CP("vector", wk[:], ctmp[:, 264:272], [ctmp], [wk])
        self.CP("vector", gtab[:], ctmp[:, 272:1296].rearrange("p (a b) -> p a b", a=8), [ctmp], [gtab])
        self.CP("vector", coef[:], ctmp[:, 1296:1328].rearrange("p (a b) -> p a b", a=4), [ctmp], [coef])
        self.MEMSET("gpsimd", cst[:, 0:1], EPS, [cst])
        self.MEMSET("gpsimd", cst[:, 1:2], 0.0, [cst])
        for i, v in enumerate([1.0 / 2048, 1.0 / 512, 1.0 / 256, 1.0 / 1024]):
            self.MEMSET("gpsimd", ones[:, i, :], v, [ones])
        self.MEMSET("gpsimd", one1[:], 1.0, [one1])
        self.MEMSET("gpsimd", Sst[:], 0.0, [Sst])

        def rope_table(pos_dram, nt, invf_ap, nf, cos_t, sin_t):
            m = self.mark()
            pi_ = self.sb("pos_i", [128, nt], I32)
            pf = self.sb("pos_f", [128, nt], F32)
            ang = self.sb("ang", [128, nt, nf], F32)
            u = self.sb("u", [128, nt, nf], F32)
            r = self.sb("r", [128, nt, nf], F32)
            self.DMA(pi_[:], pos_dram[:], [pos_dram], [pi_])
            self.CP("vector", pf[:], pi_[:], [pi_], [pf])
            self.TT("vector", ang[:], invf_ap.unsqueeze(1).to_broadcast([128, nt, nf]),
                    pf[:].unsqueeze(2).to_broadcast([128, nt, nf]), ALU.mult, [ctmp, pf], [ang])
            for which, dst in ((0, sin_t), (1, cos_t)):
                if which == 1:
                    self.TS("vector", ang[:], ang[:], float(np.pi / 2), ALU.add, [ang], [ang])
                self.TS("vector", u[:], ang[:], float(1.0 / TWO_PI), ALU.mult, [ang], [u])
                self.TS("vector", u[:], u[:], MAGIC, ALU.add, [u], [u])
                self.TS("vector", u[:], u[:], MAGIC, ALU.subtract, [u], [u])
                self.STT("vector", r[:], u[:], -CW1, ang[:], ALU.mult, ALU.add, [u, ang], [r])
                self.STT("vector", r[:], u[:], -CW2, r[:], ALU.mult, ALU.add, [u, r], [r])
                self.TS("vector", r[:], r[:], -3.1415925, ALU.max, [r], [r], s2=3.1415925, op1=ALU.min)
                self.ACT(dst[:], r[:], AF.Sin, [r], [dst])
            self.release(m)

        rope_table(pos_own, 16, ctmp[:, 1360:1424], 64, cosR, sinR)
        rope_table(pos_own, 16, ctmp[:, 1328:1360], 32, cosK, sinK)
        rope_table(pos_all, 64, ctmp[:, 1328:1360], 32, cosQ, sinQ)

        bounce_weights(0)
        gather_weights(0, ["w_in", "w_o", "w_up", "w_down"])
        for kc in range(16):
            self.DMA(xres[kc], xT[kc], [xT], [xres])
        self.release(base_mark)
        if self.stage <= 0:
            for kc in range(16):
                self.DMA(outT[kc], xres[kc], [xres], [outT])
            P.emit(final_wait_bufs=[outT.b])
            return

        bank_i = [0]

        def nb():
            bk = banks[bank_i[0] % 8]
            bank_i[0] += 1
            return bk

        def rope_tm(src, nh, hd, cos_ap, sin_ap, out_tl, out_view, scale_ap, tmp):
            src_tl, src_ap = src
            h2 = hd // 2
            xs, t1, t2 = tmp
            xsv = xs[:, 0:nh * hd].rearrange("p (a b) -> p a b", a=nh)
            t1v = t1[:, 0:nh * h2].rearrange("p (a b) -> p a b", a=nh)
            t2v = t2[:, 0:nh * h2].rearrange("p (a b) -> p a b", a=nh)
            if scale_ap is not None:
                self.TT("vector", xsv, src_ap, scale_ap.unsqueeze(2).to_broadcast([128, nh, hd]), ALU.mult,
                        [src_tl, wq, wk], [xs])
            else:
                self.CP("scalar", xsv, src_ap, [src_tl], [xs])
            cb = cos_ap.unsqueeze(1).to_broadcast([128, nh, h2])
            sbb = sin_ap.unsqueeze(1).to_broadcast([128, nh, h2])
            tabs = [cosR, sinR, cosK, sinK, cosQ, sinQ]
            x1 = xsv[:, :, 0:h2]
            x2 = xsv[:, :, h2:hd]
            self.TT("vector", t1v, x1, cb, ALU.mult, [xs] + tabs, [t1])
            self.TT("gpsimd", t2v, x2, sbb, ALU.mult, [xs] + tabs, [t2])
            self.TT("vector", out_view[:, :, 0:h2], t1v, t2v, ALU.subtract, [t1, t2], [out_tl])
            self.TT("vector", t1v, x2, cb, ALU.mult, [xs] + tabs, [t1])
            self.TT("gpsimd", t2v, x1, sbb, ALU.mult, [xs] + tabs, [t2])
            self.TT("vector", out_view[:, :, h2:hd], t1v, t2v, ALU.add, [t1, t2], [out_tl])

        for l in range(L):
            lmark = self.mark()
            w_in, w_o, w_up, w_down = wfl["w_in"][l], wfl["w_o"][l], wfl["w_up"][l], wfl["w_down"][l]
            if l + 1 < L:
                bounce_weights(l + 1)
            def wu(wt_, u_):
                return wt_.ap[u_ * 128:(u_ + 1) * 128, :]
            self.MEMSET("gpsimd", Sst[:], 0.0, [Sst])
            for h in range(2):
                m = self.mark()
                tsl = slice(h * HALF, (h + 1) * HALF)
                xb = self.sb("xb", [128, 16, HALF], BF16)
                rstd = self.sb("rstd", [128, HALF], F32)
                xring = [self.sb(f"xr{i}", [128, HALF], F32) for i in range(3)]
                sqr = [self.sb(f"sq{i}", [128, HALF], BF16) for i in range(2)]
                bA, bB = banks[0], banks[1]
                for kc in range(16):
                    xr = xring[kc % 3]
                    sq = sqr[kc % 2]
                    self.DMA(xr[:], xres[kc][:, tsl], [xres], [xr])
                    self.ACT(sq[:], xr[:], AF.Square, [xr], [sq])
                    for tt, bk in ((0, bA), (1, bB)):
                        self.MM(bk, bk[:], ones[:, 0, :], sq[:, tt * 512:(tt + 1) * 512], kc == 0, kc == 15, [ones, sq])
                for tt, bk in ((0, bA), (1, bB)):
                    self.rstd_from(rstd, rstd[:, tt * 512:(tt + 1) * 512], bk, bk[:])
                for kc in range(16):
                    xr = xring[(kc + 1) % 3]
                    self.DMA(xr[:], xres[kc][:, tsl], [xres], [xr])
                    self.STT("vector", xb[:, kc, :], xr[:],
                             gn[:, G_ATT + l * 16 + kc:G_ATT + l * 16 + kc + 1], rstd[:], ALU.mult, ALU.mult,
                             [xr, gn, rstd], [xb])
                lat = self.sb("lat", [128, 4, HALF], F32)
                lsq = self.sb("lsq", [128, HALF], BF16)
                lrs = self.sb("lrs", [128, HALF], F32)
                bnc = self.sb("bnc", [128, 7, HALF], BF16)
                for (c0, nch, onei, gcol, boff) in ((0, 4, 1, G_QN + l * 4, 0), (512, 2, 2, G_KVN + l * 2, 4)):
                    units = [(w_in, wu(w_in, boff + j), 16, 128) for j in range(nch)]

                    def cons(k, sl, wv, nch=nch):
                        for tt in range(2):
                            bk = nb()
                            for kc in range(16):
                                self.MM(bk, bk[:], wv[:, kc, :], xb[:, kc, tt * 512:(tt + 1) * 512], kc == 0, kc == 15, [sl, xb])
                            self.CP("scalar", lat[:, k, tt * 512:(tt + 1) * 512], bk[:], [bk], [lat])
                    self.stream(units, cons)
                    for tt in range(2):
                        bk = nb()
                        for j in range(nch):
                            self.ACT(lsq[:, tt * 512:(tt + 1) * 512], lat[:, j, tt * 512:(tt + 1) * 512], AF.Square, [lat], [lsq])
                            self.MM(bk, bk[:], ones[:, onei, :], lsq[:, tt * 512:(tt + 1) * 512], j == 0, j == nch - 1, [ones, lsq])
                        self.rstd_from(lrs, lrs[:, tt * 512:(tt + 1) * 512], bk, bk[:])
                    for j in range(nch):
                        self.STT("vector", bnc[:, boff + j, :], lat[:, j, :], gn[:, gcol + j:gcol + j + 1], lrs[:],
                                 ALU.mult, ALU.mult, [lat, gn, lrs], [bnc])
                rtmp = [(self.sb(f"xs_t{i}", [128, 512], F32), self.sb(f"t1_t{i}", [128, 256], F32), self.sb(f"t2_t{i}", [128, 256], F32))
                        for i in range(2)]
                rti = [0]

                def rt():
                    rti[0] += 1
                    return rtmp[rti[0] % 2]
                ktm = self.sb("ktm", [128, 8, 1024], BF16)
                orow = [self.sb(f"orow{i}", [128, 512], BF16) for i in range(2)]
                krt = self.sb("krt", [128, 128], BF16)
                oi = [0]
                gotk = self.wunit(w_in, wu(w_in, 6), 16, 64)
                for t in range(8):
                    bk = nb()
                    for kc in range(16):
                        self.MM(bk, bk[:, 0:64], xb[:, kc, t * 128:(t + 1) * 128], gotk[1][:, kc, :], kc == 0, kc == 15,
                                [xb, gotk[0]])
                    gt = h * 8 + t
                    rope_tm((bk, bk[:, 0:64].rearrange("p (a b) -> p a b", a=1)), 1, 64, cosK[:, gt, :], sinK[:, gt, :],
                            krt, krt[:, 0:64].rearrange("p (a b) -> p a b", a=1), None, rt())
                    self.CP("gpsimd", krt[:, 64:128], krt[:, 0:64], [krt], [krt])
                    bk2 = nb()
                    bv = bk2[:, 0:64].bitcast(BF16)
                    self.TR(bk2, bv, krt[:], [krt])
                    self.CP("scalar", bnc[:, 6, t * 128:(t + 1) * 128], bv, [bk2], [bnc])
                for tt in range(2):
                    gtt = h * 2 + tt
                    for c_ in range(7):
                        self.DMA(b_lat[gtt][c_ * 128:(c_ + 1) * 128, :],
                                 bnc[:, c_, tt * 512:(tt + 1) * 512], [bnc], [b_lat[gtt]])
                for gi in range(8):
                    kind = gi // 2
                    hg = gi % 2
                    units = [(w_in, wu(w_in, 7 + gi * 4 + j), 4, 512) for j in range(4)]
                    got = [self.wunit(*u) for u in units]
                    for t in range(8):
                        gt = h * 8 + t
                        bk = nb()
                        for kc in range(16):
                            self.MM(bk, bk[:], xb[:, kc, t * 128:(t + 1) * 128], got[kc // 4][1][:, kc % 4, :], kc == 0, kc == 15,
                                    [xb, got[kc // 4][0]])
                        bk3 = bk[:].rearrange("p (a b) -> p a b", a=4)
                        if kind == 0:
                            o = orow[oi[0] % 2]
                            oi[0] += 1
                            rope_tm((bk, bk3), 4, 128, cosR[:, gt, :], sinR[:, gt, :], o,
                                    o[:].rearrange("p (a b) -> p a b", a=4), wq[:, hg * 4:hg * 4 + 4], rt())
                            self.DMA(sc_q[gt][:, hg * 512:(hg + 1) * 512], o[:], [o], [sc_q])
                        elif kind == 1:
                            rope_tm((bk, bk3), 4, 128, cosR[:, gt, :], sinR[:, gt, :], ktm,
                                    ktm[:, t, hg * 512:(hg + 1) * 512].rearrange("p (a b) -> p a b", a=4),
                                    wk[:, hg * 4:hg * 4 + 4], rt())
                            self.DMA(sc_k[gt][:, hg * 512:(hg + 1) * 512], ktm[:, t, hg * 512:(hg + 1) * 512], [ktm], [sc_k])
                        elif kind == 2:
                            o = orow[oi[0] % 2]
                            oi[0] += 1
                            self.CP("scalar", o[:], bk[:], [bk], [o])
                            self.DMA(sc_v[gt][:, hg * 512:(hg + 1) * 512], o[:], [o], [sc_v])
                            bm = nb()
                            for hh in range(4):
                                self.MM(bm, bm[:, hh * 128:(hh + 1) * 128], ktm[:, t, (hg * 4 + hh) * 128:(hg * 4 + hh + 1) * 128],
                                        o[:, hh * 128:(hh + 1) * 128], True, True, [ktm, o])
                            sv = Sst[:, hg * 4:hg * 4 + 4, :]
                            self.TT("vector", sv, sv, bm[:].rearrange("p (a b) -> p a b", a=4), ALU.add, [Sst, bm], [Sst])
                            self.TT("vector", sv, sv, gtab[:, hg * 4:hg * 4 + 4, :], ALU.mult, [Sst, gtab], [Sst])
                        else:
                            o = orow[oi[0] % 2]
                            oi[0] += 1
                            self.ACT(o[:], bk[:], AF.Silu, [bk], [o])
                            self.DMA(sc_g[gt][:, hg * 512:(hg + 1) * 512], o[:], [o], [sc_g])
                self.release(m)
            self.DMA(b_st[:], Sst[:].rearrange("p a b -> p (a b)"), [Sst], [b_st])
            for i_ in range(4):
                P.cc(lambda e, i_=i_: e.collective_compute("AllGather", ALU.bypass, replica_groups=GROUPS, ins=[b_lat[i_].ap], outs=[g_lat[i_].ap]),
                     [b_lat[i_].b], [g_lat[i_].b])
            P.cc(lambda e: e.collective_compute("AllGather", ALU.bypass, replica_groups=GROUPS, ins=[b_st.ap], outs=[g_st.ap]),
                 [b_st.b], [g_st.b])
            if l + 1 < L:
                gather_weights(l + 1, ["w_in", "w_o"])
            if self.stage == 1:
                for kc in range(16):
                    self.DMA(outT[kc], xres[kc], [xres], [outT])
                P.emit(final_wait_bufs=[outT.b, g_st.b] + [g_lat[i_].b for i_ in range(4)])
                return
            m = self.mark()
            wuq = self.sb("wuq", [128, 4, 384], BF16)
            wukv = self.sb("wukv", [128, 2, 512], BF16)
            wtmp = self.sb("wtmp", [128, 4, 384], F32)
            for kc in range(4):
                self.DMA(wtmp[:, kc, :], w_uq[l][kc * 128:(kc + 1) * 128, :], [w_uq], [wtmp])
            self.CP("gpsimd", wuq[:], wtmp[:], [wtmp], [wuq])
            wtmp2 = self.sb("wtmp2", [128, 2, 512], F32)
            for kc in range(2):
                self.DMA(wtmp2[:, kc, :], w_ukv[l][kc * 128:(kc + 1) * 128, :], [w_ukv], [wtmp2])
            self.CP("gpsimd", wukv[:], wtmp2[:], [wtmp2], [wukv])
            KT = self.sb("KT", [128, 2, S], BF16)
            KR = self.sb("KR", [128, S], BF16)
            VV = self.sb("VV", [128, 64, 256], BF16)
            latr = [self.sb(f"latr{i}", [128, 6, 512], BF16) for i in range(2)]
            QN = [self.sb(f"QN{i}", [128, 2, 512], BF16) for i in range(2)]
            QR = [self.sb(f"QR{i}", [128, 512], BF16) for i in range(2)]
            qrt = [self.sb(f"qrt{i}", [128, 128], BF16) for i in range(2)]
            PT = [self.sb(f"PT{i}", [128, 512], BF16) for i in range(3)]
            rden = self.sb("rden", [128, 512], F32)
            ao = [self.sb(f"ao{i}", [128, 512], BF16) for i in range(2)]
            xs_t = self.sb("xs_t", [128, 512], F32)
            t1_t = self.sb("t1_t", [128, 256], F32)
            t2_t = self.sb("t2_t", [128, 256], F32)
            pti = [0]
            aoi = [0]
            KTr = [Tl(KT.ap, f"KT{q}") for q in range(16)]
            KRr = [Tl(KR.ap, f"KR{q}") for q in range(16)]
            VVr = [Tl(VV.ap, f"VV{q}") for q in range(16)]
            g_lat_vs = [g_lat[i_].ap.rearrange("(r c p) t -> r p c t", c=7, p=128) for i_ in range(4)]
            for qt in range(16):
                lt = latr[qt % 2]
                qn = QN[qt % 2]
                qr = QR[qt % 2]
                glv = g_lat_vs[qt % 4][qt // 4]
                for c_ in range(6):
                    self.DMA(lt[:, c_, :], glv[:, c_, :], [g_lat[qt % 4]], [lt])
                self.DMA(KR[:, qt * 512:(qt + 1) * 512], glv[:, 6, :], [g_lat[qt % 4]], [KRr[qt]])
                for hh in range(2):
                    bk = nb()
                    for kc in range(2):
                        self.MM(bk, bk[:], wukv[:, kc, hh * 128:(hh + 1) * 128], lt[:, 4 + kc, :], kc == 0, kc == 1, [wukv, lt])
                    self.CP("scalar" if hh == 0 else "vector", KT[:, hh, qt * 512:(qt + 1) * 512], bk[:], [bk], [KTr[qt]])
                for j in range(4):
                    bk = nb()
                    for kc in range(2):
                        self.MM(bk, bk[:, 0:256], lt[:, 4 + kc, j * 128:(j + 1) * 128], wukv[:, kc, 256:512], kc == 0, kc == 1, [wukv, lt])
                    self.CP("vector" if j % 2 == 0 else "scalar", VV[:, qt * 4 + j, :], bk[:, 0:256], [bk], [VVr[qt]])
                for hh in range(2):
                    bk = nb()
                    for kc in range(4):
                        self.MM(bk, bk[:], wuq[:, kc, hh * 128:(hh + 1) * 128], lt[:, kc, :], kc == 0, kc == 3, [wuq, lt])
                    self.CP("scalar" if hh == 0 else "vector", qn[:, hh, :], bk[:], [bk], [qn])
                for j in range(4):
                    bk = nb()
                    for kc in range(4):
                        self.MM(bk, bk[:, 0:128], lt[:, kc, j * 128:(j + 1) * 128], wuq[:, kc, 256:384], kc == 0, kc == 3, [wuq, lt])
                    qq = qrt[j % 2]
                    rope_tm((bk, bk[:, 0:128].rearrange("p (a b) -> p a b", a=2)), 2, 64, cosQ[:, qt * 4 + j, :], sinQ[:, qt * 4 + j, :],
                            qq, qq[:].rearrange("p (a b) -> p a b", a=2), None, (xs_t, t1_t, t2_t))
                    bk2 = nb()
                    bv = bk2[:, 0:64].bitcast(BF16)
                    self.TR(bk2, bv, qq[:], [qq])
                    self.CP("scalar", qr[:, j * 128:(j + 1) * 128], bv, [bk2], [qr])
                nkt = 4 * qt + 4
                for hh in range(2):
                    OUT = banks[6] if hh == 0 else banks[4]
                    DEN = banks[7] if hh == 0 else banks[5]
                    def emit_S(kt, hh=hh):
                        r_ = kt - 4 * qt
                        c0 = 0 if r_ <= 0 else r_ * 128
                        sbk = banks[kt % 4]
                        self.MM(sbk, sbk[:, c0:512], KT[:, hh, kt * 128:(kt + 1) * 128], qn[:, hh, c0:512], True, False, [KTr[kt // 4], qn])
                        self.MM(sbk, sbk[:, c0:512], KR[hh * 64:(hh + 1) * 64, kt * 128:(kt + 1) * 128], qr[hh * 64:(hh + 1) * 64, c0:512],
                                False, True, [KRr[kt // 4], qr])
                        pt = PT[pti[0] % 3]
                        pti[0] += 1
                        self.ACT(pt[:, c0:512], sbk[:, c0:512], AF.Exp, [sbk], [pt], scale=ATT_SCALE)
                        if r_ >= 0:
                            self.TT("gpsimd", pt[:, c0:c0 + 128], pt[:, c0:c0 + 128], mask[:], ALU.mult, [pt, mask], [pt])
                        return pt, c0

                    def emit_PV(kt, pt, c0, hh=hh, OUT=OUT, DEN=DEN):
                        self.MM(OUT, OUT[:, c0:512], VV[:, kt, hh * 128:(hh + 1) * 128], pt[:, c0:512], kt == 0, kt == nkt - 1, [VVr[kt // 4], pt])
                        self.MM(DEN, DEN[:, c0:512], one1[:], pt[:, c0:512], kt == 0, kt == nkt - 1, [one1, pt])

                    prev = None
                    for kt in range(nkt):
                        cur = emit_S(kt)
                        if prev is not None:
                            emit_PV(kt - 1, *prev)
                        prev = cur
                    emit_PV(nkt - 1, *prev)
                    self.RCP(rden[:], DEN[:], [DEN], [rden])
                    a_ = ao[aoi[0] % 2]
                    aoi[0] += 1
                    self.TT("vector", a_[:], OUT[:], rden[:], ALU.mult, [OUT, rden], [a_])
                    blk = qt // 2
                    self.DMA(b_att[blk][hh * 128:(hh + 1) * 128, (qt % 2) * 512:(qt % 2 + 1) * 512], a_[:], [a_], [b_att[blk]])
                bank_i[0] = 0
            self.release(m)
            for i_ in range(8):
                P.cc(lambda e, i_=i_: e.collective_compute("AllGather", ALU.bypass, replica_groups=GROUPS, ins=[b_att[i_].ap], outs=[g_att[i_].ap]),
                     [b_att[i_].b], [g_att[i_].b])
            if l + 1 < L:
                gather_weights(l + 1, ["w_up", "w_down"])
            if self.stage == 2:
                for kc in range(16):
                    self.DMA(outT[kc], xres[kc], [xres], [outT])
                P.emit(final_wait_bufs=[outT.b] + [g_att[i_].b for i_ in range(8)])
                return
            m = self.mark()
            gs = self.sb("gs", [128, 4, 1024], F32)
            for r_ in range(4):
                self.DMA(gs[:, r_, :], g_st.ap[r_ * 128:(r_ + 1) * 128, :], [g_st], [gs])
            stmp = self.sb("stmp", [128, 8, 128], F32)
            for r_ in range(4):
                dst = Sst if r_ == 0 else stmp
                self.TT("vector", dst[:], gs[:, r_, :].rearrange("p (a b) -> p a b", a=8),
                        coef[:, r_, :].unsqueeze(2).to_broadcast([128, 8, 128]), ALU.mult, [gs, coef], [dst])
                if r_ > 0:
                    self.TT("vector", Sst[:], Sst[:], stmp[:], ALU.add, [Sst, stmp], [Sst])
            self.CP("vector", Sbf[:], Sst[:], [Sst], [Sbf])
            self.release(m)
            for h in range(2):
                m = self.mark()
                tsl = slice(h * HALF, (h + 1) * HALF)
                xacc = self.sb("xacc", [128, 16, HALF], F32)
                mixed = self.sb("mixed", [128, 16, HALF], BF16)
                xar = [self.regions(xacc, [f"xacc_{kc}_0", f"xacc_{kc}_1"]) for kc in range(16)]
                for kc in range(16):
                    self.DMA(xacc[:, kc, :], xres[kc][:, tsl], [xres], xar[kc])
                m2 = self.mark()
                rin = [[self.sb(f"rin{i}_{j}", [128, 1024], BF16) for j in range(4)] for i in range(2)]
                QTt = self.sb("QTt", [128, 8, 128], BF16)
                KTt = self.sb("KTt", [128, 8, 128], BF16)
                PTt = self.sb("PTt", [128, 8, 128], BF16)
                osb = self.sb("osb", [128, 8, 128], F32)
                osq = self.sb("osq", [128, 8, 128], F32)
                rr = self.sb("rr", [128, 8, 128], BF16)
                st8 = self.sb("st8", [128, 4, 8], F32)
                for t in range(8):
                    gt = h * 8 + t
                    q_, k_, v_, g_ = rin[t % 2]
                    self.DMA(q_[:], sc_q[gt], [sc_q], [q_])
                    self.DMA(k_[:], sc_k[gt], [sc_k], [k_])
                    self.DMA(v_[:], sc_v[gt], [sc_v], [v_])
                    self.DMA(g_[:], sc_g[gt], [sc_g], [g_])
                    bq, bkk = banks[0], banks[1]
                    bqv = bq[:].bitcast(BF16)
                    bkv = bkk[:].bitcast(BF16)
                    for hh in range(8):
                        self.TR(bq, bqv[:, hh * 128:(hh + 1) * 128], q_[:, hh * 128:(hh + 1) * 128], [q_])
                    for hh in range(8):
                        self.TR(bkk, bkv[:, hh * 128:(hh + 1) * 128], k_[:, hh * 128:(hh + 1) * 128], [k_])
                    self.CP("scalar", QTt[:].rearrange("p a b -> p (a b)"), bqv, [bq], [QTt])
                    self.CP("vector", KTt[:].rearrange("p a b -> p (a b)"), bkv, [bkk], [KTt])
                    for half8 in range(2):
                        bs = banks[2 + half8]
                        for hh in range(4):
                            hd_ = half8 * 4 + hh
                            self.MM(bs, bs[:, hh * 128:(hh + 1) * 128], KTt[:, hd_, :], QTt[:, hd_, :], True, True, [KTt, QTt])
                        self.TT("vector", PTt[:, half8 * 4:half8 * 4 + 4, :], bs[:].rearrange("p (a b) -> p a b", a=4),
                                mask[:].unsqueeze(1).to_broadcast([128, 4, 128]), ALU.mult, [bs, mask], [PTt])
                    for half8 in range(2):
                        bo = banks[4 + half8]
                        for hh in range(4):
                            hd_ = half8 * 4 + hh
                            self.MM(bo, bo[:, hh * 128:(hh + 1) * 128], PTt[:, hd_, :], v_[:, hd_ * 128:(hd_ + 1) * 128], True, False, [PTt, v_])
                            self.MM(bo, bo[:, hh * 128:(hh + 1) * 128], QTt[:, hd_, :], Sbf[:, hd_, :], False, True, [QTt, Sbf])
                        self.CP("scalar", osb[:, half8 * 4:half8 * 4 + 4, :], bo[:].rearrange("p (a b) -> p a b", a=4), [bo], [osb])
                    for half8 in range(2):
                        bm = banks[6 + half8]
                        for hh in range(4):
                            hd_ = half8 * 4 + hh
                            self.MM(bm, bm[:, hh * 128:(hh + 1) * 128], k_[:, hd_ * 128:(hd_ + 1) * 128], v_[:, hd_ * 128:(hd_ + 1) * 128],
                                    True, True, [k_, v_])
                        sv = Sst[:, half8 * 4:half8 * 4 + 4, :]
                        self.TT("vector", sv, sv, bm[:].rearrange("p (a b) -> p a b", a=4), ALU.add, [Sst, bm], [Sst])
                        self.TT("vector", sv, sv, gtab[:, half8 * 4:half8 * 4 + 4, :], ALU.mult, [Sst, gtab], [Sst])
                    self.CP("gpsimd", Sbf[:], Sst[:], [Sst], [Sbf])
                    self.RED("vector", st8[:, 0, :], osb[:], [osb], [st8])
                    self.ACT(osq[:], osb[:], AF.Square, [osb], [osq])
                    self.RED("vector", st8[:, 1, :], osq[:], [osq], [st8])
                    self.TS("vector", st8[:, 0, :], st8[:, 0, :], 1.0 / 128, ALU.mult, [st8], [st8])
                    self.TT("vector", st8[:, 2, :], st8[:, 0, :], st8[:, 0, :], ALU.mult, [st8], [st8])
                    self.STT("vector", st8[:, 1, :], st8[:, 1, :], 1.0 / 128, st8[:, 2, :], ALU.mult, ALU.subtract, [st8], [st8])
                    self.ACT(st8[:, 3, :], st8[:, 1, :], AF.Sqrt, [st8, cst], [st8], bias=cst[:, 0:1])
                    self.RCP(st8[:, 3, :], st8[:, 3, :], [st8], [st8])
                    self.TT("vector", osb[:], osb[:], st8[:, 0, :].unsqueeze(2).to_broadcast([128, 8, 128]), ALU.subtract, [osb, st8], [osb])
                    self.TT("gpsimd", osb[:], osb[:], st8[:, 3, :].unsqueeze(2).to_broadcast([128, 8, 128]), ALU.mult, [osb, st8], [osb])
                    self.TT("vector", rr[:].rearrange("p a b -> p (a b)"), osb[:].rearrange("p a b -> p (a b)"), g_[:], ALU.mult, [osb, g_], [rr])
                    br_ = banks[0]
                    brv = br_[:].bitcast(BF16)
                    for hh in range(8):
                        self.TR(br_, brv[:, hh * 128:(hh + 1) * 128], rr[:, hh, :], [rr])
                    self.TT("vector", mixed[:, 8:16, t * 128:(t + 1) * 128], brv.rearrange("p (a b) -> p a b", a=8),
                            gn[:, G_BR + l * 8:G_BR + l * 8 + 8].unsqueeze(2).to_broadcast([128, 8, 128]), ALU.mult, [br_, gn], [mixed])
                self.release(m2)
                m2 = self.mark()
                araw = self.sb("araw", [128, 8, HALF], BF16)
                asq = self.sb("asq", [128, 512], BF16)
                ars = self.sb("ars", [128, HALF], F32)
                g_att_v = g_att_all.rearrange("b (r c p) t -> b p (r c) t", r=4, c=2, p=128)
                for j_ in range(8):
                    def dyn(e, j_=j_, h=h):
                        blk = (P.pid % 4) * 2 + h
                        return e.dma_start(out=araw[:, j_, :], in_=g_att_v[bass.ds(blk, 1)].rearrange("o p j t -> p (o j) t")[:, j_, :])
                    P.dma("sync", dyn, [t_.b for t_ in g_att], [araw.b])
                for tt in range(2):
                    bk = nb()
                    for j in range(8):
                        self.ACT(asq[:], araw[:, j, tt * 512:(tt + 1) * 512], AF.Square, [araw], [asq])
                        self.MM(bk, bk[:], ones[:, 3, :], asq[:], j == 0, j == 7, [ones, asq])
                    self.rstd_from(ars, ars[:, tt * 512:(tt + 1) * 512], bk, bk[:])
                for j in range(8):
                    self.STT("vector", mixed[:, j, :], araw[:, j, :], gn[:, G_BA + l * 8 + j:G_BA + l * 8 + j + 1], ars[:],
                             ALU.mult, ALU.mult, [araw, gn, ars], [mixed])
                self.release(m2)
                m2 = self.mark()
                units = [(w_o, wu(w_o, oc), 16, 128) for oc in range(16)]

                def cons_o(k, sl, wv):
                    for tt in range(2):
                        bk = nb()
                        for mc in range(16):
                            self.MM(bk, bk[:], wv[:, mc, :], mixed[:, mc, tt * 512:(tt + 1) * 512], mc == 0, mc == 15, [sl, mixed])
                        xa = xacc[:, k, tt * 512:(tt + 1) * 512]
                        self.TT("vector", xa, xa, bk[:], ALU.add, [xar[k][tt], bk], [xar[k][tt]])
                self.stream(units, cons_o)
                xb = self.sb("xb2", [128, 16, HALF], BF16) if False else None
                self.release(m2)
                m2 = self.mark()
                sq2 = [self.sb(f"sq2_{i}", [128, 512], BF16) for i in range(2)]
                rstd2 = self.sb("rstd2", [128, HALF], F32)
                xb = mixed
                for tt in range(2):
                    bk = nb()
                    for kc in range(16):
                        sq = sq2[kc % 2]
                        self.ACT(sq[:], xacc[:, kc, tt * 512:(tt + 1) * 512], AF.Square, [xar[kc][tt]], [sq])
                        self.MM(bk, bk[:], ones[:, 0, :], sq[:], kc == 0, kc == 15, [ones, sq])
                    self.rstd_from(rstd2, rstd2[:, tt * 512:(tt + 1) * 512], bk, bk[:])
                xbr = self.regions(xb, [f"xbr{kc}" for kc in range(16)])
                for kc in range(16):
                    self.STT("vector", xb[:, kc, :], xacc[:, kc, :],
                             gn[:, G_MLP + l * 16 + kc:G_MLP + l * 16 + kc + 1], rstd2[:], ALU.mult, ALU.mult,
                             [xar[kc][0], xar[kc][1], gn, rstd2], [xbr[kc]])
                aT = [self.sb(f"aT{i}", [128, 4, HALF], BF16) for i in range(2)]
                aTr = [[self.regions(aT[i], [f"aT{i}_{j}_0", f"aT{i}_{j}_1"]) for j in range(4)] for i in range(2)]
                rl = [self.sb(f"rl{i}", [128, 512], BF16) for i in range(2)]
                rli = [0]

                upring = [Tl(self.wsl[i].ap, f"upr{i}") for i in range(2)]
                dnring = [Tl(self.wsl[2 + i // 2].ap[:, (i % 2) * 1024:(i % 2 + 1) * 1024], f"dnr{i}") for i in range(8)]
                up_h = {}
                upi = [0]

                def load_up(g_, j_):
                    if (g_, j_) in up_h or g_ >= 16:
                        return
                    sl = upring[upi[0] % 2]
                    upi[0] += 1
                    up_h[(g_, j_)] = self.wunit_to(w_up, wu(w_up, g_ * 4 + j_), 2048, 16, sl, sl.ap)

                dn_h = {}

                def load_dn(g_, ch_, j_):
                    sl = dnring[ch_ * 4 + j_]
                    dn_h[(g_, ch_, j_)] = self.wunit_to(w_down, wu(w_down, g_ * 4 + j_)[:, ch_ * 1024:(ch_ + 1) * 1024], 1024, 8, sl, sl.ap)

                def consume_up(grp):
                    a_ = aT[grp % 2]
                    for k in range(4):
                        load_up(grp, k)
                        if k < 3:
                            load_up(grp, k + 1)
                        if grp >= 1:
                            load_dn(grp - 1, 0, k)
                        sl, wv = up_h.pop((grp, k))
                        for tt in range(2):
                            bk = nb()
                            for kc in range(16):
                                self.MM(bk, bk[:], wv[:, kc, :], xb[:, kc, tt * 512:(tt + 1) * 512], kc == 0, kc == 15, [sl, xbr[kc]])
                            r1 = rl[rli[0] % 2]
                            rli[0] += 1
                            self.ACT(r1[:], bk[:], AF.Relu, [bk], [r1])
                            self.ACT(a_[:, k, tt * 512:(tt + 1) * 512], r1[:], AF.Square, [r1], [aTr[grp % 2][k][tt]])

                def consume_down(grp):
                    a_ = aT[grp % 2]
                    for j in range(4):
                        load_dn(grp, 1, j)
                    for ch in range(2):
                        if ch == 1:
                            load_up(grp + 2, 0)
                            load_up(grp + 2, 1)
                        got = [dn_h.pop((grp, ch, j)) for j in range(4)]
                        for o8 in range(8):
                            oc = ch * 8 + o8
                            for tt in range(2):
                                bk = nb()
                                for j in range(4):
                                    self.MM(bk, bk[:], got[j][1][:, o8, :], a_[:, j, tt * 512:(tt + 1) * 512], j == 0, j == 3,
                                            [got[j][0], aTr[grp % 2][j][tt]])
                                xa = xacc[:, oc, tt * 512:(tt + 1) * 512]
                                self.TT("vector", xa, xa, bk[:], ALU.add, [xar[oc][tt], bk], [xar[oc][tt]])

                consume_up(0)
                for grp in range(16):
                    if grp + 1 < 16:
                        consume_up(grp + 1)
                    else:
                        for j in range(4):
                            load_dn(grp, 0, j)
                    consume_down(grp)
                if l < L - 1:
                    for kc in range(16):
                        self.DMA(xres[kc][:, tsl], xacc[:, kc, :], xar[kc], [xres])
                else:
                    for tt in range(2):
                        bk = nb()
                        for kc in range(16):
                            sq = sq2[kc % 2]
                            self.ACT(sq[:], xacc[:, kc, tt * 512:(tt + 1) * 512], AF.Square, [xar[kc][tt]], [sq])
                            self.MM(bk, bk[:], ones[:, 0, :], sq[:], kc == 0, kc == 15, [ones, sq])
                        self.rstd_from(rstd2, rstd2[:, tt * 512:(tt + 1) * 512], bk, bk[:])
                    for kc in range(16):
                        self.STT("vector", xacc[:, kc, :], xacc[:, kc, :],
                                 gn[:, G_FIN + kc:G_FIN + kc + 1], rstd2[:], ALU.mult, ALU.mult, xar[kc] + [gn, rstd2], xar[kc])
                        self.DMA(outT[kc][:, tsl], xacc[:, kc, :], xar[kc], [outT])
                self.release(m2)
                self.release(m)
            self.release(lmark)
        P.emit(final_wait_bufs=[outT.b])


def _consts(g):
    c = np.zeros((128, 2048), np.float32)
    c[:, 0:128] = np.eye(128, dtype=np.float32)
    k = np.arange(128)[:, None]
    q = np.arange(128)[None, :]
    c[:, 128:256] = (q >= k).astype(np.float32)
    hh = np.arange(8, dtype=np.float64)
    gamma = 1.0 - np.exp2(-5.0 - hh)
    t = np.arange(128, dtype=np.float64)[:, None]
    c[:, 256:264] = (gamma[None, :] ** (t + 1.0)) * (128.0 ** -0.5)
    c[:, 264:272] = gamma[None, :] ** (-(t + 1.0))
    c[:, 272:1296] = np.repeat((gamma ** 128.0)[None, :], 128, axis=1).reshape(1, 1024).repeat(128, 0) if False else \
        np.broadcast_to(np.repeat(gamma ** 128.0, 128)[None, :], (128, 1024))
    coef = np.zeros((4, 8), np.float64)
    for i in range(4):
        if i < g:
            coef[i] = gamma ** (2048.0 * (g - 1 - i))
    c[:, 1296:1328] = np.broadcast_to(coef.reshape(1, 32), (128, 32))
    invf64 = (np.float32(10000.0) ** (-np.arange(0, 64, 2, dtype=np.float32) / np.float32(64))).astype(np.float32)
    invf128 = (np.float32(10000.0) ** (-np.arange(0, 128, 2, dtype=np.float32) / np.float32(128))).astype(np.float32)
    c[:, 1328:1360] = invf64[None, :]
    c[:, 1360:1424] = invf128[None, :]
    return c


def _gains(attn_norm, mlp_norm, q_norm, kv_norm, beta_attn, beta_ret, final_norm):
    gcols = np.zeros((128, 256), np.float32)
    for l in range(NL):
        gcols[:, 0 + l * 16:0 + (l + 1) * 16] = attn_norm[l].reshape(16, 128).T
        gcols[:, 64 + l * 16:64 + (l + 1) * 16] = mlp_norm[l].reshape(16, 128).T
        gcols[:, 128 + l * 4:128 + (l + 1) * 4] = q_norm[l].reshape(4, 128).T
        gcols[:, 144 + l * 2:144 + (l + 1) * 2] = kv_norm[l].reshape(2, 128).T
        gcols[:, 152 + l * 8:152 + (l + 1) * 8] = beta_attn[l].reshape(8, 128).T
        gcols[:, 184 + l * 8:184 + (l + 1) * 8] = beta_ret[l].reshape(8, 128).T
    gcols[:, 216:232] = final_norm.reshape(16, 128).T
    return gcols


_NC_CACHE = {}


def kernel(x, positions, attn_norm, w_in, q_norm, kv_norm, w_uq, w_ukv, beta_attn, beta_ret,
           w_o, mlp_norm, w_up, w_down, final_norm, _n_layers=NL, _stage=99):
    x = np.asarray(x, np.float32)
    positions = np.asarray(positions, np.int32)
    w_in = np.ascontiguousarray(np.asarray(w_in, np.float32))
    w_uq = np.asarray(w_uq, np.float32)
    w_ukv = np.asarray(w_ukv, np.float32)
    w_o = np.ascontiguousarray(np.asarray(w_o, np.float32))
    w_up = np.ascontiguousarray(np.asarray(w_up, np.float32))
    w_down = np.ascontiguousarray(np.asarray(w_down, np.float32))
    gains = _gains(*[np.asarray(a, np.float32) for a in (attn_norm, mlp_norm, q_norm, kv_norm, beta_attn, beta_ret, final_norm)])
    if (_n_layers, _stage) not in _NC_CACHE:
        _NC_CACHE[(_n_layers, _stage)] = Builder(_n_layers, _stage).build()
    nc = _NC_CACHE[(_n_layers, _stage)]
    def fm_units(w, ncolblk):
        nl, kdim, _ = w.shape
        kc_n = kdim // 128
        t = w[:, :, :ncolblk * 128].reshape(nl, kc_n, 128, ncolblk, 128).transpose(0, 3, 2, 1, 4)
        return t.reshape(nl, ncolblk, 128, kc_n * 128)
    w_in_t = np.zeros((NL, 40, 128, 2048), np.float32)
    w_in_t[:, 0:6] = fm_units(w_in[:, :, 0:768], 6)
    w_in_t[:, 6, :, 0:1024] = w_in[:, :, 768:832].reshape(NL, 16, 128, 64).transpose(0, 2, 1, 3).reshape(NL, 128, 1024)
    for gi in range(8):
        c0 = 832 + gi * 512
        blk = w_in[:, :, c0:c0 + 512].reshape(NL, 4, 4, 128, 512).transpose(0, 1, 3, 2, 4)
        w_in_t[:, 7 + gi * 4:7 + gi * 4 + 4] = blk.reshape(NL, 4, 128, 2048)
    w_in_t = w_in_t.reshape(NL, 40 * 128, 2048)
    w_o_t = np.ascontiguousarray(fm_units(w_o, 16)).reshape(NL, 16 * 128, 2048)
    w_up_t = np.ascontiguousarray(fm_units(w_up, 64)).reshape(NL, 64 * 128, 2048)
    in_maps = []
    for c in range(8):
        b, g = c // 4, c % 4
        xs = x[b, g * TOK:(g + 1) * TOK, :]
        xT = np.ascontiguousarray(xs.T).reshape(16, 128, TOK)
        po = np.ascontiguousarray(positions[b, g * TOK:(g + 1) * TOK].reshape(16, 128).T)
        pa = np.ascontiguousarray(positions[b].reshape(64, 128).T)
        h0, h1 = 2 * g, 2 * g + 1
        wuq_my = np.ascontiguousarray(np.concatenate(
            [w_uq[:, :, h0 * 192:h0 * 192 + 128], w_uq[:, :, h1 * 192:h1 * 192 + 128],
             w_uq[:, :, h0 * 192 + 128:(h0 + 1) * 192], w_uq[:, :, h1 * 192 + 128:(h1 + 1) * 192]], axis=2))
        wukv_my = np.ascontiguousarray(np.concatenate(
            [w_ukv[:, :, h0 * 256:h0 * 256 + 128], w_ukv[:, :, h1 * 256:h1 * 256 + 128],
             w_ukv[:, :, h0 * 256 + 128:(h0 + 1) * 256], w_ukv[:, :, h1 * 256 + 128:(h1 + 1) * 256]], axis=2))
        in_maps.append({
            "xT": xT, "pos_own": po, "pos_all": pa, "w_uq_my": wuq_my, "w_ukv_my": wukv_my,
            "w_in_t": w_in_t, "w_o_t": w_o_t, "w_up_t": w_up_t, "w_down_t": w_down,
            "gains": gains, "cfs": _consts(g),
        })
    res = run_bass_kernel_spmd(nc, in_maps, core_ids=list(range(8)))
    out = np.empty((2, S, D), np.float32)
    for c in range(8):
        b, g = c // 4, c % 4
        oT = np.asarray(res.results[c]["outT"]).reshape(D, TOK)
        out[b, g * TOK:(g + 1) * TOK, :] = oT.T
    return out
```
